# Optimizing a Trainium2 kernel written in Bass

```python
import math
import jax, jax.numpy as jnp
from jax import lax
import numpy as np

D_MODEL = 1024
BATCH = 2
SEQ = 16384
DEPTH = 4

CHUNK = 64
Q_BLOCK = 128
NORM_EPS = 1e-6
DA_HEADS = 4
DA_DK = 64
DA_DV = 2 * DA_DK
HG_HEADS = 4
HG_DK = 128
HG_DV = 128
ML_HEADS = 4
ML_DK = 128
ML_DV = 128
ML_CONV = 4
N_BRANCH = 3
BRANCH_W = 512
D_FF = 2816
N_EXPERTS = 8
TOP_K = 2
N_DENSE = (DEPTH + 1) // 2
N_MOE = DEPTH // 2

A_QK = DA_HEADS * DA_DK
COLS = (('a_q1', A_QK), ('a_q2', A_QK), ('a_k1', A_QK), ('a_k2', A_QK),
        ('a_v', DA_HEADS * DA_DV),
        ('b_q', HG_HEADS * HG_DK), ('b_f', HG_HEADS * HG_DK),
        ('b_i', HG_HEADS * HG_DV), ('b_g', HG_HEADS * HG_DV),
        ('c_qk', 2 * ML_HEADS * ML_DK), ('c_v', ML_HEADS * ML_DV),
        ('c_o', ML_HEADS * ML_DV), ('c_i', ML_HEADS), ('c_f', ML_HEADS),
        ('gate', N_BRANCH * D_MODEL))
COL_NAMES = tuple(n for n, _ in COLS)
COL_OFFSETS = tuple(int(v) for v in np.cumsum([s for _, s in COLS])[:-1])
D_IN = sum(s for _, s in COLS)

kernel_name = 'hybrid_diffattn_hgrn2_mlstm_moe_block'


def _rmsnorm(x, w):
    xf = x.astype(jnp.float32)
    y = xf * lax.rsqrt(jnp.mean(xf * xf, axis=-1, keepdims=True) + NORM_EPS)
    return (y * w.astype(jnp.float32)).astype(x.dtype)


def _modulate(xn, shift, scale):
    return xn * (1.0 + scale[:, None, :]) + shift[:, None, :]


def _swiglu(h, w1, w3, w2):
    return (jax.nn.silu(h @ w1) * (h @ w3)) @ w2


def _causal_conv(u, w, b):
    K = w.shape[0]
    S = u.shape[1]
    up = jnp.pad(u, ((0, 0), (K - 1, 0), (0, 0)))
    y = up[:, 0:S] * w[0] + b
    for j in range(1, K):
        y = y + up[:, j:j + S] * w[j]
    return y


def _diff_attention(q1, q2, k1, k2, v, lam, lam_init, qn_w, kn_w, subln_w):
    B, S, H, dk = q1.shape
    dv = v.shape[-1]
    f32 = jnp.float32
    q = _rmsnorm(jnp.stack([q1, q2], axis=2).astype(f32), qn_w) * (dk ** -0.5)
    k = _rmsnorm(jnp.stack([k1, k2], axis=2).astype(f32), kn_w)
    q = q.transpose(0, 3, 2, 1, 4)
    k = k.transpose(0, 3, 2, 1, 4)
    vf = v.astype(f32).transpose(0, 2, 1, 3)
    slopes = jnp.asarray(2.0 ** (-8.0 * np.arange(1, H + 1) / H), dtype=f32)
    kpos = jnp.arange(S)
    kchunk = kpos // CHUNK

    def block(j):
        start = j * Q_BLOCK
        qb = lax.dynamic_slice_in_dim(q, start, Q_BLOCK, axis=3)
        qpos = start + jnp.arange(Q_BLOCK)
        s = jnp.einsum('bhmqd,bhmkd->bhmqk', qb, k)
        dist = jnp.abs(qpos[:, None] - kpos[None, :]).astype(f32)
        mask = kchunk[None, :] <= (qpos // CHUNK)[:, None]
        s = jnp.where(mask, s - (slopes[:, None, None] * dist)[None, :, None], -jnp.inf)
        p = jax.nn.softmax(s, axis=-1)
        a = p[:, :, 0] - lam * p[:, :, 1]
        return jnp.einsum('bhqk,bhkd->bhqd', a, vf)

    o = lax.map(block, jnp.arange(S // Q_BLOCK))
    o = o.transpose(1, 0, 3, 2, 4).reshape(B, S, H, dv)
    o = _rmsnorm(o, subln_w) * (1.0 - lam_init)
    return o.reshape(B, S, H * dv).astype(v.dtype)


def _hgrn2(q, f_pre, i, g, lb, gnorm_w):
    B, S, H, dk = q.shape
    dv = i.shape[-1]
    n = S // CHUNK
    f32 = jnp.float32
    qf = jax.nn.silu(q.astype(f32))
    lb = lb.astype(f32).reshape(H, dk)
    logf = jnp.logaddexp(jnp.log(lb), jnp.log1p(-lb) + jax.nn.log_sigmoid(f_pre.astype(f32)))
    kf = -jnp.expm1(logf)
    vf = i.astype(f32)

    def to_chunks(t):
        return t.reshape(B, n, CHUNK, H, t.shape[-1]).transpose(1, 0, 3, 2, 4)

    causal = jnp.tril(jnp.ones((CHUNK, CHUNK), dtype=bool))

    def step(state, inp):
        qc, kc, vc, lfc = inp
        b = jnp.cumsum(lfc, axis=2)
        o_inter = jnp.einsum('bhtd,bhde->bhte', qc * jnp.exp(b), state)
        diff = b[:, :, :, None, :] - b[:, :, None, :, :]
        dec = jnp.exp(jnp.where(causal[:, :, None], diff, -jnp.inf))
        att = jnp.einsum('bhtd,bhsd,bhtsd->bhts', qc, kc, dec)
        o_intra = jnp.einsum('bhts,bhse->bhte', att, vc)
        b_last = b[:, :, -1:, :]
        state = (jnp.exp(b_last[:, :, 0, :])[..., None] * state
                 + jnp.einsum('bhsd,bhse->bhde', kc * jnp.exp(b_last - b), vc))
        return state, o_inter + o_intra

    s0 = jnp.zeros((B, H, dk, dv), f32)
    _, o = lax.scan(step, s0, (to_chunks(qf), to_chunks(kf), to_chunks(vf), to_chunks(logf)))
    o = o.transpose(1, 0, 3, 2, 4).reshape(B, S, H, dv)
    o = _rmsnorm(o, gnorm_w) * jax.nn.silu(g.astype(f32))
    return o.reshape(B, S, H * dv).astype(g.dtype)


def _mlstm(q, k, v, o_pre, i_pre, f_pre, norm_w):
    B, S, H, dk = q.shape
    dv = v.shape[-1]
    n = S // CHUNK
    f32 = jnp.float32
    qf = q.astype(f32)
    kf = k.astype(f32) * (dk ** -0.5)
    vf = v.astype(f32)
    ig = i_pre.astype(f32)
    lf = jax.nn.log_sigmoid(f_pre.astype(f32))

    def to_chunks(t):
        return t.reshape(B, n, CHUNK, H, t.shape[-1]).transpose(1, 0, 3, 2, 4)

    def gate_chunks(t):
        return t.reshape(B, n, CHUNK, H).transpose(1, 0, 3, 2)

    causal = jnp.tril(jnp.ones((CHUNK, CHUNK), dtype=bool))

    def step(carry, inp):
        C, nv, m = carry
        qc, kc, vc, igc, lfc = inp
        b = jnp.cumsum(lfc, axis=-1)
        log_d = jnp.where(causal, b[..., :, None] - b[..., None, :] + igc[..., None, :], -jnp.inf)
        log_inter = b + m[..., None]
        m_t = jnp.maximum(log_inter, jnp.max(log_d, axis=-1))
        w_intra = jnp.exp(log_d - m_t[..., None]) * jnp.einsum('bhtd,bhsd->bhts', qc, kc)
        w_inter = jnp.exp(log_inter - m_t)
        num = (w_inter[..., None] * jnp.einsum('bhtd,bhde->bhte', qc, C)
               + jnp.einsum('bhts,bhse->bhte', w_intra, vc))
        dot = w_inter * jnp.einsum('bhtd,bhd->bht', qc, nv) + jnp.sum(w_intra, axis=-1)
        h = num / jnp.maximum(jnp.abs(dot), jnp.exp(-m_t))[..., None]
        g = b[..., -1]
        log_w = g[..., None] - b + igc
        m_new = jnp.maximum(g + m, jnp.max(log_w, axis=-1))
        w_s = jnp.exp(log_w - m_new[..., None])
        decay = jnp.exp(g + m - m_new)
        C = decay[..., None, None] * C + jnp.einsum('bhs,bhsd,bhse->bhde', w_s, kc, vc)
        nv = decay[..., None] * nv + jnp.einsum('bhs,bhsd->bhd', w_s, kc)
        return (C, nv, m_new), h

    init = (jnp.zeros((B, H, dk, dv), f32), jnp.zeros((B, H, dk), f32), jnp.zeros((B, H), f32))
    _, h = lax.scan(step, init, (to_chunks(qf), to_chunks(kf), to_chunks(vf),
                                 gate_chunks(ig), gate_chunks(lf)))
    h = h.transpose(1, 0, 3, 2, 4).reshape(B, S, H, dv)
    h = _rmsnorm(h, norm_w) * jax.nn.sigmoid(o_pre.astype(f32))
    return h.reshape(B, S, H * dv).astype(v.dtype)


def _mixer(h, w_in, conv_w, conv_b, qn_w, kn_w, lq1, lk1, lq2, lk2, subln_w, lam_init,
           lb, gnorm_w, ib, fb, cnorm_w, w_branch, w_out):
    B, S, _ = h.shape
    f32 = jnp.float32
    parts = dict(zip(COL_NAMES, jnp.split(w_in, COL_OFFSETS, axis=1)))

    def proj(name):
        return h @ parts[name]

    def heads(t, nh):
        return t.reshape(B, S, nh, -1)

    lam = (jnp.exp(jnp.sum(lq1.astype(f32) * lk1.astype(f32)))
           - jnp.exp(jnp.sum(lq2.astype(f32) * lk2.astype(f32))) + lam_init)
    y_a = _diff_attention(heads(proj('a_q1'), DA_HEADS), heads(proj('a_q2'), DA_HEADS),
                          heads(proj('a_k1'), DA_HEADS), heads(proj('a_k2'), DA_HEADS),
                          heads(proj('a_v'), DA_HEADS), lam, lam_init, qn_w, kn_w, subln_w)
    y_b = _hgrn2(heads(proj('b_q'), HG_HEADS), heads(proj('b_f'), HG_HEADS),
                 heads(proj('b_i'), HG_HEADS), heads(proj('b_g'), HG_HEADS), lb, gnorm_w)
    qk = jax.nn.silu(_causal_conv(proj('c_qk'), conv_w, conv_b))
    c_q, c_k = jnp.split(qk, 2, axis=-1)
    y_c = _mlstm(heads(c_q, ML_HEADS), heads(c_k, ML_HEADS), heads(proj('c_v'), ML_HEADS),
                 heads(proj('c_o'), ML_HEADS), proj('c_i') + ib, proj('c_f') + fb, cnorm_w)
    gates = jax.nn.sigmoid(proj('gate')).reshape(B, S, N_BRANCH, D_MODEL)
    merged = (gates[:, :, 0] * (y_a @ w_branch[0])
              + gates[:, :, 1] * (y_b @ w_branch[1])
              + gates[:, :, 2] * (y_c @ w_branch[2]))
    return merged @ w_out


def _moe(h, router_w, router_b, w1, w3, w2):
    f32 = jnp.float32
    logits = (h @ router_w).astype(f32) + router_b.astype(f32)
    probs = jax.nn.softmax(logits, axis=-1)
    top_p, top_i = lax.top_k(probs, TOP_K)
    top_p = top_p / jnp.sum(top_p, axis=-1, keepdims=True)
    combine = jnp.sum(jax.nn.one_hot(top_i, N_EXPERTS, dtype=f32) * top_p[..., None], axis=-2)
    out = jnp.zeros_like(h)
    for e in range(N_EXPERTS):
        out = out + combine[..., e:e + 1].astype(h.dtype) * _swiglu(h, w1[e], w3[e], w2[e])
    return out


def setup_inputs(seed: int = 0) -> dict:
    key = jax.random.key(seed)
    ks = jax.random.split(key, 32)
    f32 = jnp.float32
    D = D_MODEL

    def nrm(k, shape, scale):
        return scale * jax.random.normal(k, shape, f32)

    def gain(k, shape):
        return 1.0 + 0.02 * jax.random.normal(k, shape, f32)

    return {
        'x': nrm(ks[0], (BATCH, SEQ, D), 1.0),
        'c': nrm(ks[1], (BATCH, D), 1.0),
        'w_mod': nrm(ks[2], (DEPTH, D, 6 * D), 0.5 * D ** -0.5),
        'b_mod': nrm(ks[3], (DEPTH, 6 * D), 0.02),
        'norm1_w': gain(ks[4], (DEPTH, D)),
        'norm2_w': gain(ks[5], (DEPTH, D)),
        'w_in': nrm(ks[6], (DEPTH, D, D_IN), D ** -0.5),
        'c_conv_w': nrm(ks[7], (DEPTH, ML_CONV, 2 * ML_HEADS * ML_DK), ML_CONV ** -0.5),
        'c_conv_b': nrm(ks[8], (DEPTH, 2 * ML_HEADS * ML_DK), 0.02),
        'a_qnorm_w': gain(ks[9], (DEPTH, DA_DK)),
        'a_knorm_w': gain(ks[10], (DEPTH, DA_DK)),
        'a_lambda_q1': nrm(ks[11], (DEPTH, DA_DK), 0.1),
        'a_lambda_k1': nrm(ks[12], (DEPTH, DA_DK), 0.1),
        'a_lambda_q2': nrm(ks[13], (DEPTH, DA_DK), 0.1),
        'a_lambda_k2': nrm(ks[14], (DEPTH, DA_DK), 0.1),
        'a_subln_w': gain(ks[15], (DEPTH, DA_DV)),
        'b_lb_logits': nrm(ks[16], (DEPTH, HG_HEADS * HG_DK), 1.0),
        'b_gnorm_w': gain(ks[17], (DEPTH, HG_DV)),
        'c_igate_b': nrm(ks[18], (DEPTH, ML_HEADS), 0.1),
        'c_fgate_b': jnp.linspace(3.0, 6.0, ML_HEADS, dtype=f32)[None, :] + nrm(ks[19], (DEPTH, ML_HEADS), 0.1),
        'c_norm_w': gain(ks[20], (DEPTH, ML_DV)),
        'w_branch': nrm(ks[21], (DEPTH, N_BRANCH, BRANCH_W, D), BRANCH_W ** -0.5),
        'w_out': nrm(ks[22], (DEPTH, D, D), D ** -0.5),
        'ffn_w1': nrm(ks[23], (N_DENSE, D, D_FF), D ** -0.5),
        'ffn_w3': nrm(ks[24], (N_DENSE, D, D_FF), D ** -0.5),
        'ffn_w2': nrm(ks[25], (N_DENSE, D_FF, D), D_FF ** -0.5),
        'moe_router_w': nrm(ks[26], (N_MOE, D, N_EXPERTS), D ** -0.5),
        'moe_router_b': nrm(ks[27], (N_MOE, N_EXPERTS), 0.01),
        'moe_w1': nrm(ks[28], (N_MOE, N_EXPERTS, D, D_FF), D ** -0.5),
        'moe_w3': nrm(ks[29], (N_MOE, N_EXPERTS, D, D_FF), D ** -0.5),
        'moe_w2': nrm(ks[30], (N_MOE, N_EXPERTS, D_FF, D), D_FF ** -0.5),
    }


def reference(x, c, w_mod, b_mod, norm1_w, norm2_w, w_in, c_conv_w, c_conv_b,
              a_qnorm_w, a_knorm_w, a_lambda_q1, a_lambda_k1, a_lambda_q2, a_lambda_k2,
              a_subln_w, b_lb_logits, b_gnorm_w, c_igate_b, c_fgate_b, c_norm_w,
              w_branch, w_out, ffn_w1, ffn_w3, ffn_w2,
              moe_router_w, moe_router_b, moe_w1, moe_w3, moe_w2):
    lb_all = jnp.cumsum(jax.nn.softmax(b_lb_logits.astype(jnp.float32), axis=0), axis=0)
    lb_all = lb_all - lb_all[:1]
    cond = jax.nn.silu(c)
    for l in range(DEPTH):
        mod = cond @ w_mod[l] + b_mod[l]
        sh1, sc1, g1, sh2, sc2, g2 = jnp.split(mod, 6, axis=-1)
        lam_init = 0.8 - 0.6 * math.exp(-0.3 * l)
        h = _modulate(_rmsnorm(x, norm1_w[l]), sh1, sc1)
        y = _mixer(h, w_in[l], c_conv_w[l], c_conv_b[l], a_qnorm_w[l], a_knorm_w[l],
                   a_lambda_q1[l], a_lambda_k1[l], a_lambda_q2[l], a_lambda_k2[l],
                   a_subln_w[l], lam_init, lb_all[l], b_gnorm_w[l],
                   c_igate_b[l], c_fgate_b[l], c_norm_w[l], w_branch[l], w_out[l])
        x = x + g1[:, None, :] * y
        h = _modulate(_rmsnorm(x, norm2_w[l]), sh2, sc2)
        if l % 2 == 0:
            i = l // 2
            y = _swiglu(h, ffn_w1[i], ffn_w3[i], ffn_w2[i])
        else:
            i = l // 2
            y = _moe(h, moe_router_w[i], moe_router_b[i], moe_w1[i], moe_w3[i], moe_w2[i])
        x = x + g2[:, None, :] * y
    return x
```

```python
import contextlib
import math
import numpy as np
import concourse.bass as bass
import concourse.mybir as mybir
from concourse.bass_utils import run_bass_kernel_spmd

F32 = mybir.dt.float32
BF16 = mybir.dt.bfloat16
ALU = mybir.AluOpType
AF = mybir.ActivationFunctionType

D = 1024
DEPTH = 4
SEQ = 16384
BATCH = 2
D_FF = 2816
NE = 8
EPS = 1e-6
D_IN = 8712


class Tok:
    __slots__ = ("w", "r")

    def __init__(self):
        self.w = None
        self.r = {}


class Chan:
    def __init__(self, sem, name):
        self.sem = sem
        self.count = 0
        self.name = name


class R:
    __slots__ = ("ap", "tok")

    def __init__(self, ap, tok):
        self.ap = ap
        self.tok = tok


class Buf:
    def __init__(self, t, tok=None):
        self.t = t
        self.tok = tok or Tok()

    def __getitem__(self, idx):
        return R(self.t[idx], self.tok)

    def v(self, idx, tok):
        return R(self.t[idx], tok)


def _ap(x):
    return x.ap if isinstance(x, R) else x


class Eng:
    def __init__(self, fw, e, name, sem):
        self.fw = fw
        self.e = e
        self.name = name
        self.ch = Chan(sem, name)
        self.seen = {}

    def _wait(self, deps):
        best = {}
        for d in deps:
            if d is None:
                continue
            ch, c = d
            if best.get(ch, 0) < c:
                best[ch] = c
        for ch, c in best.items():
            if ch is self.ch and self.name == "pe":
                continue
            if self.seen.get(ch, 0) >= c:
                continue
            self.e.wait_ge(ch.sem, c)
            self.seen[ch] = c

    @staticmethod
    def _deps(reads, writes):
        deps = []
        for t in reads:
            deps.append(t.w)
        for t in writes:
            deps.extend(t.r.items())
            deps.append(t.w)
        return deps

    def op(self, fn, reads=(), writes=()):
        self._wait(self._deps(reads, writes))
        inst = fn(self.e)
        self.ch.count += 1
        inst.then_inc(self.ch.sem, 1)
        me = (self.ch, self.ch.count)
        for t in reads:
            t.r[self.ch] = self.ch.count
        for t in writes:
            t.w = me
            t.r = {}
        return inst

    def dma(self, out, in_, **kw):
        reads = [in_.tok] if isinstance(in_, R) else []
        writes = [out.tok] if isinstance(out, R) else []
        ch = self.fw.next_dma_chan(self)
        deps = self._deps(reads, writes)
        if ch.count > 0:
            deps.append((ch, ch.count))
        self._wait(deps)
        inst = self.e.dma_start(out=_ap(out), in_=_ap(in_), **kw)
        ch.count += 16
        inst.then_inc(ch.sem, 16)
        for t in reads:
            t.r[ch] = ch.count
        for t in writes:
            t.w = (ch, ch.count)
            t.r = {}
        return inst


class FW:
    def __init__(self, nc, stack, n_dma_sems=16):
        self.nc = nc
        self.stack = stack
        self.block = stack.enter_context(nc.Block())
        mk = lambda n: stack.enter_context(nc.semaphore(n))
        self.pe = Eng(self, nc.tensor, "pe", mk("s_pe"))
        self.act = Eng(self, nc.scalar, "act", mk("s_act"))
        self.dve = Eng(self, nc.vector, "dve", mk("s_dve"))
        self.pool = Eng(self, nc.gpsimd, "pool", mk("s_pool"))
        self.sp = Eng(self, nc.sync, "sp", mk("s_sp"))
        self.dma_rings = {}
        for en in ("sp", "pool", "act"):
            self.dma_rings[en] = [[Chan(mk(f"s_dma_{en}{i}"), f"dma_{en}{i}") for i in range(n_dma_sems)], 0]
        self.dma_chans = [c for r in self.dma_rings.values() for c in r[0]]
        self.engs = [self.pe, self.act, self.dve, self.pool, self.sp]
        self._rr = 0

    def next_dma_chan(self, eng):
        ring = self.dma_rings[eng.name]
        ch = ring[0][ring[1] % len(ring[0])]
        ring[1] += 1
        return ch

    def sb(self, name, shape, dt=F32):
        return Buf(self.stack.enter_context(self.nc.sbuf_tensor("sb_" + name, list(shape), dt)))

    def ps(self, name, shape, dt=F32):
        return Buf(self.stack.enter_context(self.nc.psum_tensor("ps_" + name, list(shape), dt)))

    @staticmethod
    def _toks(*xs):
        return [x.tok for x in xs if isinstance(x, R)]

    def mm(self, out, lhsT, rhs, start=True, stop=True):
        return self.pe.op(lambda e: e.matmul(out.ap, lhsT.ap, rhs.ap, start=start, stop=stop),
                          self._toks(lhsT, rhs), [out.tok])

    def actv(self, out, in_, func, bias=0.0, scale=1.0):
        return self.act.op(
            lambda e: e.activation(out=out.ap, in_=in_.ap, func=func, bias=_ap(bias), scale=_ap(scale)),
            self._toks(in_, bias, scale), [out.tok])

    def tt(self, eng, out, in0, in1, op):
        return eng.op(lambda e: e.tensor_tensor(out=out.ap, in0=in0.ap, in1=in1.ap, op=op),
                      self._toks(in0, in1), [out.tok])

    def ts(self, eng, out, in0, s1, op0, s2=None, op1=None):
        if s2 is None:
            f = lambda e: e.tensor_scalar(out=out.ap, in0=in0.ap, scalar1=_ap(s1), scalar2=None, op0=op0)
        else:
            f = lambda e: e.tensor_scalar(out=out.ap, in0=in0.ap, scalar1=_ap(s1), scalar2=_ap(s2),
                                          op0=op0, op1=op1)
        return eng.op(f, self._toks(in0, s1, s2), [out.tok])

    def stt(self, eng, out, in0, scalar, in1, op0, op1):
        return eng.op(
            lambda e: e.scalar_tensor_tensor(out=out.ap, in0=in0.ap, scalar=_ap(scalar), in1=in1.ap,
                                             op0=op0, op1=op1),
            self._toks(in0, scalar, in1), [out.tok])

    def copy(self, eng, out, in_):
        if eng is self.act:
            return eng.op(lambda e: e.activation(out=out.ap, in_=in_.ap, func=AF.Copy),
                          self._toks(in_), [out.tok])
        return eng.op(lambda e: e.tensor_copy(out=out.ap, in_=in_.ap), self._toks(in_), [out.tok])

    def recip(self, out, in_):
        return self.dve.op(lambda e: e.reciprocal(out=out.ap, in_=in_.ap), self._toks(in_), [out.tok])

    def memset(self, eng, out, val):
        return eng.op(lambda e: e.memset(out.ap, val), [], [out.tok])

    def alt(self):
        self._rr += 1
        return self.dve if (self._rr & 1) else self.pool

    def barrier(self):
        for e in self.engs:
            deps = [(o.ch, o.ch.count) for o in self.engs if o is not e and o.ch.count > 0]
            deps += [(c, c.count) for c in self.dma_chans if c.count > 0]
            e._wait(deps)

    def finish(self, toks):
        self.sp._wait([t.w for t in toks])
        deps = [(o.ch, o.ch.count) for o in self.engs if o is not self.sp and o.ch.count > 0]
        deps += [(c, c.count) for c in self.dma_chans if c.count > 0]
        self.sp._wait(deps)


def rms_rstd(fw, out, ss_psum, n, tmp):
    fw.actv(tmp, ss_psum, AF.Ln, bias=EPS, scale=1.0 / n)
    fw.actv(out, tmp, AF.Exp, scale=-0.5)


A_CH = [("q1", 64), ("q2", 64), ("k1", 64), ("k2", 64), ("av", 128), ("bq", 128), ("bf", 128),
        ("bi", 128), ("bg", 128), ("cq", 128), ("ck", 128), ("cv", 128), ("co", 128),
        ("ci", 128), ("cf", 128)]
A_OFF = {}
_o = 0
for _n, _w in A_CH:
    A_OFF[_n] = (_o, _w)
    _o += _w
NCOL_A = _o
A_IDX = {n: i for i, (n, _) in enumerate(A_CH)}
NPV = 64
PV_COND, PV_NW, PV_BSH, PV_BSC = 0, 8, 16, 24
PV_QNW, PV_KNW, PV_SUBLN, PV_GNORM, PV_CNORM = 32, 33, 34, 35, 36
PV_CWQ, PV_CWK, PV_CBQ, PV_CBK = 37, 41, 45, 46
PV_LBL, PV_LMASK, PV_IB, PV_FB, PV_LAMI, PV_1MLAMI = 47, 51, 55, 56, 57, 58
NCST = 128 + 128 + 512 + 256
MASKVAL = -200.0


def build_A(S):
    NT = S // 512
    NB = S // 128
    nc = bass.Bass("TRN2", target_bir_lowering=False)

    def din(name, shape):
        return nc.dram_tensor(name, list(shape), F32, kind="ExternalInput").ap()

    xT = din("xT", [D, S])
    w_all = din("w_all", [D, NCOL_A])
    modw = din("modw", [D, 2 * D])
    pv_d = din("pv", [128, NPV])
    rowv = din("rowv", [1, 256])
    cst = din("cst", [128, NCST])
    bd_d = din("bd", [128, 4 * 512])
    alq = din("alq", [5, S])
    alk = din("alk", [5, S])
    y_d = nc.dram_tensor("y", [3 * 128, S], F32, kind="ExternalOutput").ap()
    xT_v = xT.rearrange("(c p) s -> p c s", p=128)

    with contextlib.ExitStack() as st:
        fw = FW(nc, st)
        dve, pool, act, pe, sp = fw.dve, fw.pool, fw.act, fw.pe, fw.sp
        pv = fw.sb("pv", [128, NPV])
        ident_f = fw.sb("ident_f", [128, 128])
        ident_b = fw.sb("ident_b", [128, 128], BF16)
        mask2 = fw.sb("mask2", [128, 128])
        rmask = fw.sb("rmask", [128, 512])
        maskD = fw.sb("maskD", [128, 128])
        maskX = fw.sb("maskX", [128, 128])
        ones_b = fw.sb("ones_b", [128, 128], BF16)
        ones_f = fw.sb("ones_f", [128, 128])
        bdt = fw.sb("bdt", [128, 4, 512])
        w_bf = fw.sb("w_bf", [128, 8, NCOL_A], BF16)
        ka = [fw.sb(f"ka{m}", [69, S], BF16) for m in range(2)]
        va = fw.sb("va", [128, NB, 128], BF16)
        qa = [fw.sb(f"qa{m}", [69, 512], BF16) for m in range(2)]
        ka_tok = [[Tok() for _ in range(NT)] for _ in range(2)]
        va_tok = [Tok() for _ in range(NT)]
        modv = fw.sb("modv", [128, 16])
        g1v = fw.sb("g1v", [128, 8])
        sh_b = fw.sb("sh_b", [128, 8], BF16)
        sh_rep = fw.sb("sh_rep", [128, 8, 128], BF16)
        bias_c = fw.sb("bias_c", [128, 16])
        bias_r = fw.sb("bias_r", [128, 3, 128])
        cvec = fw.sb("cvec", [128, 16])
        lamt = fw.sb("lamt", [1, 8])
        Sb = fw.sb("Sb", [128, 128])
        Sb_bf = fw.sb("Sb_bf", [128, 128], BF16)
        Sc = fw.sb("Sc", [128, 129])
        Sc_bf = fw.sb("Sc_bf", [128, 128], BF16)
        n_bc = fw.sb("n_bc", [128, 128], BF16)
        ucq = fw.sb("ucq", [128, 515])
        uck = fw.sb("uck", [128, 515])
        sc_ps = [fw.ps(f"sc{i}", [128, 512]) for i in range(2)]
        O_ps = fw.ps("O_ps", [128, 512])
        L_ps = fw.ps("L_ps", [128, 512])
        pj = fw.ps("pj", [128, 512])
        misc = fw.ps("misc", [128, 512])
        num_ps = fw.ps("num_ps", [128, 512])
        dot_ps = fw.ps("dot_ps", [128, 512])
        m_aT = misc
        m_st = misc
        m_tp = misc
        m_sm = misc
        x32 = [fw.sb(f"x32_{i}", [128, 512]) for i in range(2)]
        xsq = fw.sb("xsq", [128, 8, 512], BF16)
        xg = fw.sb("xg", [128, 8, 512], BF16)
        rstd = fw.sb("rstd", [128, 512])
        rcol = fw.sb("rcol", [128, 4])
        F = {}
        for nm in ["t0", "t1", "t2", "t3", "t4", "t5", "t6", "t7", "t8", "t9", "t10"]:
            F[nm] = fw.sb(nm, [128, 512])
        Bh = {}
        for nm in ["b0", "b1", "b2", "b3", "b4", "b5", "b6"]:
            Bh[nm] = fw.sb(nm, [128, 512], BF16)
        Pt = [fw.sb(f"Pt{i}", [128, 512], BF16) for i in range(3)]
        kst_tm = fw.sb("kst_tm", [128, 4, 128], BF16)
        vb_tm = fw.sb("vb_tm", [128, 4, 128], BF16)
        vc_tm = fw.sb("vc_tm", [128, 4, 129], BF16)
        A2 = [fw.sb(f"A2_{i}", [128, 128], BF16) for i in range(2)]
        dec = fw.sb("dec", [128, 8])
        _yo = fw.sb("yout0", [128, 512])
        yout = [_yo, _yo, _yo]

        sp.dma(pv[:, :], pv_d[:, :])
        sp.dma(ident_f[:, :], cst[:, 0:128])
        sp.dma(mask2[:, :], cst[:, 128:256])
        sp.dma(rmask[:, :], cst[:, 256:768])
        sp.dma(maskD[:, :], cst[:, 768:896])
        sp.dma(maskX[:, :], cst[:, 896:1024])
        sp.dma(bdt[:, :, :], bd_d.rearrange("p (j q) -> p j q", j=4))
        lamrow = Buf(F["t3"].t[0:1, 0:256], F["t3"].tok)
        sp.dma(lamrow[:, :], rowv[:, :])
        fw.memset(pool, ones_b[:, :], 1.0)
        fw.memset(pool, ones_f[:, :], 1.0)
        fw.copy(dve, ident_b[:, :], ident_f[:, :])
        fw.memset(dve, Sb[:, :], 0.0)
        fw.memset(dve, Sb_bf[:, :], 0.0)
        fw.memset(dve, Sc[:, :], 0.0)
        fw.memset(dve, Sc_bf[:, :], 0.0)
        fw.memset(pool, n_bc[:, :], 0.0)
        fw.memset(pool, ucq[:, 0:3], 0.0)
        fw.memset(pool, uck[:, 0:3], 0.0)
        fw.memset(pool, vc_tm[:, :, 128:129], 1.0)
        w_v = w_all.rearrange("(c p) n -> p c n", p=128)
        for kc in range(8):
            pool.dma(w_bf[:, kc, :], w_v[:, kc, :])
        for m in range(2):
            for tt_ in range(NT):
                pool.dma(ka[m].v((slice(64, 69), slice(512 * tt_, 512 * tt_ + 512)), ka_tok[m][tt_]),
                         alk[:, 512 * tt_:512 * tt_ + 512])
        cond = F["t2"]
        fw.actv(cond[:, 0:8], pv[:, PV_COND:PV_COND + 8], AF.Silu)
        modw_v = modw.rearrange("(c p) n -> p c n", p=128)
        for g in range(16):
            stg = [F["t0"], F["t1"]]
            for hh in range(2):
                sp.dma(R(stg[hh].t[:, :].rearrange("p (c n) -> p c n", c=4), stg[hh].tok),
                       modw_v[:, 4 * hh:4 * hh + 4, g * 128:(g + 1) * 128])
            for kc in range(8):
                fw.mm(m_sm[:, 448 + g:449 + g], stg[kc // 4][:, (kc % 4) * 128:(kc % 4 + 1) * 128],
                      cond[:, kc:kc + 1], start=(kc == 0), stop=(kc == 7))
        fw.tt(dve, modv[:, :], m_sm[:, 448:464], pv[:, PV_BSH:PV_BSH + 16], ALU.add)
        fw.stt(dve, g1v[:, :], modv[:, 8:16], 1.0, pv[:, PV_NW:PV_NW + 8], ALU.add, ALU.mult)
        fw.copy(dve, sh_b[:, :], modv[:, 0:8])
        for kc in range(8):
            fw.ts(fw.alt(), sh_rep[:, kc, :], ones_b[:, :], modv[:, kc:kc + 1], ALU.mult)
        for i, (nm, wd) in enumerate(A_CH):
            off = A_OFF[nm][0]
            for kc in range(8):
                fw.mm(m_sm[0:wd, 448 + i:449 + i], w_bf[:, kc, off:off + wd], sh_b[:, kc:kc + 1],
                      start=(kc == 0), stop=(kc == 7))
        fw.copy(dve, bias_c[:, 0:16], m_sm[:, 448:464])
        for i, nm in enumerate(["av", "bi", "cv"]):
            off = A_OFF[nm][0]
            for kc in range(8):
                fw.mm(m_aT[:, 0:128], sh_rep[:, kc, :], w_bf[:, kc, off:off + 128],
                      start=(kc == 0), stop=(kc == 7))
            fw.copy(dve, bias_r[:, i, :], m_aT[:, 0:128])
        fw.tt(dve, cvec[:, 7:8], bias_c[:, A_IDX["ci"]:A_IDX["ci"] + 1], pv[:, PV_IB:PV_IB + 1], ALU.add)
        fw.tt(dve, cvec[:, 8:9], bias_c[:, A_IDX["cf"]:A_IDX["cf"] + 1], pv[:, PV_FB:PV_FB + 1], ALU.add)
        fw.ts(dve, cvec[:, 0:1], pv[:, PV_QNW:PV_QNW + 1], 0.125, ALU.mult)
        fw.copy(dve, cvec[:, 1:2], pv[:, PV_KNW:PV_KNW + 1])
        lbe = F["t0"]
        fw.actv(lbe[:, 0:4], pv[:, PV_LBL:PV_LBL + 4], AF.Exp)
        fw.tt(dve, lbe[:, 4:8], lbe[:, 0:4], pv[:, PV_LMASK:PV_LMASK + 4], ALU.mult)
        dve.op(lambda e: e.reduce_sum(out=lbe.t[:, 8:9], in_=lbe.t[:, 0:4], axis=mybir.AxisListType.X),
               [lbe.tok], [lbe.tok])
        dve.op(lambda e: e.reduce_sum(out=lbe.t[:, 9:10], in_=lbe.t[:, 4:8], axis=mybir.AxisListType.X),
               [lbe.tok], [lbe.tok])
        fw.recip(lbe[:, 10:11], lbe[:, 8:9])
        fw.tt(dve, cvec[:, 2:3], lbe[:, 9:10], lbe[:, 10:11], ALU.mult)
        fw.ts(dve, cvec[:, 3:4], cvec[:, 2:3], -1.0, ALU.mult, 1.0, ALU.add)
        fw.tt(dve, lamrow[:, 0:64], lamrow[:, 0:64], lamrow[:, 64:128], ALU.mult)
        fw.tt(dve, lamrow[:, 128:192], lamrow[:, 128:192], lamrow[:, 192:256], ALU.mult)
        dve.op(lambda e: e.reduce_sum(out=lamt.t[:, 0:1], in_=lamrow.t[:, 0:64], axis=mybir.AxisListType.X),
               [lamrow.tok], [lamt.tok])
        dve.op(lambda e: e.reduce_sum(out=lamt.t[:, 1:2], in_=lamrow.t[:, 128:192], axis=mybir.AxisListType.X),
               [lamrow.tok], [lamt.tok])
        fw.actv(lamt[:, 2:4], lamt[:, 0:2], AF.Exp)
        fw.tt(dve, lamt[:, 4:5], lamt[:, 2:3], lamt[:, 3:4], ALU.subtract)
        fw.mm(m_sm[:, 470:471], ones_f[0:1, :], lamt[0:1, 4:5])
        fw.tt(dve, cvec[:, 4:5], m_sm[:, 470:471], pv[:, PV_LAMI:PV_LAMI + 1], ALU.add)
        fw.ts(dve, cvec[:, 5:6], cvec[:, 4:5], -1.0, ALU.mult)
        fw.tt(dve, cvec[:, 6:7], pv[:, PV_SUBLN:PV_SUBLN + 1], pv[:, PV_1MLAMI:PV_1MLAMI + 1], ALU.mult)

        def rmsnorm_out(o_sb, wcol, gate, yo, sqb):
            fw.actv(sqb[:, :], o_sb[:, :], AF.Square)
            fw.mm(pj[:, :], ones_b[:, :], sqb[:, :])
            rms_rstd(fw, F["t0"][:, :], pj[:, :], 128, F["t0"][:, :])
            fw.stt(dve, yo[:, :], o_sb[:, :], wcol, F["t0"][:, :], ALU.mult, ALU.mult)
            if gate is not None:
                fw.tt(pool, yo[:, :], yo[:, :], gate[:, :], ALU.mult)

        for t in range(NT):
            c0 = 512 * t
            for m in range(2):
                pool.dma(qa[m][64:69, :], alq[:, c0:c0 + 512])
            for kc in range(8):
                xs = x32[kc % 2]
                sp.dma(xs[:, :], xT_v[:, kc, c0:c0 + 512])
                fw.actv(xsq[:, kc, :], xs[:, :], AF.Square)
                fw.ts(fw.alt(), xg[:, kc, :], xs[:, :], g1v[:, kc:kc + 1], ALU.mult)
            for kc in range(8):
                fw.mm(pj[:, :], ones_b[:, :], xsq[:, kc, :], start=(kc == 0), stop=(kc == 7))
            rms_rstd(fw, rstd[:, :], pj[:, :], D, F["t0"][:, :])
            for blk in range(4):
                for kc in range(8):
                    fw.mm(m_sm[:, 448 + blk:449 + blk], xsq[:, kc, blk * 128:(blk + 1) * 128],
                          ones_b[:, 0:1], start=(kc == 0), stop=(kc == 7))
            rms_rstd(fw, rcol[:, :], m_sm[:, 448:452], D, F["t0"][:, 0:4])

            def proj_fm(nm, dst):
                off, wd = A_OFF[nm]
                for kc in range(8):
                    fw.mm(pj[0:wd, :], w_bf[:, kc, off:off + wd], xg[:, kc, :],
                          start=(kc == 0), stop=(kc == 7))
                fw.tt(dve, dst[0:wd, :], pj[0:wd, :], rstd[0:wd, :], ALU.mult)

            def bcol(nm, wd=128):
                i = A_IDX[nm]
                return bias_c[0:wd, i:i + 1]

            def proj_tm(nm, bi, dst3, ncols=128):
                off, wd = A_OFF[nm]
                for blk in range(4):
                    for kc in range(8):
                        fw.mm(m_aT[:, 0:128], xg[:, kc, blk * 128:(blk + 1) * 128], w_bf[:, kc, off:off + 128],
                              start=(kc == 0), stop=(kc == 7))
                    fw.stt(dve, dst3[:, blk, 0:128], m_aT[:, 0:128], rcol[:, blk:blk + 1],
                           bias_r[:, bi, :], ALU.mult, ALU.add)

            for nm, m, isq in [("q1", 0, True), ("q2", 1, True), ("k1", 0, False), ("k2", 1, False)]:
                raw = F["t1"]
                proj_fm(nm, raw)
                fw.ts(pool, raw[0:64, :], raw[0:64, :], bcol(nm, 64), ALU.add)
                sqb = Bh["b0"]
                fw.actv(sqb[0:64, :], raw[0:64, :], AF.Square)
                fw.mm(pj[0:64, :], ones_b[0:64, 0:64], sqb[0:64, :])
                rms_rstd(fw, F["t2"][0:64, :], pj[0:64, :], 64, F["t2"][0:64, :])
                if isq:
                    fw.stt(dve, qa[m][0:64, :], raw[0:64, :], cvec[0:64, 0:1], F["t2"][0:64, :],
                           ALU.mult, ALU.mult)
                else:
                    fw.stt(dve, ka[m].v((slice(0, 64), slice(c0, c0 + 512)), ka_tok[m][t]),
                           raw[0:64, :], cvec[0:64, 1:2], F["t2"][0:64, :], ALU.mult, ALU.mult)
            va_t = Buf(va.t, va_tok[t])
            off = A_OFF["av"][0]
            for blk in range(4):
                for kc in range(8):
                    fw.mm(m_aT[:, 0:128], xg[:, kc, blk * 128:(blk + 1) * 128], w_bf[:, kc, off:off + 128],
                          start=(kc == 0), stop=(kc == 7))
                fw.stt(dve, va_t[:, 4 * t + blk, :], m_aT[:, 0:128], rcol[:, blk:blk + 1],
                       bias_r[:, 0, :], ALU.mult, ALU.add)

            qf, ff, logf, bb, kmid, est, gs = F["t1"], F["t2"], F["t3"], F["t4"], F["t5"], F["t6"], F["t7"]
            proj_fm("bq", qf)
            fw.actv(qf[:, :], qf[:, :], AF.Silu, bias=bcol("bq"))
            proj_fm("bg", gs)
            fw.actv(gs[:, :], gs[:, :], AF.Silu, bias=bcol("bg"))
            proj_fm("bf", ff)
            fw.actv(ff[:, :], ff[:, :], AF.Sigmoid, bias=bcol("bf"))
            fw.ts(dve, ff[:, :], ff[:, :], cvec[:, 3:4], ALU.mult, cvec[:, 2:3], ALU.add)
            fw.actv(logf[:, :], ff[:, :], AF.Ln)
            fw.ts(pool, ff[:, :], ff[:, :], -1.0, ALU.mult, 1.0, ALU.add)
            dve.op(lambda e: e.tensor_tensor_scan(out=bb.t[:, :], data0=rmask.t[:, :], data1=logf.t[:, :],
                                                  initial=0.0, op0=ALU.mult, op1=ALU.add),
                   [rmask.tok, logf.tok], [bb.tok])
            qin_b, kst_b = Bh["b1"], Bh["b4"]
            fw.actv(est[:, :], bb[:, :], AF.Exp)
            fw.tt(dve, qin_b[:, :], qf[:, :], est[:, :], ALU.mult)
            qx_b, kx_b = Bh["b2"], Bh["b3"]
            qd_b, kd_b = Bh["b5"], Bh["b6"]
            for c in range(8):
                cs = slice(64 * c, 64 * c + 64)
                fw.actv(kmid[:, cs], bb[:, cs], AF.Exp, bias=bb[:, 64 * c + 31:64 * c + 32], scale=-1.0)
            fw.stt(dve, kx_b[:, :], kmid[:, :], 1.0, ff[:, :], ALU.min, ALU.mult)
            fw.recip(kmid[:, :], kmid[:, :])
            fw.stt(dve, qx_b[:, :], kmid[:, :], 1.0, qf[:, :], ALU.min, ALU.mult)
            for c in range(16):
                cs = slice(32 * c, 32 * c + 32)
                fw.actv(kmid[:, cs], bb[:, cs], AF.Exp, bias=bb[:, 32 * c + 15:32 * c + 16], scale=-1.0)
            fw.tt(dve, kd_b[:, :], ff[:, :], kmid[:, :], ALU.mult)
            fw.recip(kmid[:, :], kmid[:, :])
            fw.tt(pool, qd_b[:, :], qf[:, :], kmid[:, :], ALU.mult)
            for c in range(8):
                cs = slice(64 * c, 64 * c + 64)
                fw.actv(est[:, cs], bb[:, cs], AF.Exp, bias=bb[:, 64 * c + 63:64 * c + 64], scale=-1.0)
            fw.tt(dve, kst_b[:, :], ff[:, :], est[:, :], ALU.mult)
            fw.actv(dec[:, :], R(bb.t[:, :].rearrange("p (c l) -> p c l", l=64)[:, :, 63], bb.tok), AF.Exp)
            proj_tm("bi", 1, vb_tm)
            for blk in range(4):
                fw.mm(m_tp[:, 320:448], kst_b[:, blk * 128:(blk + 1) * 128], ident_b[:, :])
                fw.copy(act, kst_tm[:, blk, :], m_tp[:, 320:448])
            for blk in range(4):
                bs = slice(128 * blk, 128 * blk + 128)
                fw.mm(m_aT[:, 0:128], kd_b[:, bs], qd_b[:, bs])
                fw.mm(dot_ps[:, 0:128], kx_b[:, bs], qx_b[:, bs])
                a2 = A2[blk % 2]
                fw.tt(dve, F["t8"][:, 0:128], m_aT[:, 0:128], maskD[:, :], ALU.mult)
                fw.tt(dve, F["t8"][:, 128:256], dot_ps[:, 0:128], maskX[:, :], ALU.mult)
                fw.tt(dve, a2[:, :], F["t8"][:, 0:128], F["t8"][:, 128:256], ALU.add)
                for half in range(2):
                    c = 2 * blk + half
                    cs = slice(64 * c, 64 * c + 64)
                    p0 = 64 * half
                    fw.mm(num_ps[:, cs], Sb_bf[:, :], qin_b[:, cs], start=True, stop=False)
                    fw.mm(num_ps[:, cs], vb_tm[:, blk, :], a2[:, 64 * half:64 * half + 64], start=False, stop=True)
                    fw.mm(m_st[:, 128:256], kst_tm[p0:p0 + 64, blk, :], vb_tm[p0:p0 + 64, blk, :])
                    fw.stt(dve, Sb[:, :], Sb[:, :], dec[:, c:c + 1], m_st[:, 128:256], ALU.mult, ALU.add)
                    fw.copy(act, Sb_bf[:, :], Sb[:, :])
            o_b = F["t5"]
            fw.copy(act, o_b[:, :], num_ps[:, :])
            rmsnorm_out(o_b, pv[:, PV_GNORM:PV_GNORM + 1], gs, yout[1], Bh["b0"])
            sp.dma(y_d[128:256, c0:c0 + 512], yout[1][:, :])

            qc, kc_, ipre, lf, bc = F["t1"], F["t2"], F["t3"], F["t4"], F["t6"]
            og = F["t7"]
            for nm, ub, cw, cb, dst in [("cq", ucq, PV_CWQ, PV_CBQ, qc), ("ck", uck, PV_CWK, PV_CBK, kc_)]:
                proj_fm(nm, F["t8"])
                fw.ts(pool, ub[:, 3:515], F["t8"][:, :], bcol(nm), ALU.add)
                fw.ts(dve, dst[:, :], ub[:, 0:512], pv[:, cw:cw + 1], ALU.mult, pv[:, cb:cb + 1], ALU.add)
                for j in range(1, 4):
                    fw.stt(dve, dst[:, :], ub[:, j:j + 512], pv[:, cw + j:cw + j + 1], dst[:, :],
                           ALU.mult, ALU.add)
                fw.actv(dst[:, :], dst[:, :], AF.Silu)
                fw.copy(pool, F["t8"][:, 0:3], ub[:, 512:515])
                fw.copy(pool, ub[:, 0:3], F["t8"][:, 0:3])
            proj_fm("co", og)
            fw.actv(og[:, :], og[:, :], AF.Sigmoid, bias=bcol("co"))
            proj_fm("ci", ipre)
            fw.ts(pool, ipre[:, :], ipre[:, :], cvec[:, 7:8], ALU.add)
            proj_fm("cf", lf)
            fw.actv(lf[:, :], lf[:, :], AF.Sigmoid, bias=cvec[:, 8:9])
            fw.actv(lf[:, :], lf[:, :], AF.Ln)
            dve.op(lambda e: e.tensor_tensor_scan(out=bc.t[:, :], data0=rmask.t[:, :], data1=lf.t[:, :],
                                                  initial=0.0, op0=ALU.mult, op1=ALU.add),
                   [rmask.tok, lf.tok], [bc.tok])
            eqc, imb, estc = F["t8"], F["t9"], F["t10"]
            qt_b, kt_b, kstc_b = Bh["b1"], Bh["b2"], Bh["b4"]
            fw.actv(eqc[:, :], bc[:, :], AF.Exp)
            fw.tt(dve, qt_b[:, :], qc[:, :], eqc[:, :], ALU.mult)
            fw.tt(pool, imb[:, :], ipre[:, :], bc[:, :], ALU.subtract)
            fw.actv(eqc[:, :], imb[:, :], AF.Exp)
            fw.stt(dve, kt_b[:, :], kc_[:, :], 128.0 ** -0.5, eqc[:, :], ALU.mult, ALU.mult)
            for c in range(8):
                cs = slice(64 * c, 64 * c + 64)
                fw.actv(estc[:, cs], imb[:, cs], AF.Exp, bias=bc[:, 64 * c + 63:64 * c + 64])
            fw.stt(dve, kstc_b[:, :], kc_[:, :], 128.0 ** -0.5, estc[:, :], ALU.mult, ALU.mult)
            fw.actv(dec[:, :], R(bc.t[:, :].rearrange("p (c l) -> p c l", l=64)[:, :, 63], bc.tok), AF.Exp)
            proj_tm("cv", 2, vc_tm)
            for blk in range(4):
                fw.mm(m_tp[:, 320:448], kstc_b[:, blk * 128:(blk + 1) * 128], ident_b[:, :])
                fw.copy(act, kst_tm[:, blk, :], m_tp[:, 320:448])
            for blk in range(4):
                bs = slice(128 * blk, 128 * blk + 128)
                fw.mm(m_aT[:, 0:128], kt_b[:, bs], qt_b[:, bs])
                a2 = A2[blk % 2]
                fw.tt(dve, a2[:, :], m_aT[:, 0:128], mask2[:, :], ALU.mult)
                for half in range(2):
                    c = 2 * blk + half
                    cs = slice(64 * c, 64 * c + 64)
                    hs = slice(64 * half, 64 * half + 64)
                    p0 = 64 * half
                    fw.mm(num_ps[:, cs], Sc_bf[:, :], qt_b[:, cs], start=True, stop=False)
                    fw.mm(num_ps[:, cs], vc_tm[:, blk, 0:128], a2[:, hs], start=False, stop=True)
                    fw.mm(dot_ps[:, cs], n_bc[:, :], qt_b[:, cs], start=True, stop=False)
                    fw.mm(dot_ps[:, cs], ones_b[:, :], a2[:, hs], start=False, stop=True)
                    fw.mm(m_st[:, 128:257], kst_tm[p0:p0 + 64, blk, :], vc_tm[p0:p0 + 64, blk, :])
                    fw.stt(dve, Sc[:, :], Sc[:, :], dec[:, c:c + 1], m_st[:, 128:257], ALU.mult, ALU.add)
                    fw.copy(act, Sc_bf[:, :], Sc[:, 0:128])
                    fw.ts(pool, n_bc[:, :], ones_b[:, :], Sc[:, 128:129], ALU.mult)
            den, h_b = F["t8"], F["t9"]
            fw.ts(dve, den[:, :], dot_ps[:, :], -1.0, ALU.mult, 1.0, ALU.max)
            fw.tt(dve, den[:, :], den[:, :], dot_ps[:, :], ALU.max)
            fw.recip(den[:, :], den[:, :])
            fw.tt(dve, h_b[:, :], num_ps[:, :], den[:, :], ALU.mult)
            rmsnorm_out(h_b, pv[:, PV_CNORM:PV_CNORM + 1], og, yout[2], Bh["b0"])
            sp.dma(y_d[256:384, c0:c0 + 512], yout[2][:, :])

            om = [F["t1"], F["t2"]]
            nblk = 4 * (t + 1)
            pi = 0
            for m in range(2):
                for j in range(nblk):
                    scb = sc_ps[j % 2]
                    P = Pt[pi % 3]
                    pi += 1
                    tj = j // 4
                    kreg = lambda rows: ka[m].v((rows, slice(128 * j, 128 * j + 128)), ka_tok[m][tj])
                    if j < 4 * t:
                        fw.mm(scb[:, :], kreg(slice(0, 69)), qa[m][0:69, :])
                        fw.actv(P[:, :], scb[:, :], AF.Exp)
                    else:
                        jj = j - 4 * t
                        fw.mm(scb[:, :], kreg(slice(0, 64)), qa[m][0:64, :])
                        fw.tt(dve, F["t10"][:, :], scb[:, :], bdt[:, jj, :], ALU.add)
                        fw.actv(P[:, :], F["t10"][:, :], AF.Exp)
                    vreg = va.v((slice(None), j, slice(None)), va_tok[tj])
                    fw.mm(O_ps[:, :], vreg, P[:, :], start=(j == 0), stop=(j == nblk - 1))
                    fw.mm(L_ps[:, :], ones_b[:, :], P[:, :], start=(j == 0), stop=(j == nblk - 1))
                fw.recip(F["t10"][:, :], L_ps[:, :])
                fw.tt(dve, om[m][:, :], O_ps[:, :], F["t10"][:, :], ALU.mult)
            o_a = F["t4"]
            fw.stt(dve, o_a[:, :], om[1][:, :], cvec[:, 5:6], om[0][:, :], ALU.mult, ALU.add)
            rmsnorm_out(o_a, cvec[:, 6:7], None, yout[0], Bh["b0"])
            sp.dma(y_d[0:128, c0:c0 + 512], yout[0][:, :])

        fw.finish([yout[0].tok, yout[1].tok, yout[2].tok])
    return nc


def _pk(v):
    return np.ascontiguousarray(np.asarray(v, np.float32).reshape(8, 128).T)


def consts_A(h, S):
    slope = 2.0 ** (-2.0 * (h + 1))
    cst = np.zeros((128, NCST), np.float32)
    cst[:, 0:128] = np.eye(128, dtype=np.float32)
    s = np.arange(128)[:, None]
    t = np.arange(128)[None, :]
    cst[:, 128:256] = ((s // 64 == t // 64) & (s <= t)).astype(np.float32)
    rm = np.ones(512, np.float32)
    rm[::64] = 0.0
    cst[:, 256:768] = rm[None, :]
    cst[:, 768:896] = ((s // 32 == t // 32) & (s <= t)).astype(np.float32)
    cst[:, 896:1024] = ((s // 64 == t // 64) & (s % 64 < 32) & (t % 64 >= 32)).astype(np.float32)
    kk = np.arange(128)[:, None]
    qq = np.arange(512)[None, :]
    bd = np.zeros((128, 4, 512), np.float32)
    for jj in range(4):
        kpos = 128 * jj + kk
        allowed = (kpos // 64) <= (qq // 64)
        bd[:, jj, :] = np.where(allowed, -slope * np.abs(qq - kpos), MASKVAL)
    pos = np.arange(S)
    one = np.ones(S)
    alq = np.stack([-slope * 512.0 * (pos // 512), -slope * 256.0 * ((pos % 512) // 256),
                    -slope * (pos % 256), one, one]).astype(np.float32)
    alk = np.stack([one, one, one, slope * 128.0 * (pos // 128), slope * (pos % 128)]).astype(np.float32)
    return cst, np.ascontiguousarray(bd.reshape(128, 2048)), alq, alk


def prep_A(inp, l, b, h, xT_b, S):
    w = inp["w_in"][l]
    cols = []
    for nm, wd in A_CH:
        if nm in ("q1", "q2", "k1", "k2"):
            base = {"q1": 0, "q2": 256, "k1": 512, "k2": 768}[nm] + h * 64
            cols.append(w[:, base:base + 64])
        elif nm == "ci":
            cols.append(np.repeat(w[:, 5632 + h:5633 + h], 128, axis=1))
        elif nm == "cf":
            cols.append(np.repeat(w[:, 5636 + h:5637 + h], 128, axis=1))
        else:
            base = {"av": 1024, "bq": 1536, "bf": 2048, "bi": 2560, "bg": 3072, "cq": 3584,
                    "ck": 4096, "cv": 4608, "co": 5120}[nm] + h * 128
            cols.append(w[:, base:base + 128])
    w_all = np.ascontiguousarray(np.concatenate(cols, axis=1), dtype=np.float32)
    pv = np.zeros((128, NPV), np.float32)
    pv[:, PV_COND:PV_COND + 8] = _pk(inp["c"][b])
    pv[:, PV_NW:PV_NW + 8] = _pk(inp["norm1_w"][l])
    pv[:, PV_BSH:PV_BSH + 8] = _pk(inp["b_mod"][l][0:1024])
    pv[:, PV_BSC:PV_BSC + 8] = _pk(inp["b_mod"][l][1024:2048])
    pv[:, PV_QNW] = np.tile(inp["a_qnorm_w"][l], 2)
    pv[:, PV_KNW] = np.tile(inp["a_knorm_w"][l], 2)
    pv[:, PV_SUBLN] = inp["a_subln_w"][l]
    pv[:, PV_GNORM] = inp["b_gnorm_w"][l]
    pv[:, PV_CNORM] = inp["c_norm_w"][l]
    for j in range(4):
        pv[:, PV_CWQ + j] = inp["c_conv_w"][l][j, h * 128:(h + 1) * 128]
        pv[:, PV_CWK + j] = inp["c_conv_w"][l][j, 512 + h * 128:512 + (h + 1) * 128]
    pv[:, PV_CBQ] = inp["c_conv_b"][l][h * 128:(h + 1) * 128]
    pv[:, PV_CBK] = inp["c_conv_b"][l][512 + h * 128:512 + (h + 1) * 128]
    pv[:, PV_LBL:PV_LBL + 4] = inp["b_lb_logits"][:, h * 128:(h + 1) * 128].T
    for j in range(4):
        pv[:, PV_LMASK + j] = 1.0 if 1 <= j <= l else 0.0
    pv[:, PV_IB] = inp["c_igate_b"][l][h]
    pv[:, PV_FB] = inp["c_fgate_b"][l][h]
    lam_init = 0.8 - 0.6 * math.exp(-0.3 * l)
    pv[:, PV_LAMI] = lam_init
    pv[:, PV_1MLAMI] = 1.0 - lam_init
    rowv = np.concatenate([inp["a_lambda_q1"][l], inp["a_lambda_k1"][l],
                           inp["a_lambda_q2"][l], inp["a_lambda_k2"][l]]).astype(np.float32)[None, :]
    cst, bd, alq, alk = consts_A(h, S)
    return {"xT": np.ascontiguousarray(xT_b, dtype=np.float32), "w_all": w_all,
            "modw": np.ascontiguousarray(inp["w_mod"][l][:, 0:2048], dtype=np.float32),
            "pv": pv, "rowv": np.ascontiguousarray(rowv), "cst": cst, "bd": bd, "alq": alq, "alk": alk}


NPVB = 80
PB_COND, PB_NW1, PB_NW2, PB_BMOD = 0, 8, 16, 24
FC_GROUPS = [(0, 4), (4, 4), (8, 4), (12, 4), (16, 4), (20, 2)]


def build_B(SC, moe):
    NEX = NE if moe else 1
    ST = 2048 if SC >= 2048 else SC
    NST = SC // ST
    TPS = ST // 512
    nc = bass.Bass("TRN2", target_bir_lowering=False)

    def din(name, shape):
        return nc.dram_tensor(name, list(shape), F32, kind="ExternalInput").ap()

    xT = din("xT", [D, SC])
    yT = din("yT", [1536, SC])
    wg = din("wg", [D, 3072])
    wbr = din("wbr", [1536, D])
    wout = din("wout", [D, D])
    modw = din("modw", [D, 6 * D])
    pv_d = din("pv", [128, NPVB])
    w1 = din("w1", [NEX * D, D_FF])
    w3 = din("w3", [NEX * D, D_FF])
    w2 = din("w2", [NEX * D_FF, D])
    ident_d = din("ident", [128, 128])
    if moe:
        wr = din("wr", [D, 8])
        rb_d = din("rb", [8, 1])
        selm_d = din("selm", [8, 8 * 128])
    xo = nc.dram_tensor("xo", [D, SC], F32, kind="ExternalOutput").ap()
    xT_v = xT.rearrange("(c p) s -> p c s", p=128)
    xo_v = xo.rearrange("(c p) s -> p c s", p=128)
    yT_v = yT.rearrange("(j p) s -> p j s", p=128)
    wg_v = wg.rearrange("(c p) n -> p c n", p=128)
    w1_v = w1.rearrange("(e c p) n -> p e c n", p=128, c=8)
    w3_v = w3.rearrange("(e c p) n -> p e c n", p=128, c=8)
    w2_v = w2.rearrange("(e f p) n -> p e f n", p=128, f=22)
    modw_v = modw.rearrange("(c p) n -> p c n", p=128)

    with contextlib.ExitStack() as st:
        fw = FW(nc, st)
        dve, pool, act, pe, sp = fw.dve, fw.pool, fw.act, fw.pe, fw.sp
        pv = fw.sb("pv", [128, NPVB])
        ones_b = fw.sb("ones_b", [128, 128], BF16)
        ident_f = fw.sb("ident_f", [128, 128])
        modv = fw.sb("modv", [128, 48])
        g1v = fw.sb("g1v", [128, 8])
        g2v = fw.sb("g2v", [128, 8])
        sh1_b = fw.sb("sh1_b", [128, 8], BF16)
        sh2_b = fw.sb("sh2_b", [128, 8], BF16)
        bias_g = fw.sb("bias_g", [128, 24])
        x1buf = fw.sb("x1buf", [128, 8, ST])
        xg2 = fw.sb("xg2", [128, 8, ST], BF16)
        rstd2 = fw.sb("rstd2", [128, ST])
        x1_tok = [Tok() for _ in range(TPS)]
        xg2_tok = [Tok() for _ in range(TPS)]
        r2_tok = [Tok() for _ in range(TPS)]
        T = {nm: fw.sb(nm, [128, 512]) for nm in ["u0", "u1", "u2", "u3"]}
        if moe:
            wr_f = fw.sb("wr_f", [128, 8, 8])
            wr_s = fw.sb("wr_s", [128, 8, 8])
            sh2_f = fw.sb("sh2_f", [128, 8])
            rbias = fw.sb("rbias", [8, 2])
            combT = fw.sb("combT", [8, ST])
            cb_tok = [Tok() for _ in range(TPS)]
            rt = fw.sb("rt", [128, 64])
        pA = [fw.ps(f"pA{i}", [128, 512]) for i in range(2)]
        pB = [fw.ps(f"pB{i}", [128, 512]) for i in range(2)]
        pC = [fw.ps(f"pC{i}", [128, 512]) for i in range(2)]
        pM = fw.ps("pM", [128, 512])
        pBC = fw.ps("pBC", [128, 512])

        sp.dma(pv[:, :], pv_d[:, :])
        sp.dma(ident_f[:, :], ident_d[:, :])
        fw.memset(pool, ones_b[:, :], 1.0)
        cond = T["u2"]
        fw.actv(cond[:, 0:8], pv[:, PB_COND:PB_COND + 8], AF.Silu)
        for g in range(48):
            stg = [T["u0"], T["u1"]]
            for hh in range(2):
                sp.dma(R(stg[hh].t[:, :].rearrange("p (c n) -> p c n", c=4), stg[hh].tok),
                       modw_v[:, 4 * hh:4 * hh + 4, g * 128:(g + 1) * 128])
            for kc in range(8):
                fw.mm(pM[:, g:g + 1], stg[kc // 4][:, (kc % 4) * 128:(kc % 4 + 1) * 128],
                      cond[:, kc:kc + 1], start=(kc == 0), stop=(kc == 7))
        fw.tt(dve, modv[:, :], pM[:, 0:48], pv[:, PB_BMOD:PB_BMOD + 48], ALU.add)
        fw.stt(dve, g1v[:, :], modv[:, 8:16], 1.0, pv[:, PB_NW1:PB_NW1 + 8], ALU.add, ALU.mult)
        fw.stt(dve, g2v[:, :], modv[:, 32:40], 1.0, pv[:, PB_NW2:PB_NW2 + 8], ALU.add, ALU.mult)
        fw.copy(dve, sh1_b[:, :], modv[:, 0:8])
        fw.copy(dve, sh2_b[:, :], modv[:, 24:32])
        if moe:
            sp.dma(wr_f[:, :, :], wr.rearrange("(c p) e -> p c e", p=128))
            sp.dma(rbias[:, 0:1], rb_d[:, :])
            fw.copy(dve, sh2_f[:, :], modv[:, 24:32])
            for kc in range(8):
                fw.ts(dve, wr_s[:, kc, :], wr_f[:, kc, :], g2v[:, kc:kc + 1], ALU.mult)
                fw.mm(pM[0:8, 60:61], wr_f[:, kc, :], sh2_f[:, kc:kc + 1], start=(kc == 0), stop=(kc == 7))
            fw.tt(dve, rbias[:, 1:2], pM[0:8, 60:61], rbias[:, 0:1], ALU.add)

        wgc = None
        for s_i in range(NST):
            with contextlib.ExitStack() as st1:
                top_stack = fw.stack
                fw.stack = st1
                wbr_bf = fw.sb(f"wbr_bf{s_i}", [128, 12, D], BF16)
                wout_bf = fw.sb(f"wout_bf{s_i}", [128, 8, D], BF16)
                wgc = [fw.sb(f"wgc{s_i}_{i}", [128, 8, 384], BF16) for i in range(2)]
                xsq = fw.sb(f"xsq{s_i}", [128, 8, 512], BF16)
                xg1 = fw.sb(f"xg1{s_i}", [128, 8, 512], BF16)
                ybf = fw.sb(f"ybf{s_i}", [128, 12, 512], BF16)
                mrg = xsq
                rstd1 = fw.sb(f"rstd1{s_i}", [128, 512])
                fw.stack = top_stack
                for j in range(12):
                    pool.dma(wbr_bf[:, j, :], wbr[j * 128:(j + 1) * 128, :])
                for kc in range(8):
                    pool.dma(wout_bf[:, kc, :], wout[kc * 128:(kc + 1) * 128, :])
                wi = 0

                def load_wgc(mc):
                    nonlocal wi
                    buf = wgc[wi % 2]
                    wi += 1
                    for br in range(3):
                        pool.dma(buf[:, :, br * 128:(br + 1) * 128],
                                 wg_v[:, :, br * 1024 + mc * 128:br * 1024 + (mc + 1) * 128])
                    return buf

                if s_i == 0:
                    for mc in range(8):
                        buf = load_wgc(mc)
                        for br in range(3):
                            for kc in range(8):
                                fw.mm(pM[:, 64 + br * 8 + mc:65 + br * 8 + mc], buf[:, kc, br * 128:(br + 1) * 128],
                                      sh1_b[:, kc:kc + 1], start=(kc == 0), stop=(kc == 7))
                    fw.copy(dve, bias_g[:, :], pM[:, 64:88])
                for tl in range(TPS):
                    c0 = s_i * ST + tl * 512
                    cs = slice(tl * 512, tl * 512 + 512)
                    x1r = lambda kc: x1buf.v((slice(None), kc, cs), x1_tok[tl])
                    for kc in range(8):
                        sp.dma(x1r(kc), xT_v[:, kc, c0:c0 + 512])
                    for kc in range(8):
                        fw.actv(xsq[:, kc, :], x1r(kc), AF.Square)
                        fw.ts(fw.alt(), xg1[:, kc, :], x1r(kc), g1v[:, kc:kc + 1], ALU.mult)
                    for kc in range(8):
                        fw.mm(pM[:, :], ones_b[:, :], xsq[:, kc, :], start=(kc == 0), stop=(kc == 7))
                    rms_rstd(fw, rstd1[:, :], pM[:, :], D, T["u0"][:, :])
                    for j in range(12):
                        pool.dma(ybf[:, j, :], yT_v[:, j, c0:c0 + 512])
                    for mc in range(8):
                        buf = load_wgc(mc)
                        mg = T["u1"]
                        for br in range(3):
                            pa, pb = pA[br % 2], pB[br % 2]
                            for kc in range(8):
                                fw.mm(pa[:, :], buf[:, kc, br * 128:(br + 1) * 128], xg1[:, kc, :],
                                      start=(kc == 0), stop=(kc == 7))
                            for k4 in range(4):
                                fw.mm(pb[:, :], wbr_bf[:, br * 4 + k4, mc * 128:(mc + 1) * 128], ybf[:, br * 4 + k4, :],
                                      start=(k4 == 0), stop=(k4 == 3))
                            gt = T["u2"]
                            fw.tt(dve, gt[:, :], pa[:, :], rstd1[:, :], ALU.mult)
                            fw.actv(gt[:, :], gt[:, :], AF.Sigmoid, bias=bias_g[:, br * 8 + mc:br * 8 + mc + 1])
                            if br == 0:
                                fw.tt(dve, mg[:, :], gt[:, :], pb[:, :], ALU.mult)
                            else:
                                tmp = T["u3"]
                                fw.tt(dve, tmp[:, :], gt[:, :], pb[:, :], ALU.mult)
                                if br == 1:
                                    fw.tt(pool, mg[:, :], mg[:, :], tmp[:, :], ALU.add)
                                else:
                                    fw.tt(pool, mrg[:, mc, :], mg[:, :], tmp[:, :], ALU.add)
                    for mc in range(8):
                        po = pC[mc % 2]
                        for kc in range(8):
                            fw.mm(po[:, :], wout_bf[:, kc, mc * 128:(mc + 1) * 128], mrg[:, kc, :],
                                  start=(kc == 0), stop=(kc == 7))
                        fw.stt(dve, x1r(mc), po[:, :], modv[:, 16 + mc:17 + mc], x1r(mc), ALU.mult, ALU.add)
                    for kc in range(8):
                        fw.actv(xsq[:, kc, :], x1r(kc), AF.Square)
                        fw.ts(fw.alt(), xg2.v((slice(None), kc, cs), xg2_tok[tl]), x1r(kc), g2v[:, kc:kc + 1], ALU.mult)
                    for kc in range(8):
                        fw.mm(pM[:, :], ones_b[:, :], xsq[:, kc, :], start=(kc == 0), stop=(kc == 7))
                    r2 = rstd2.v((slice(None), cs), r2_tok[tl])
                    rms_rstd(fw, r2, pM[:, :], D, T["u0"][:, :])
                    if moe:
                        for kc in range(8):
                            fw.mm(pM[0:8, :], wr_s[:, kc, :], x1r(kc), start=(kc == 0), stop=(kc == 7))
                        lg = T["u2"]
                        fw.tt(dve, lg[0:8, :], pM[0:8, :], R(rstd2.t[0:8, cs], r2_tok[tl]), ALU.mult)
                        fw.ts(dve, lg[0:8, :], lg[0:8, :], rbias[:, 1:2], ALU.add)
                        for blk in range(4):
                            bs = slice(blk * 128, blk * 128 + 128)
                            fw.mm(pM[:, 0:8], lg[0:8, bs], ident_f[0:8, 0:8])
                            L8 = rt[:, 0:8]
                            fw.copy(dve, L8, pM[:, 0:8])
                            dve.op(lambda e: e.reduce_max(out=rt.t[:, 8:9], in_=rt.t[:, 0:8], axis=mybir.AxisListType.X),
                                   [rt.tok], [rt.tok])
                            fw.ts(dve, rt[:, 16:24], rt[:, 0:8], rt[:, 8:9], ALU.is_equal, -1e30, ALU.mult)
                            fw.tt(dve, rt[:, 16:24], rt[:, 16:24], rt[:, 0:8], ALU.add)
                            dve.op(lambda e: e.reduce_max(out=rt.t[:, 9:10], in_=rt.t[:, 16:24], axis=mybir.AxisListType.X),
                                   [rt.tok], [rt.tok])
                            fw.ts(dve, rt[:, 24:32], rt[:, 0:8], rt[:, 9:10], ALU.is_ge)
                            fw.ts(dve, rt[:, 10:11], rt[:, 8:9], -1.0, ALU.mult)
                            fw.actv(rt[:, 32:40], rt[:, 0:8], AF.Exp, bias=rt[:, 10:11])
                            fw.tt(dve, rt[:, 32:40], rt[:, 32:40], rt[:, 24:32], ALU.mult)
                            dve.op(lambda e: e.reduce_sum(out=rt.t[:, 11:12], in_=rt.t[:, 32:40], axis=mybir.AxisListType.X),
                                   [rt.tok], [rt.tok])
                            fw.recip(rt[:, 12:13], rt[:, 11:12])
                            fw.ts(dve, rt[:, 40:48], rt[:, 32:40], rt[:, 12:13], ALU.mult)
                            fw.mm(pM[0:8, 128:256], rt[:, 40:48], ident_f[:, :])
                            fw.copy(dve, combT.v((slice(None), slice(tl * 512 + blk * 128, tl * 512 + blk * 128 + 128)), cb_tok[tl]),
                                    pM[0:8, 128:256])
                fw.barrier()
            with contextlib.ExitStack() as st2:
                top_stack = fw.stack
                fw.stack = st2
                w1g = [fw.sb(f"w1g{s_i}_{i}", [128, 8, 512], BF16) for i in range(2)]
                w3g = [fw.sb(f"w3g{s_i}_{i}", [128, 8, 512], BF16) for i in range(2)]
                w2g = [fw.sb(f"w2g{s_i}_{i}", [128, 4, D], BF16) for i in range(2)]
                hh = [fw.sb(f"hh{s_i}_{i}", [128, 4, 512], BF16) for i in range(2)]
                bfc = [fw.sb(f"bfc{s_i}_{i}", [128, 8]) for i in range(2)]
                fw.stack = top_stack
                gi = 0
                hi = 0
                for e in range(NEX):
                    for (f0, fn) in FC_GROUPS:
                        b_ = gi % 2
                        gi += 1
                        pool.dma(w1g[b_][:, :, 0:fn * 128], w1_v[:, e, :, f0 * 128:(f0 + fn) * 128])
                        pool.dma(w3g[b_][:, :, 0:fn * 128], w3_v[:, e, :, f0 * 128:(f0 + fn) * 128])
                        pool.dma(w2g[b_][:, 0:fn, :], w2_v[:, e, f0:f0 + fn, :])
                        for f in range(fn):
                            for kc in range(8):
                                fw.mm(pM[:, f:f + 1], w1g[b_][:, kc, f * 128:(f + 1) * 128], sh2_b[:, kc:kc + 1],
                                      start=(kc == 0), stop=(kc == 7))
                            for kc in range(8):
                                fw.mm(pM[:, 4 + f:5 + f], w3g[b_][:, kc, f * 128:(f + 1) * 128], sh2_b[:, kc:kc + 1],
                                      start=(kc == 0), stop=(kc == 7))
                        fw.copy(dve, bfc[b_][:, :], pM[:, 0:8])
                        for tl in range(TPS):
                            cs = slice(tl * 512, tl * 512 + 512)
                            r2 = rstd2.v((slice(None), cs), r2_tok[tl])
                            hb = hh[hi % 2]
                            hi += 1
                            if moe:
                                fw.mm(pBC[:, :], R(ident_f.t[0:8, e:e + 1].to_broadcast([8, 128]), ident_f.tok),
                                      combT.v((slice(None), cs), cb_tok[tl]))
                            for f in range(fn):
                                pa, pb = pA[f % 2], pB[f % 2]
                                for kc in range(8):
                                    fw.mm(pa[:, :], w1g[b_][:, kc, f * 128:(f + 1) * 128],
                                          xg2.v((slice(None), kc, cs), xg2_tok[tl]), start=(kc == 0), stop=(kc == 7))
                                for kc in range(8):
                                    fw.mm(pb[:, :], w3g[b_][:, kc, f * 128:(f + 1) * 128],
                                          xg2.v((slice(None), kc, cs), xg2_tok[tl]), start=(kc == 0), stop=(kc == 7))
                                t1, t3 = T["u2"], T["u3"]
                                fw.tt(dve, t1[:, :], pa[:, :], r2, ALU.mult)
                                fw.actv(t1[:, :], t1[:, :], AF.Silu, bias=bfc[b_][:, f:f + 1])
                                fw.tt(dve, t3[:, :], pb[:, :], r2, ALU.mult)
                                fw.ts(pool, t3[:, :], t3[:, :], bfc[b_][:, 4 + f:5 + f], ALU.add)
                                if moe:
                                    fw.tt(pool, t3[:, :], t3[:, :], t1[:, :], ALU.mult)
                                    fw.tt(dve, hb[:, f, :], t3[:, :], pBC[:, :], ALU.mult)
                                else:
                                    fw.tt(pool, hb[:, f, :], t3[:, :], t1[:, :], ALU.mult)
                            for mc in range(8):
                                po = pC[mc % 2]
                                for f in range(fn):
                                    fw.mm(po[:, :], w2g[b_][:, f, mc * 128:(mc + 1) * 128], hb[:, f, :],
                                          start=(f == 0), stop=(f == fn - 1))
                                xr = x1buf.v((slice(None), mc, cs), x1_tok[tl])
                                fw.stt(dve, xr, po[:, :], modv[:, 40 + mc:41 + mc], xr, ALU.mult, ALU.add)
                for tl in range(TPS):
                    c0 = s_i * ST + tl * 512
                    cs = slice(tl * 512, tl * 512 + 512)
                    for kc in range(8):
                        sp.dma(xo_v[:, kc, c0:c0 + 512], x1buf.v((slice(None), kc, cs), x1_tok[tl]))
                fw.barrier()
        fw.finish(x1_tok)
    return nc


def prep_B(inp, l, b, xT_slice, yT_slice, moe):
    i2 = l // 2
    pv = np.zeros((128, NPVB), np.float32)
    pv[:, PB_COND:PB_COND + 8] = _pk(inp["c"][b])
    pv[:, PB_NW1:PB_NW1 + 8] = _pk(inp["norm1_w"][l])
    pv[:, PB_NW2:PB_NW2 + 8] = _pk(inp["norm2_w"][l])
    for k in range(6):
        pv[:, PB_BMOD + 8 * k:PB_BMOD + 8 * k + 8] = _pk(inp["b_mod"][l][k * 1024:(k + 1) * 1024])
    m = {"xT": np.ascontiguousarray(xT_slice, dtype=np.float32),
         "yT": np.ascontiguousarray(yT_slice, dtype=np.float32),
         "wg": np.ascontiguousarray(inp["w_in"][l][:, 5640:8712]),
         "wbr": np.ascontiguousarray(inp["w_branch"][l].reshape(1536, D)),
         "wout": np.ascontiguousarray(inp["w_out"][l]),
         "modw": np.ascontiguousarray(inp["w_mod"][l]),
         "pv": pv, "ident": np.eye(128, dtype=np.float32)}
    if moe:
        m["w1"] = np.ascontiguousarray(inp["moe_w1"][i2].reshape(NE * D, D_FF))
        m["w3"] = np.ascontiguousarray(inp["moe_w3"][i2].reshape(NE * D, D_FF))
        m["w2"] = np.ascontiguousarray(inp["moe_w2"][i2].reshape(NE * D_FF, D))
        m["wr"] = np.ascontiguousarray(inp["moe_router_w"][i2])
        m["rb"] = np.ascontiguousarray(inp["moe_router_b"][i2].reshape(8, 1))
        selm = np.zeros((8, 8 * 128), np.float32)
        for e in range(8):
            selm[e, e * 128:(e + 1) * 128] = 1.0
        m["selm"] = selm
    else:
        m["w1"] = np.ascontiguousarray(inp["ffn_w1"][i2])
        m["w3"] = np.ascontiguousarray(inp["ffn_w3"][i2])
        m["w2"] = np.ascontiguousarray(inp["ffn_w2"][i2])
    return m


_NC_CACHE = {}


def _get_nc(key):
    if key not in _NC_CACHE:
        if key == "A":
            _NC_CACHE[key] = build_A(SEQ)
        elif key == "Bd":
            _NC_CACHE[key] = build_B(SEQ // 4, False)
        else:
            _NC_CACHE[key] = build_B(SEQ // 4, True)
    return _NC_CACHE[key]


def kernel(**inputs):
    inp = {k: np.asarray(v) for k, v in inputs.items()}
    x = inp["x"]
    S = x.shape[1]
    SC = S // 4
    xT = [np.ascontiguousarray(x[b].T.astype(np.float32)) for b in range(BATCH)]
    cores = list(range(8))
    for l in range(DEPTH):
        moe = (l % 2 == 1)
        in_maps = [prep_A(inp, l, c // 4, c % 4, xT[c // 4], S) for c in cores]
        res = run_bass_kernel_spmd(_get_nc("A"), in_maps, core_ids=cores).results
        yT = []
        for b in range(BATCH):
            rows = [res[b * 4 + h]["y"][br * 128:(br + 1) * 128] for br in range(3) for h in range(4)]
            yT.append(np.concatenate(rows, axis=0))
        del res, in_maps
        in_maps = []
        for c in cores:
            b, s = c // 4, c % 4
            in_maps.append(prep_B(inp, l, b, xT[b][:, s * SC:(s + 1) * SC], yT[b][:, s * SC:(s + 1) * SC], moe))
        res = run_bass_kernel_spmd(_get_nc("Bm" if moe else "Bd"), in_maps, core_ids=cores).results
        for c in cores:
            b, s = c // 4, c % 4
            xT[b][:, s * SC:(s + 1) * SC] = res[c]["xo"]
        del res, in_maps
    out = np.stack([np.ascontiguousarray(xT[b].T) for b in range(BATCH)], axis=0)
    return out.astype(np.float32)
```

```python
import contextlib
import math
import numpy as np
import concourse.bass as bass
import concourse.mybir as mybir
from concourse.bass_utils import run_bass_kernel_spmd

F32 = mybir.dt.float32
BF16 = mybir.dt.bfloat16
ALU = mybir.AluOpType
AF = mybir.ActivationFunctionType

D = 1024
DEPTH = 4
SEQ = 16384
BATCH = 2
D_FF = 2816
NE = 8
EPS = 1e-6
D_IN = 8712


class Tok:
    __slots__ = ("w", "r")

    def __init__(self):
        self.w = None
        self.r = {}


class Chan:
    def __init__(self, sem, name):
        self.sem = sem
        self.count = 0
        self.name = name


class R:
    __slots__ = ("ap", "tok")

    def __init__(self, ap, tok):
        self.ap = ap
        self.tok = tok


class Buf:
    def __init__(self, t, tok=None):
        self.t = t
        self.tok = tok or Tok()

    def __getitem__(self, idx):
        return R(self.t[idx], self.tok)

    def v(self, idx, tok):
        return R(self.t[idx], tok)


def _ap(x):
    return x.ap if isinstance(x, R) else x


class Eng:
    def __init__(self, fw, e, name, sem):
        self.fw = fw
        self.e = e
        self.name = name
        self.ch = Chan(sem, name)
        self.seen = {}

    def _wait(self, deps):
        best = {}
        for d in deps:
            if d is None:
                continue
            ch, c = d
            if best.get(ch, 0) < c:
                best[ch] = c
        for ch, c in best.items():
            if ch is self.ch and self.name == "pe":
                continue
            if self.seen.get(ch, 0) >= c:
                continue
            self.e.wait_ge(ch.sem, c)
            self.seen[ch] = c

    @staticmethod
    def _deps(reads, writes):
        deps = []
        for t in reads:
            deps.append(t.w)
        for t in writes:
            deps.extend(t.r.items())
            deps.append(t.w)
        return deps

    def op(self, fn, reads=(), writes=()):
        self._wait(self._deps(reads, writes))
        inst = fn(self.e)
        self.ch.count += 1
        inst.then_inc(self.ch.sem, 1)
        me = (self.ch, self.ch.count)
        for t in reads:
            t.r[self.ch] = self.ch.count
        for t in writes:
            t.w = me
            t.r = {}
        return inst

    def dma(self, out, in_, **kw):
        reads = [in_.tok] if isinstance(in_, R) else []
        writes = [out.tok] if isinstance(out, R) else []
        ch = self.fw.next_dma_chan(self)
        deps = self._deps(reads, writes)
        if ch.count > 0:
            deps.append((ch, ch.count))
        self._wait(deps)
        inst = self.e.dma_start(out=_ap(out), in_=_ap(in_), **kw)
        ch.count += 16
        inst.then_inc(ch.sem, 16)
        for t in reads:
            t.r[ch] = ch.count
        for t in writes:
            t.w = (ch, ch.count)
            t.r = {}
        return inst


class FW:
    def __init__(self, nc, stack, n_dma_sems=16):
        self.nc = nc
        self.stack = stack
        self.block = stack.enter_context(nc.Block())
        mk = lambda n: stack.enter_context(nc.semaphore(n))
        self.pe = Eng(self, nc.tensor, "pe", mk("s_pe"))
        self.act = Eng(self, nc.scalar, "act", mk("s_act"))
        self.dve = Eng(self, nc.vector, "dve", mk("s_dve"))
        self.pool = Eng(self, nc.gpsimd, "pool", mk("s_pool"))
        self.sp = Eng(self, nc.sync, "sp", mk("s_sp"))
        self.dma_rings = {}
        for en in ("sp", "pool", "act"):
            self.dma_rings[en] = [[Chan(mk(f"s_dma_{en}{i}"), f"dma_{en}{i}") for i in range(n_dma_sems)], 0]
        self.dma_chans = [c for r in self.dma_rings.values() for c in r[0]]
        self.engs = [self.pe, self.act, self.dve, self.pool, self.sp]
        self._rr = 0
        self.pfx = ""
        self.cc = Chan(mk("s_cc"), "cc")

    def next_dma_chan(self, eng):
        ring = self.dma_rings[eng.name]
        ch = ring[0][ring[1] % len(ring[0])]
        ring[1] += 1
        return ch

    def sb(self, name, shape, dt=F32):
        return Buf(self.stack.enter_context(self.nc.sbuf_tensor(self.pfx + "sb_" + name, list(shape), dt)))

    def ps(self, name, shape, dt=F32):
        return Buf(self.stack.enter_context(self.nc.psum_tensor(self.pfx + "ps_" + name, list(shape), dt)))

    @staticmethod
    def _toks(*xs):
        return [x.tok for x in xs if isinstance(x, R)]

    def mm(self, out, lhsT, rhs, start=True, stop=True):
        return self.pe.op(lambda e: e.matmul(out.ap, lhsT.ap, rhs.ap, start=start, stop=stop),
                          self._toks(lhsT, rhs), [out.tok])

    def actv(self, out, in_, func, bias=0.0, scale=1.0):
        return self.act.op(
            lambda e: e.activation(out=out.ap, in_=in_.ap, func=func, bias=_ap(bias), scale=_ap(scale)),
            self._toks(in_, bias, scale), [out.tok])

    def tt(self, eng, out, in0, in1, op):
        return eng.op(lambda e: e.tensor_tensor(out=out.ap, in0=in0.ap, in1=in1.ap, op=op),
                      self._toks(in0, in1), [out.tok])

    def ts(self, eng, out, in0, s1, op0, s2=None, op1=None):
        if s2 is None:
            f = lambda e: e.tensor_scalar(out=out.ap, in0=in0.ap, scalar1=_ap(s1), scalar2=None, op0=op0)
        else:
            f = lambda e: e.tensor_scalar(out=out.ap, in0=in0.ap, scalar1=_ap(s1), scalar2=_ap(s2),
                                          op0=op0, op1=op1)
        return eng.op(f, self._toks(in0, s1, s2), [out.tok])

    def stt(self, eng, out, in0, scalar, in1, op0, op1):
        return eng.op(
            lambda e: e.scalar_tensor_tensor(out=out.ap, in0=in0.ap, scalar=_ap(scalar), in1=in1.ap,
                                             op0=op0, op1=op1),
            self._toks(in0, scalar, in1), [out.tok])

    def copy(self, eng, out, in_):
        if eng is self.act:
            return eng.op(lambda e: e.activation(out=out.ap, in_=in_.ap, func=AF.Copy),
                          self._toks(in_), [out.tok])
        return eng.op(lambda e: e.tensor_copy(out=out.ap, in_=in_.ap), self._toks(in_), [out.tok])

    def recip(self, out, in_):
        return self.dve.op(lambda e: e.reciprocal(out=out.ap, in_=in_.ap), self._toks(in_), [out.tok])

    def memset(self, eng, out, val):
        return eng.op(lambda e: e.memset(out.ap, val), [], [out.tok])

    def alt(self):
        self._rr += 1
        return self.dve if (self._rr & 1) else self.pool

    def gather(self, out, table, idx):
        eng = self.pool
        ch = self.next_dma_chan(eng)
        deps = Eng._deps([idx.tok], [out.tok])
        if ch.count > 0:
            deps.append((ch, ch.count))
        eng._wait(deps)
        inst = eng.e.indirect_dma_start(out=out.ap, out_offset=None, in_=table,
                                        in_offset=bass.IndirectOffsetOnAxis(ap=idx.ap, axis=0))
        ch.count += 16
        inst.then_inc(ch.sem, 16)
        idx.tok.r[ch] = ch.count
        out.tok.w = (ch, ch.count)
        out.tok.r = {}
        return inst

    def all_gather(self, pairs, groups):
        self.barrier()
        for src, dst in pairs:
            inst = self.pool.e.collective_compute("AllGather", ALU.bypass, replica_groups=groups,
                                                  ins=[src.ap()], outs=[dst.ap()])
            self.cc.count += 1
            inst.then_inc(self.cc.sem, 1)
        for e in self.engs:
            e._wait([(self.cc, self.cc.count)])

    def barrier(self):
        for e in self.engs:
            deps = [(o.ch, o.ch.count) for o in self.engs if o is not e and o.ch.count > 0]
            deps += [(c, c.count) for c in self.dma_chans if c.count > 0]
            if self.cc.count > 0:
                deps.append((self.cc, self.cc.count))
            e._wait(deps)

    def finish(self, toks):
        self.sp._wait([t.w for t in toks])
        deps = [(o.ch, o.ch.count) for o in self.engs if o is not self.sp and o.ch.count > 0]
        deps += [(c, c.count) for c in self.dma_chans if c.count > 0]
        self.sp._wait(deps)


def rms_rstd(fw, out, ss_psum, n, tmp):
    fw.actv(tmp, ss_psum, AF.Ln, bias=EPS, scale=1.0 / n)
    fw.actv(out, tmp, AF.Exp, scale=-0.5)


A_CH = [("q1", 64), ("q2", 64), ("k1", 64), ("k2", 64), ("av", 128), ("bq", 128), ("bf", 128),
        ("bi", 128), ("bg", 128), ("cq", 128), ("ck", 128), ("cv", 128), ("co", 128),
        ("ci", 128), ("cf", 128)]
A_OFF = {}
_o = 0
for _n, _w in A_CH:
    A_OFF[_n] = (_o, _w)
    _o += _w
NCOL_A = _o
A_IDX = {n: i for i, (n, _) in enumerate(A_CH)}
NPV = 64
PV_COND, PV_NW, PV_BSH, PV_BSC = 0, 8, 16, 24
PV_QNW, PV_KNW, PV_SUBLN, PV_GNORM, PV_CNORM = 32, 33, 34, 35, 36
PV_CWQ, PV_CWK, PV_CBQ, PV_CBK = 37, 41, 45, 46
PV_LBL, PV_LMASK, PV_IB, PV_FB, PV_LAMI, PV_1MLAMI = 47, 51, 55, 56, 57, 58
NCST = 128 + 128 + 512 + 256
MASKVAL = -200.0


def emit_A(nc, fw, S, d):
    NT = S // 512
    NB = S // 128
    w_all, modw, pv_d, rowv = d["w_all"], d["modw"], d["pvA"], d["rowv"]
    cst, bd_d, alq, alk = d["cst"], d["bd"], d["alq"], d["alk"]
    xload, ystore = d["xload"], d["ystore"]

    with contextlib.ExitStack() as st:
        top_stack = fw.stack
        fw.stack = st
        dve, pool, act, pe, sp = fw.dve, fw.pool, fw.act, fw.pe, fw.sp
        pv = fw.sb("pv", [128, NPV])
        ident_f = fw.sb("ident_f", [128, 128])
        ident_b = fw.sb("ident_b", [128, 128], BF16)
        mask2 = fw.sb("mask2", [128, 128])
        rmask = fw.sb("rmask", [128, 512])
        maskD = fw.sb("maskD", [128, 128])
        maskX = fw.sb("maskX", [128, 128])
        ones_b = fw.sb("ones_b", [128, 128], BF16)
        ones_f = fw.sb("ones_f", [128, 128])
        bdt = fw.sb("bdt", [128, 4, 512])
        w_bf = fw.sb("w_bf", [128, 8, NCOL_A], BF16)
        ka = [fw.sb(f"ka{m}", [69, S], BF16) for m in range(2)]
        va = fw.sb("va", [128, NB, 128], BF16)
        qa = [fw.sb(f"qa{m}", [69, 512], BF16) for m in range(2)]
        ka_tok = [[Tok() for _ in range(NT)] for _ in range(2)]
        va_tok = [Tok() for _ in range(NT)]
        modv = fw.sb("modv", [128, 16])
        g1v = fw.sb("g1v", [128, 8])
        sh_b = fw.sb("sh_b", [128, 8], BF16)
        sh_rep = fw.sb("sh_rep", [128, 8, 128], BF16)
        bias_c = fw.sb("bias_c", [128, 16])
        bias_r = fw.sb("bias_r", [128, 3, 128])
        cvec = fw.sb("cvec", [128, 16])
        lamt = fw.sb("lamt", [1, 8])
        Sb = fw.sb("Sb", [128, 128])
        Sb_bf = fw.sb("Sb_bf", [128, 128], BF16)
        Sc = fw.sb("Sc", [128, 129])
        Sc_bf = fw.sb("Sc_bf", [128, 128], BF16)
        n_bc = fw.sb("n_bc", [128, 128], BF16)
        ucq = fw.sb("ucq", [128, 515])
        uck = fw.sb("uck", [128, 515])
        sc_ps = [fw.ps(f"sc{i}", [128, 512]) for i in range(2)]
        O_ps = fw.ps("O_ps", [128, 512])
        L_ps = fw.ps("L_ps", [128, 512])
        pj = fw.ps("pj", [128, 512])
        misc = fw.ps("misc", [128, 512])
        num_ps = fw.ps("num_ps", [128, 512])
        dot_ps = fw.ps("dot_ps", [128, 512])
        m_aT = misc
        m_st = misc
        m_tp = misc
        m_sm = misc
        x32 = [fw.sb(f"x32_{i}", [128, 512]) for i in range(2)]
        xsq = fw.sb("xsq", [128, 8, 512], BF16)
        xg = fw.sb("xg", [128, 8, 512], BF16)
        rstd = fw.sb("rstd", [128, 512])
        rcol = fw.sb("rcol", [128, 4])
        F = {}
        for nm in ["t0", "t1", "t2", "t3", "t4", "t5", "t6", "t7", "t8", "t9", "t10"]:
            F[nm] = fw.sb(nm, [128, 512])
        Bh = {}
        for nm in ["b0", "b1", "b2", "b3", "b4", "b5", "b6"]:
            Bh[nm] = fw.sb(nm, [128, 512], BF16)
        Pt = [fw.sb(f"Pt{i}", [128, 512], BF16) for i in range(3)]
        kst_tm = fw.sb("kst_tm", [128, 4, 128], BF16)
        vb_tm = fw.sb("vb_tm", [128, 4, 128], BF16)
        vc_tm = fw.sb("vc_tm", [128, 4, 129], BF16)
        A2 = [fw.sb(f"A2_{i}", [128, 128], BF16) for i in range(2)]
        dec = fw.sb("dec", [128, 8])
        yo32 = fw.sb("yout0", [128, 512])
        yob = fw.sb("yout_bf", [128, 512], BF16)

        sp.dma(pv[:, :], pv_d[:, :])
        sp.dma(ident_f[:, :], cst[:, 0:128])
        sp.dma(mask2[:, :], cst[:, 128:256])
        sp.dma(rmask[:, :], cst[:, 256:768])
        sp.dma(maskD[:, :], cst[:, 768:896])
        sp.dma(maskX[:, :], cst[:, 896:1024])
        sp.dma(bdt[:, :, :], bd_d.rearrange("p (j q) -> p j q", j=4))
        lamrow = Buf(F["t3"].t[0:1, 0:256], F["t3"].tok)
        sp.dma(lamrow[:, :], rowv[:, :])
        fw.memset(pool, ones_b[:, :], 1.0)
        fw.memset(pool, ones_f[:, :], 1.0)
        fw.copy(dve, ident_b[:, :], ident_f[:, :])
        fw.memset(dve, Sb[:, :], 0.0)
        fw.memset(dve, Sb_bf[:, :], 0.0)
        fw.memset(dve, Sc[:, :], 0.0)
        fw.memset(dve, Sc_bf[:, :], 0.0)
        fw.memset(pool, n_bc[:, :], 0.0)
        fw.memset(pool, ucq[:, 0:3], 0.0)
        fw.memset(pool, uck[:, 0:3], 0.0)
        fw.memset(pool, vc_tm[:, :, 128:129], 1.0)
        w_v = w_all.rearrange("(c p) n -> p c n", p=128)
        for kc in range(8):
            pool.dma(w_bf[:, kc, :], w_v[:, kc, :])
        for m in range(2):
            for tt_ in range(NT):
                pool.dma(ka[m].v((slice(64, 69), slice(512 * tt_, 512 * tt_ + 512)), ka_tok[m][tt_]),
                         alk[:, 512 * tt_:512 * tt_ + 512])
        cond = F["t2"]
        fw.actv(cond[:, 0:8], pv[:, PV_COND:PV_COND + 8], AF.Silu)
        modw_v = modw.rearrange("(c p) n -> p c n", p=128)
        for g in range(16):
            stg = [F["t0"], F["t1"]]
            for hh in range(2):
                sp.dma(R(stg[hh].t[:, :].rearrange("p (c n) -> p c n", c=4), stg[hh].tok),
                       modw_v[:, 4 * hh:4 * hh + 4, g * 128:(g + 1) * 128])
            for kc in range(8):
                fw.mm(m_sm[:, 448 + g:449 + g], stg[kc // 4][:, (kc % 4) * 128:(kc % 4 + 1) * 128],
                      cond[:, kc:kc + 1], start=(kc == 0), stop=(kc == 7))
        fw.tt(dve, modv[:, :], m_sm[:, 448:464], pv[:, PV_BSH:PV_BSH + 16], ALU.add)
        fw.stt(dve, g1v[:, :], modv[:, 8:16], 1.0, pv[:, PV_NW:PV_NW + 8], ALU.add, ALU.mult)
        fw.copy(dve, sh_b[:, :], modv[:, 0:8])
        for kc in range(8):
            fw.ts(fw.alt(), sh_rep[:, kc, :], ones_b[:, :], modv[:, kc:kc + 1], ALU.mult)
        for i, (nm, wd) in enumerate(A_CH):
            off = A_OFF[nm][0]
            for kc in range(8):
                fw.mm(m_sm[0:wd, 448 + i:449 + i], w_bf[:, kc, off:off + wd], sh_b[:, kc:kc + 1],
                      start=(kc == 0), stop=(kc == 7))
        fw.copy(dve, bias_c[:, 0:16], m_sm[:, 448:464])
        for i, nm in enumerate(["av", "bi", "cv"]):
            off = A_OFF[nm][0]
            for kc in range(8):
                fw.mm(m_aT[:, 0:128], sh_rep[:, kc, :], w_bf[:, kc, off:off + 128],
                      start=(kc == 0), stop=(kc == 7))
            fw.copy(dve, bias_r[:, i, :], m_aT[:, 0:128])
        fw.tt(dve, cvec[:, 7:8], bias_c[:, A_IDX["ci"]:A_IDX["ci"] + 1], pv[:, PV_IB:PV_IB + 1], ALU.add)
        fw.tt(dve, cvec[:, 8:9], bias_c[:, A_IDX["cf"]:A_IDX["cf"] + 1], pv[:, PV_FB:PV_FB + 1], ALU.add)
        fw.ts(dve, cvec[:, 0:1], pv[:, PV_QNW:PV_QNW + 1], 0.125, ALU.mult)
        fw.copy(dve, cvec[:, 1:2], pv[:, PV_KNW:PV_KNW + 1])
        lbe = F["t0"]
        fw.actv(lbe[:, 0:4], pv[:, PV_LBL:PV_LBL + 4], AF.Exp)
        fw.tt(dve, lbe[:, 4:8], lbe[:, 0:4], pv[:, PV_LMASK:PV_LMASK + 4], ALU.mult)
        dve.op(lambda e: e.reduce_sum(out=lbe.t[:, 8:9], in_=lbe.t[:, 0:4], axis=mybir.AxisListType.X),
               [lbe.tok], [lbe.tok])
        dve.op(lambda e: e.reduce_sum(out=lbe.t[:, 9:10], in_=lbe.t[:, 4:8], axis=mybir.AxisListType.X),
               [lbe.tok], [lbe.tok])
        fw.recip(lbe[:, 10:11], lbe[:, 8:9])
        fw.tt(dve, cvec[:, 2:3], lbe[:, 9:10], lbe[:, 10:11], ALU.mult)
        fw.ts(dve, cvec[:, 3:4], cvec[:, 2:3], -1.0, ALU.mult, 1.0, ALU.add)
        fw.tt(dve, lamrow[:, 0:64], lamrow[:, 0:64], lamrow[:, 64:128], ALU.mult)
        fw.tt(dve, lamrow[:, 128:192], lamrow[:, 128:192], lamrow[:, 192:256], ALU.mult)
        dve.op(lambda e: e.reduce_sum(out=lamt.t[:, 0:1], in_=lamrow.t[:, 0:64], axis=mybir.AxisListType.X),
               [lamrow.tok], [lamt.tok])
        dve.op(lambda e: e.reduce_sum(out=lamt.t[:, 1:2], in_=lamrow.t[:, 128:192], axis=mybir.AxisListType.X),
               [lamrow.tok], [lamt.tok])
        fw.actv(lamt[:, 2:4], lamt[:, 0:2], AF.Exp)
        fw.tt(dve, lamt[:, 4:5], lamt[:, 2:3], lamt[:, 3:4], ALU.subtract)
        fw.mm(m_sm[:, 470:471], ones_f[0:1, :], lamt[0:1, 4:5])
        fw.tt(dve, cvec[:, 4:5], m_sm[:, 470:471], pv[:, PV_LAMI:PV_LAMI + 1], ALU.add)
        fw.ts(dve, cvec[:, 5:6], cvec[:, 4:5], -1.0, ALU.mult)
        fw.tt(dve, cvec[:, 6:7], pv[:, PV_SUBLN:PV_SUBLN + 1], pv[:, PV_1MLAMI:PV_1MLAMI + 1], ALU.mult)

        def rmsnorm_out(o_sb, wcol, gate, sqb):
            fw.actv(sqb[:, :], o_sb[:, :], AF.Square)
            fw.mm(pj[:, :], ones_b[:, :], sqb[:, :])
            rms_rstd(fw, F["t0"][:, :], pj[:, :], 128, F["t0"][:, :])
            if gate is None:
                fw.stt(dve, yob[:, :], o_sb[:, :], wcol, F["t0"][:, :], ALU.mult, ALU.mult)
            else:
                fw.stt(dve, yo32[:, :], o_sb[:, :], wcol, F["t0"][:, :], ALU.mult, ALU.mult)
                fw.tt(pool, yob[:, :], yo32[:, :], gate[:, :], ALU.mult)

        for t in range(NT):
            c0 = 512 * t
            for m in range(2):
                pool.dma(qa[m][64:69, :], alq[:, c0:c0 + 512])
            for kc in range(8):
                xs = x32[kc % 2]
                sp.dma(xs[:, :], xload(kc, t))
                fw.actv(xsq[:, kc, :], xs[:, :], AF.Square)
                fw.ts(fw.alt(), xg[:, kc, :], xs[:, :], g1v[:, kc:kc + 1], ALU.mult)
            for kc in range(8):
                fw.mm(pj[:, :], ones_b[:, :], xsq[:, kc, :], start=(kc == 0), stop=(kc == 7))
            rms_rstd(fw, rstd[:, :], pj[:, :], D, F["t0"][:, :])
            for kc in range(8):
                fw.tt(fw.alt(), xg[:, kc, :], xg[:, kc, :], rstd[:, :], ALU.mult)

            def proj_mm(nm):
                off, wd = A_OFF[nm]
                for kc in range(8):
                    fw.mm(pj[0:wd, :], w_bf[:, kc, off:off + wd], xg[:, kc, :],
                          start=(kc == 0), stop=(kc == 7))

            def bcol(nm, wd=128):
                i = A_IDX[nm]
                return bias_c[0:wd, i:i + 1]

            def proj_tm(nm, bi, dst3, ncols=128):
                off, wd = A_OFF[nm]
                for blk in range(4):
                    for kc in range(8):
                        fw.mm(m_aT[:, 0:128], xg[:, kc, blk * 128:(blk + 1) * 128], w_bf[:, kc, off:off + 128],
                              start=(kc == 0), stop=(kc == 7))
                    fw.tt(dve, dst3[:, blk, 0:128], m_aT[:, 0:128], bias_r[:, bi, :], ALU.add)

            for nm, m, isq in [("q1", 0, True), ("q2", 1, True), ("k1", 0, False), ("k2", 1, False)]:
                raw = F["t1"]
                proj_mm(nm)
                fw.ts(dve, raw[0:64, :], pj[0:64, :], bcol(nm, 64), ALU.add)
                sqb = Bh["b0"]
                fw.actv(sqb[0:64, :], raw[0:64, :], AF.Square)
                fw.mm(pj[0:64, :], ones_b[0:64, 0:64], sqb[0:64, :])
                rms_rstd(fw, F["t2"][0:64, :], pj[0:64, :], 64, F["t2"][0:64, :])
                if isq:
                    fw.stt(dve, qa[m][0:64, :], raw[0:64, :], cvec[0:64, 0:1], F["t2"][0:64, :],
                           ALU.mult, ALU.mult)
                else:
                    fw.stt(dve, ka[m].v((slice(0, 64), slice(c0, c0 + 512)), ka_tok[m][t]),
                           raw[0:64, :], cvec[0:64, 1:2], F["t2"][0:64, :], ALU.mult, ALU.mult)
            va_t = Buf(va.t, va_tok[t])
            off = A_OFF["av"][0]
            for blk in range(4):
                for kc in range(8):
                    fw.mm(m_aT[:, 0:128], xg[:, kc, blk * 128:(blk + 1) * 128], w_bf[:, kc, off:off + 128],
                          start=(kc == 0), stop=(kc == 7))
                fw.tt(dve, va_t[:, 4 * t + blk, :], m_aT[:, 0:128], bias_r[:, 0, :], ALU.add)

            qf, ff, logf, bb, kmid, est, gs = F["t1"], F["t2"], F["t3"], F["t4"], F["t5"], F["t6"], F["t7"]
            proj_mm("bq")
            fw.actv(qf[:, :], pj[:, :], AF.Silu, bias=bcol("bq"))
            proj_mm("bg")
            fw.actv(gs[:, :], pj[:, :], AF.Silu, bias=bcol("bg"))
            proj_mm("bf")
            fw.actv(ff[:, :], pj[:, :], AF.Sigmoid, bias=bcol("bf"))
            fw.ts(dve, ff[:, :], ff[:, :], cvec[:, 3:4], ALU.mult, cvec[:, 2:3], ALU.add)
            fw.actv(logf[:, :], ff[:, :], AF.Ln)
            fw.ts(pool, ff[:, :], ff[:, :], -1.0, ALU.mult, 1.0, ALU.add)
            dve.op(lambda e: e.tensor_tensor_scan(out=bb.t[:, :], data0=rmask.t[:, :], data1=logf.t[:, :],
                                                  initial=0.0, op0=ALU.mult, op1=ALU.add),
                   [rmask.tok, logf.tok], [bb.tok])
            qin_b, kst_b = Bh["b1"], Bh["b4"]
            fw.actv(est[:, :], bb[:, :], AF.Exp)
            fw.tt(dve, qin_b[:, :], qf[:, :], est[:, :], ALU.mult)
            qx_b, kx_b = Bh["b2"], Bh["b3"]
            qd_b, kd_b = Bh["b5"], Bh["b6"]
            for c in range(8):
                cs = slice(64 * c, 64 * c + 64)
                fw.actv(kmid[:, cs], bb[:, cs], AF.Exp, bias=bb[:, 64 * c + 31:64 * c + 32], scale=-1.0)
            fw.stt(dve, kx_b[:, :], kmid[:, :], 1.0, ff[:, :], ALU.min, ALU.mult)
            fw.recip(kmid[:, :], kmid[:, :])
            fw.stt(dve, qx_b[:, :], kmid[:, :], 1.0, qf[:, :], ALU.min, ALU.mult)
            for c in range(16):
                cs = slice(32 * c, 32 * c + 32)
                fw.actv(kmid[:, cs], bb[:, cs], AF.Exp, bias=bb[:, 32 * c + 15:32 * c + 16], scale=-1.0)
            fw.tt(dve, kd_b[:, :], ff[:, :], kmid[:, :], ALU.mult)
            fw.recip(kmid[:, :], kmid[:, :])
            fw.tt(pool, qd_b[:, :], qf[:, :], kmid[:, :], ALU.mult)
            for c in range(8):
                cs = slice(64 * c, 64 * c + 64)
                fw.actv(est[:, cs], bb[:, cs], AF.Exp, bias=bb[:, 64 * c + 63:64 * c + 64], scale=-1.0)
            fw.tt(dve, kst_b[:, :], ff[:, :], est[:, :], ALU.mult)
            fw.actv(dec[:, :], R(bb.t[:, :].rearrange("p (c l) -> p c l", l=64)[:, :, 63], bb.tok), AF.Exp)
            proj_tm("bi", 1, vb_tm)
            for blk in range(4):
                fw.mm(m_tp[:, 320:448], kst_b[:, blk * 128:(blk + 1) * 128], ident_b[:, :])
                fw.copy(act, kst_tm[:, blk, :], m_tp[:, 320:448])
            for blk in range(4):
                bs = slice(128 * blk, 128 * blk + 128)
                fw.mm(m_aT[:, 0:128], kd_b[:, bs], qd_b[:, bs])
                fw.mm(dot_ps[:, 0:128], kx_b[:, bs], qx_b[:, bs])
                a2 = A2[blk % 2]
                fw.tt(dve, F["t8"][:, 0:128], m_aT[:, 0:128], maskD[:, :], ALU.mult)
                fw.tt(dve, F["t8"][:, 128:256], dot_ps[:, 0:128], maskX[:, :], ALU.mult)
                fw.tt(dve, a2[:, :], F["t8"][:, 0:128], F["t8"][:, 128:256], ALU.add)
                for half in range(2):
                    c = 2 * blk + half
                    cs = slice(64 * c, 64 * c + 64)
                    p0 = 64 * half
                    fw.mm(num_ps[:, cs], Sb_bf[:, :], qin_b[:, cs], start=True, stop=False)
                    fw.mm(num_ps[:, cs], vb_tm[:, blk, :], a2[:, 64 * half:64 * half + 64], start=False, stop=True)
                    fw.mm(m_st[:, 128:256], kst_tm[p0:p0 + 64, blk, :], vb_tm[p0:p0 + 64, blk, :])
                    fw.stt(dve, Sb[:, :], Sb[:, :], dec[:, c:c + 1], m_st[:, 128:256], ALU.mult, ALU.add)
                    fw.copy(act, Sb_bf[:, :], Sb[:, :])
            o_b = F["t5"]
            fw.copy(act, o_b[:, :], num_ps[:, :])
            rmsnorm_out(o_b, pv[:, PV_GNORM:PV_GNORM + 1], gs, Bh["b0"])
            sp.dma(ystore(t, 1), yob[:, :])

            qc, kc_, ipre, lf, bc = F["t1"], F["t2"], F["t3"], F["t4"], F["t6"]
            og = F["t7"]
            for nm, ub, cw, cb, dst in [("cq", ucq, PV_CWQ, PV_CBQ, qc), ("ck", uck, PV_CWK, PV_CBK, kc_)]:
                proj_mm(nm)
                fw.ts(dve, ub[:, 3:515], pj[:, :], bcol(nm), ALU.add)
                fw.ts(dve, dst[:, :], ub[:, 0:512], pv[:, cw:cw + 1], ALU.mult, pv[:, cb:cb + 1], ALU.add)
                for j in range(1, 4):
                    fw.stt(dve, dst[:, :], ub[:, j:j + 512], pv[:, cw + j:cw + j + 1], dst[:, :],
                           ALU.mult, ALU.add)
                fw.actv(dst[:, :], dst[:, :], AF.Silu)
                fw.copy(pool, F["t8"][:, 0:3], ub[:, 512:515])
                fw.copy(pool, ub[:, 0:3], F["t8"][:, 0:3])
            proj_mm("co")
            fw.actv(og[:, :], pj[:, :], AF.Sigmoid, bias=bcol("co"))
            proj_mm("ci")
            fw.ts(dve, ipre[:, :], pj[:, :], cvec[:, 7:8], ALU.add)
            proj_mm("cf")
            fw.actv(lf[:, :], pj[:, :], AF.Sigmoid, bias=cvec[:, 8:9])
            fw.actv(lf[:, :], lf[:, :], AF.Ln)
            dve.op(lambda e: e.tensor_tensor_scan(out=bc.t[:, :], data0=rmask.t[:, :], data1=lf.t[:, :],
                                                  initial=0.0, op0=ALU.mult, op1=ALU.add),
                   [rmask.tok, lf.tok], [bc.tok])
            eqc, imb, estc = F["t8"], F["t9"], F["t10"]
            qt_b, kt_b, kstc_b = Bh["b1"], Bh["b2"], Bh["b4"]
            fw.actv(eqc[:, :], bc[:, :], AF.Exp)
            fw.tt(dve, qt_b[:, :], qc[:, :], eqc[:, :], ALU.mult)
            fw.tt(pool, imb[:, :], ipre[:, :], bc[:, :], ALU.subtract)
            fw.actv(eqc[:, :], imb[:, :], AF.Exp)
            fw.stt(dve, kt_b[:, :], kc_[:, :], 128.0 ** -0.5, eqc[:, :], ALU.mult, ALU.mult)
            for c in range(8):
                cs = slice(64 * c, 64 * c + 64)
                fw.actv(estc[:, cs], imb[:, cs], AF.Exp, bias=bc[:, 64 * c + 63:64 * c + 64])
            fw.stt(dve, kstc_b[:, :], kc_[:, :], 128.0 ** -0.5, estc[:, :], ALU.mult, ALU.mult)
            fw.actv(dec[:, :], R(bc.t[:, :].rearrange("p (c l) -> p c l", l=64)[:, :, 63], bc.tok), AF.Exp)
            proj_tm("cv", 2, vc_tm)
            for blk in range(4):
                fw.mm(m_tp[:, 320:448], kstc_b[:, blk * 128:(blk + 1) * 128], ident_b[:, :])
                fw.copy(act, kst_tm[:, blk, :], m_tp[:, 320:448])
            for blk in range(4):
                bs = slice(128 * blk, 128 * blk + 128)
                fw.mm(m_aT[:, 0:128], kt_b[:, bs], qt_b[:, bs])
                a2 = A2[blk % 2]
                fw.tt(dve, a2[:, :], m_aT[:, 0:128], mask2[:, :], ALU.mult)
                for half in range(2):
                    c = 2 * blk + half
                    cs = slice(64 * c, 64 * c + 64)
                    hs = slice(64 * half, 64 * half + 64)
                    p0 = 64 * half
                    fw.mm(num_ps[:, cs], Sc_bf[:, :], qt_b[:, cs], start=True, stop=False)
                    fw.mm(num_ps[:, cs], vc_tm[:, blk, 0:128], a2[:, hs], start=False, stop=True)
                    fw.mm(dot_ps[:, cs], n_bc[:, :], qt_b[:, cs], start=True, stop=False)
                    fw.mm(dot_ps[:, cs], ones_b[:, :], a2[:, hs], start=False, stop=True)
                    fw.mm(m_st[:, 128:257], kst_tm[p0:p0 + 64, blk, :], vc_tm[p0:p0 + 64, blk, :])
                    fw.stt(dve, Sc[:, :], Sc[:, :], dec[:, c:c + 1], m_st[:, 128:257], ALU.mult, ALU.add)
                    fw.copy(act, Sc_bf[:, :], Sc[:, 0:128])
                    fw.ts(pool, n_bc[:, :], ones_b[:, :], Sc[:, 128:129], ALU.mult)
            den, h_b = F["t8"], F["t9"]
            fw.ts(dve, den[:, :], dot_ps[:, :], -1.0, ALU.mult, 1.0, ALU.max)
            fw.tt(dve, den[:, :], den[:, :], dot_ps[:, :], ALU.max)
            fw.recip(den[:, :], den[:, :])
            fw.tt(dve, h_b[:, :], num_ps[:, :], den[:, :], ALU.mult)
            rmsnorm_out(h_b, pv[:, PV_CNORM:PV_CNORM + 1], og, Bh["b0"])
            sp.dma(ystore(t, 2), yob[:, :])

            om = [F["t1"], F["t2"]]
            lacc = [F["t8"], F["t9"]]
            nblk = 4 * (t + 1)
            pi = 0
            for m in range(2):
                for j in range(nblk):
                    scb = sc_ps[j % 2]
                    P = Pt[pi % 3]
                    pi += 1
                    tj = j // 4
                    kreg = lambda rows: ka[m].v((rows, slice(128 * j, 128 * j + 128)), ka_tok[m][tj])
                    if j < 4 * t:
                        fw.mm(scb[:, :], kreg(slice(0, 69)), qa[m][0:69, :])
                        fw.actv(P[:, :], scb[:, :], AF.Exp)
                    else:
                        jj = j - 4 * t
                        fw.mm(scb[:, :], kreg(slice(0, 64)), qa[m][0:64, :])
                        fw.tt(dve, F["t10"][:, :], scb[:, :], bdt[:, jj, :], ALU.add)
                        fw.actv(P[:, :], F["t10"][:, :], AF.Exp)
                    vreg = va.v((slice(None), j, slice(None)), va_tok[tj])
                    fw.mm(O_ps[:, :], vreg, P[:, :], start=(j == 0), stop=(j == nblk - 1))
                    ai = 1 if (j % 3 == 2) else 0
                    aeng = pool if ai else dve
                    if j == (2 if ai else 0):
                        fw.copy(aeng, lacc[ai][:, :], P[:, :])
                    else:
                        fw.tt(aeng, lacc[ai][:, :], lacc[ai][:, :], P[:, :], ALU.add)
                fw.tt(dve, Bh["b1"][:, :], lacc[0][:, :], lacc[1][:, :], ALU.add)
                fw.mm(L_ps[:, :], ones_b[:, :], Bh["b1"][:, :])
                fw.recip(F["t10"][:, :], L_ps[:, :])
                fw.tt(dve, om[m][:, :], O_ps[:, :], F["t10"][:, :], ALU.mult)
            o_a = F["t4"]
            fw.stt(dve, o_a[:, :], om[1][:, :], cvec[:, 5:6], om[0][:, :], ALU.mult, ALU.add)
            rmsnorm_out(o_a, cvec[:, 6:7], None, Bh["b0"])
            sp.dma(ystore(t, 0), yob[:, :])

        fw.barrier()
        fw.stack = top_stack


def _pk(v):
    return np.ascontiguousarray(np.asarray(v, np.float32).reshape(8, 128).T)


def consts_A(h, S):
    slope = 2.0 ** (-2.0 * (h + 1))
    cst = np.zeros((128, NCST), np.float32)
    cst[:, 0:128] = np.eye(128, dtype=np.float32)
    s = np.arange(128)[:, None]
    t = np.arange(128)[None, :]
    cst[:, 128:256] = ((s // 64 == t // 64) & (s <= t)).astype(np.float32)
    rm = np.ones(512, np.float32)
    rm[::64] = 0.0
    cst[:, 256:768] = rm[None, :]
    cst[:, 768:896] = ((s // 32 == t // 32) & (s <= t)).astype(np.float32)
    cst[:, 896:1024] = ((s // 64 == t // 64) & (s % 64 < 32) & (t % 64 >= 32)).astype(np.float32)
    kk = np.arange(128)[:, None]
    qq = np.arange(512)[None, :]
    bd = np.zeros((128, 4, 512), np.float32)
    for jj in range(4):
        kpos = 128 * jj + kk
        allowed = (kpos // 64) <= (qq // 64)
        bd[:, jj, :] = np.where(allowed, -slope * np.abs(qq - kpos), MASKVAL)
    pos = np.arange(S)
    one = np.ones(S)
    alq = np.stack([-slope * 512.0 * (pos // 512), -slope * 256.0 * ((pos % 512) // 256),
                    -slope * (pos % 256), one, one]).astype(np.float32)
    alk = np.stack([one, one, one, slope * 128.0 * (pos // 128), slope * (pos % 128)]).astype(np.float32)
    return cst, np.ascontiguousarray(bd.reshape(128, 2048)), alq, alk


def prep_A(inp, l, b, h):
    w = inp["w_in"][l]
    cols = []
    for nm, wd in A_CH:
        if nm in ("q1", "q2", "k1", "k2"):
            base = {"q1": 0, "q2": 256, "k1": 512, "k2": 768}[nm] + h * 64
            cols.append(w[:, base:base + 64])
        elif nm == "ci":
            cols.append(np.repeat(w[:, 5632 + h:5633 + h], 128, axis=1))
        elif nm == "cf":
            cols.append(np.repeat(w[:, 5636 + h:5637 + h], 128, axis=1))
        else:
            base = {"av": 1024, "bq": 1536, "bf": 2048, "bi": 2560, "bg": 3072, "cq": 3584,
                    "ck": 4096, "cv": 4608, "co": 5120}[nm] + h * 128
            cols.append(w[:, base:base + 128])
    w_all = np.ascontiguousarray(np.concatenate(cols, axis=1), dtype=np.float32)
    pv = np.zeros((128, NPV), np.float32)
    pv[:, PV_COND:PV_COND + 8] = _pk(inp["c"][b])
    pv[:, PV_NW:PV_NW + 8] = _pk(inp["norm1_w"][l])
    pv[:, PV_BSH:PV_BSH + 8] = _pk(inp["b_mod"][l][0:1024])
    pv[:, PV_BSC:PV_BSC + 8] = _pk(inp["b_mod"][l][1024:2048])
    pv[:, PV_QNW] = np.tile(inp["a_qnorm_w"][l], 2)
    pv[:, PV_KNW] = np.tile(inp["a_knorm_w"][l], 2)
    pv[:, PV_SUBLN] = inp["a_subln_w"][l]
    pv[:, PV_GNORM] = inp["b_gnorm_w"][l]
    pv[:, PV_CNORM] = inp["c_norm_w"][l]
    for j in range(4):
        pv[:, PV_CWQ + j] = inp["c_conv_w"][l][j, h * 128:(h + 1) * 128]
        pv[:, PV_CWK + j] = inp["c_conv_w"][l][j, 512 + h * 128:512 + (h + 1) * 128]
    pv[:, PV_CBQ] = inp["c_conv_b"][l][h * 128:(h + 1) * 128]
    pv[:, PV_CBK] = inp["c_conv_b"][l][512 + h * 128:512 + (h + 1) * 128]
    pv[:, PV_LBL:PV_LBL + 4] = inp["b_lb_logits"][:, h * 128:(h + 1) * 128].T
    for j in range(4):
        pv[:, PV_LMASK + j] = 1.0 if 1 <= j <= l else 0.0
    pv[:, PV_IB] = inp["c_igate_b"][l][h]
    pv[:, PV_FB] = inp["c_fgate_b"][l][h]
    lam_init = 0.8 - 0.6 * math.exp(-0.3 * l)
    pv[:, PV_LAMI] = lam_init
    pv[:, PV_1MLAMI] = 1.0 - lam_init
    rowv = np.concatenate([inp["a_lambda_q1"][l], inp["a_lambda_k1"][l],
                           inp["a_lambda_q2"][l], inp["a_lambda_k2"][l]]).astype(np.float32)[None, :]
    return {f"w_all{l}": w_all, f"pvA{l}": pv, f"rowv{l}": np.ascontiguousarray(rowv)}


NPVB = 80
PB_COND, PB_NW1, PB_NW2, PB_BMOD = 0, 8, 16, 24
FC_GROUPS = [(0, 4), (4, 4), (8, 4), (12, 4), (16, 4), (20, 2)]


def emit_B(nc, fw, SC, moe, d):
    NEX = NE if moe else 1
    ST = 2048 if SC >= 2048 else SC
    NST = SC // ST
    TPS = ST // 512
    wg, wbr, wout, modw, pv_d = d["wg"], d["wbr"], d["wout"], d["modw"], d["pvB"]
    w1, w3, w2, ident_d, yidx_d = d["w1"], d["w3"], d["w2"], d["ident"], d["yidx"]
    xsrc, xdst, ytab = d["xsrc"], d["xdst"], d["ytab"]
    if moe:
        wr, rb_d = d["wr"], d["rb"]
    wg_v = wg.rearrange("(c p) n -> p c n", p=128)
    w1_v = w1.rearrange("(e c p) n -> p e c n", p=128, c=8)
    w3_v = w3.rearrange("(e c p) n -> p e c n", p=128, c=8)
    w2_v = w2.rearrange("(e f p) n -> p e f n", p=128, f=22)
    modw_v = modw.rearrange("(c p) n -> p c n", p=128)

    with contextlib.ExitStack() as st:
        top_stack0 = fw.stack
        fw.stack = st
        dve, pool, act, pe, sp = fw.dve, fw.pool, fw.act, fw.pe, fw.sp
        yidx = fw.sb("yidx", [128, 8], mybir.dt.int32)
        YPS = 2 if (SC // 512) % 2 == 0 else 1
        sp.dma(yidx[:, :], yidx_d[:, :])
        pv = fw.sb("pv", [128, NPVB])
        ones_b = fw.sb("ones_b", [128, 128], BF16)
        ident_f = fw.sb("ident_f", [128, 128])
        modv = fw.sb("modv", [128, 48])
        g1v = fw.sb("g1v", [128, 8])
        g2v = fw.sb("g2v", [128, 8])
        sh1_b = fw.sb("sh1_b", [128, 8], BF16)
        sh2_b = fw.sb("sh2_b", [128, 8], BF16)
        bias_g = fw.sb("bias_g", [128, 24])
        x1buf = fw.sb("x1buf", [128, 8, ST])
        xg2 = fw.sb("xg2", [128, 8, ST], BF16)
        rstd2 = fw.sb("rstd2", [128, ST])
        x1_tok = [Tok() for _ in range(TPS)]
        xg2_tok = [Tok() for _ in range(TPS)]
        r2_tok = [Tok() for _ in range(TPS)]
        T = {nm: fw.sb(nm, [128, 512]) for nm in ["u0", "u1", "u2", "u3"]}
        if moe:
            wr_f = fw.sb("wr_f", [128, 8, 8])
            wr_s = fw.sb("wr_s", [128, 8, 8])
            sh2_f = fw.sb("sh2_f", [128, 8])
            rbias = fw.sb("rbias", [8, 2])
            combT = fw.sb("combT", [8, ST])
            cb_tok = [Tok() for _ in range(TPS)]
            rt = fw.sb("rt", [128, 64])
        pA = [fw.ps(f"pA{i}", [128, 512]) for i in range(2)]
        pB = [fw.ps(f"pB{i}", [128, 512]) for i in range(2)]
        pC = [fw.ps(f"pC{i}", [128, 512]) for i in range(2)]
        pM = fw.ps("pM", [128, 512])
        pBC = fw.ps("pBC", [128, 512])

        sp.dma(pv[:, :], pv_d[:, :])
        sp.dma(ident_f[:, :], ident_d[:, :])
        fw.memset(pool, ones_b[:, :], 1.0)
        cond = T["u2"]
        fw.actv(cond[:, 0:8], pv[:, PB_COND:PB_COND + 8], AF.Silu)
        for g in range(48):
            stg = [T["u0"], T["u1"]]
            for hh in range(2):
                sp.dma(R(stg[hh].t[:, :].rearrange("p (c n) -> p c n", c=4), stg[hh].tok),
                       modw_v[:, 4 * hh:4 * hh + 4, g * 128:(g + 1) * 128])
            for kc in range(8):
                fw.mm(pM[:, g:g + 1], stg[kc // 4][:, (kc % 4) * 128:(kc % 4 + 1) * 128],
                      cond[:, kc:kc + 1], start=(kc == 0), stop=(kc == 7))
        fw.tt(dve, modv[:, :], pM[:, 0:48], pv[:, PB_BMOD:PB_BMOD + 48], ALU.add)
        fw.stt(dve, g1v[:, :], modv[:, 8:16], 1.0, pv[:, PB_NW1:PB_NW1 + 8], ALU.add, ALU.mult)
        fw.stt(dve, g2v[:, :], modv[:, 32:40], 1.0, pv[:, PB_NW2:PB_NW2 + 8], ALU.add, ALU.mult)
        fw.copy(dve, sh1_b[:, :], modv[:, 0:8])
        fw.copy(dve, sh2_b[:, :], modv[:, 24:32])
        if moe:
            sp.dma(wr_f[:, :, :], wr.rearrange("(c p) e -> p c e", p=128))
            sp.dma(rbias[:, 0:1], rb_d[:, :])
            fw.copy(dve, sh2_f[:, :], modv[:, 24:32])
            for kc in range(8):
                fw.ts(dve, wr_s[:, kc, :], wr_f[:, kc, :], g2v[:, kc:kc + 1], ALU.mult)
                fw.mm(pM[0:8, 60:61], wr_f[:, kc, :], sh2_f[:, kc:kc + 1], start=(kc == 0), stop=(kc == 7))
            fw.tt(dve, rbias[:, 1:2], pM[0:8, 60:61], rbias[:, 0:1], ALU.add)

        wgc = None
        for s_i in range(NST):
            with contextlib.ExitStack() as st1:
                top_stack = fw.stack
                fw.stack = st1
                wbr_bf = fw.sb(f"wbr_bf{s_i}", [128, 12, D], BF16)
                wout_bf = fw.sb(f"wout_bf{s_i}", [128, 8, D], BF16)
                wgc = [fw.sb(f"wgc{s_i}_{i}", [128, 8, 384], BF16) for i in range(2)]
                xsq = fw.sb(f"xsq{s_i}", [128, 8, 512], BF16)
                xg1 = fw.sb(f"xg1{s_i}", [128, 8, 512], BF16)
                ybf = fw.sb(f"ybf{s_i}", [128, 12, 512], BF16)
                mrg = xsq
                rstd1 = fw.sb(f"rstd1{s_i}", [128, 512])
                fw.stack = top_stack
                for j in range(12):
                    pool.dma(wbr_bf[:, j, :], wbr[j * 128:(j + 1) * 128, :])
                for kc in range(8):
                    pool.dma(wout_bf[:, kc, :], wout[kc * 128:(kc + 1) * 128, :])
                wi = 0

                def load_wgc(mc):
                    nonlocal wi
                    buf = wgc[wi % 2]
                    wi += 1
                    for br in range(3):
                        pool.dma(buf[:, :, br * 128:(br + 1) * 128],
                                 wg_v[:, :, br * 1024 + mc * 128:br * 1024 + (mc + 1) * 128])
                    return buf

                if s_i == 0:
                    for mc in range(8):
                        buf = load_wgc(mc)
                        for br in range(3):
                            for kc in range(8):
                                fw.mm(pM[:, 64 + br * 8 + mc:65 + br * 8 + mc], buf[:, kc, br * 128:(br + 1) * 128],
                                      sh1_b[:, kc:kc + 1], start=(kc == 0), stop=(kc == 7))
                    fw.copy(dve, bias_g[:, :], pM[:, 64:88])
                for tl in range(TPS):
                    c0 = s_i * ST + tl * 512
                    cs = slice(tl * 512, tl * 512 + 512)
                    x1r = lambda kc: x1buf.v((slice(None), kc, cs), x1_tok[tl])
                    for kc in range(8):
                        sp.dma(x1r(kc), xsrc(kc, c0))
                    for kc in range(8):
                        fw.actv(xsq[:, kc, :], x1r(kc), AF.Square)
                    for kc in range(8):
                        fw.mm(pM[:, :], ones_b[:, :], xsq[:, kc, :], start=(kc == 0), stop=(kc == 7))
                    rms_rstd(fw, rstd1[:, :], pM[:, :], D, T["u0"][:, :])
                    for kc in range(8):
                        fw.stt(dve, xg1[:, kc, :], x1r(kc), g1v[:, kc:kc + 1], rstd1[:, :], ALU.mult, ALU.mult)
                    for j in range(12):
                        ic = ((c0 // 512) % YPS) * 4 + j % 4
                        fw.gather(ybf[:, j, :], ytab(c0 // 512, j // 4), yidx[:, ic:ic + 1])
                    for mc in range(8):
                        buf = load_wgc(mc)
                        mg = T["u1"]
                        for br in range(3):
                            pa, pb = pA[br % 2], pB[br % 2]
                            for kc in range(8):
                                fw.mm(pa[:, :], buf[:, kc, br * 128:(br + 1) * 128], xg1[:, kc, :],
                                      start=(kc == 0), stop=(kc == 7))
                            for k4 in range(4):
                                fw.mm(pb[:, :], wbr_bf[:, br * 4 + k4, mc * 128:(mc + 1) * 128], ybf[:, br * 4 + k4, :],
                                      start=(k4 == 0), stop=(k4 == 3))
                            gt = T["u2"]
                            fw.actv(gt[:, :], pa[:, :], AF.Sigmoid, bias=bias_g[:, br * 8 + mc:br * 8 + mc + 1])
                            if br == 0:
                                fw.tt(dve, mg[:, :], gt[:, :], pb[:, :], ALU.mult)
                            else:
                                tmp = T["u3"]
                                fw.tt(dve, tmp[:, :], gt[:, :], pb[:, :], ALU.mult)
                                if br == 1:
                                    fw.tt(pool, mg[:, :], mg[:, :], tmp[:, :], ALU.add)
                                else:
                                    fw.tt(pool, mrg[:, mc, :], mg[:, :], tmp[:, :], ALU.add)
                    for mc in range(8):
                        po = pC[mc % 2]
                        for kc in range(8):
                            fw.mm(po[:, :], wout_bf[:, kc, mc * 128:(mc + 1) * 128], mrg[:, kc, :],
                                  start=(kc == 0), stop=(kc == 7))
                        fw.stt(dve, x1r(mc), po[:, :], modv[:, 16 + mc:17 + mc], x1r(mc), ALU.mult, ALU.add)
                    for kc in range(8):
                        fw.actv(xsq[:, kc, :], x1r(kc), AF.Square)
                    for kc in range(8):
                        fw.mm(pM[:, :], ones_b[:, :], xsq[:, kc, :], start=(kc == 0), stop=(kc == 7))
                    r2 = rstd2.v((slice(None), cs), r2_tok[tl])
                    rms_rstd(fw, r2, pM[:, :], D, T["u0"][:, :])
                    for kc in range(8):
                        fw.stt(dve, xg2.v((slice(None), kc, cs), xg2_tok[tl]), x1r(kc), g2v[:, kc:kc + 1], r2,
                               ALU.mult, ALU.mult)
                    if moe:
                        for kc in range(8):
                            fw.mm(pM[0:8, :], wr_s[:, kc, :], x1r(kc), start=(kc == 0), stop=(kc == 7))
                        lg = T["u2"]
                        fw.tt(dve, lg[0:8, :], pM[0:8, :], R(rstd2.t[0:8, cs], r2_tok[tl]), ALU.mult)
                        fw.ts(dve, lg[0:8, :], lg[0:8, :], rbias[:, 1:2], ALU.add)
                        for blk in range(4):
                            bs = slice(blk * 128, blk * 128 + 128)
                            fw.mm(pM[:, 0:8], lg[0:8, bs], ident_f[0:8, 0:8])
                            L8 = rt[:, 0:8]
                            fw.copy(dve, L8, pM[:, 0:8])
                            dve.op(lambda e: e.reduce_max(out=rt.t[:, 8:9], in_=rt.t[:, 0:8], axis=mybir.AxisListType.X),
                                   [rt.tok], [rt.tok])
                            fw.ts(dve, rt[:, 16:24], rt[:, 0:8], rt[:, 8:9], ALU.is_equal, -1e30, ALU.mult)
                            fw.tt(dve, rt[:, 16:24], rt[:, 16:24], rt[:, 0:8], ALU.add)
                            dve.op(lambda e: e.reduce_max(out=rt.t[:, 9:10], in_=rt.t[:, 16:24], axis=mybir.AxisListType.X),
                                   [rt.tok], [rt.tok])
                            fw.ts(dve, rt[:, 24:32], rt[:, 0:8], rt[:, 9:10], ALU.is_ge)
                            fw.ts(dve, rt[:, 10:11], rt[:, 8:9], -1.0, ALU.mult)
                            fw.actv(rt[:, 32:40], rt[:, 0:8], AF.Exp, bias=rt[:, 10:11])
                            fw.tt(dve, rt[:, 32:40], rt[:, 32:40], rt[:, 24:32], ALU.mult)
                            dve.op(lambda e: e.reduce_sum(out=rt.t[:, 11:12], in_=rt.t[:, 32:40], axis=mybir.AxisListType.X),
                                   [rt.tok], [rt.tok])
                            fw.recip(rt[:, 12:13], rt[:, 11:12])
                            fw.ts(dve, rt[:, 40:48], rt[:, 32:40], rt[:, 12:13], ALU.mult)
                            fw.mm(pM[0:8, 128:256], rt[:, 40:48], ident_f[:, :])
                            fw.copy(dve, combT.v((slice(None), slice(tl * 512 + blk * 128, tl * 512 + blk * 128 + 128)), cb_tok[tl]),
                                    pM[0:8, 128:256])
                fw.barrier()
            with contextlib.ExitStack() as st2:
                top_stack = fw.stack
                fw.stack = st2
                w1g = [fw.sb(f"w1g{s_i}_{i}", [128, 8, 512], BF16) for i in range(2)]
                w3g = [fw.sb(f"w3g{s_i}_{i}", [128, 8, 512], BF16) for i in range(2)]
                w2g = [fw.sb(f"w2g{s_i}_{i}", [128, 4, D], BF16) for i in range(2)]
                hh = [fw.sb(f"hh{s_i}_{i}", [128, 4, 512], BF16) for i in range(2)]
                bfc = [fw.sb(f"bfc{s_i}_{i}", [128, 8]) for i in range(2)]
                if moe:
                    cbs = fw.sb(f"cbs{s_i}", [128, TPS, 512])
                fw.stack = top_stack
                T1 = [T["u0"], T["u1"]]
                T3 = [T["u2"], T["u3"]]
                gi = 0
                hi = 0
                for e in range(NEX):
                    if moe:
                        for tl in range(TPS):
                            cs = slice(tl * 512, tl * 512 + 512)
                            fw.mm(pBC[:, :], R(ident_f.t[0:8, e:e + 1].to_broadcast([8, 128]), ident_f.tok),
                                  combT.v((slice(None), cs), cb_tok[tl]))
                            fw.copy(act, cbs[:, tl, :], pBC[:, :])
                    for (f0, fn) in FC_GROUPS:
                        b_ = gi % 2
                        gi += 1
                        pool.dma(w1g[b_][:, :, 0:fn * 128], w1_v[:, e, :, f0 * 128:(f0 + fn) * 128])
                        pool.dma(w3g[b_][:, :, 0:fn * 128], w3_v[:, e, :, f0 * 128:(f0 + fn) * 128])
                        pool.dma(w2g[b_][:, 0:fn, :], w2_v[:, e, f0:f0 + fn, :])
                        for f in range(fn):
                            for kc in range(8):
                                fw.mm(pM[:, f:f + 1], w1g[b_][:, kc, f * 128:(f + 1) * 128], sh2_b[:, kc:kc + 1],
                                      start=(kc == 0), stop=(kc == 7))
                            for kc in range(8):
                                fw.mm(pM[:, 4 + f:5 + f], w3g[b_][:, kc, f * 128:(f + 1) * 128], sh2_b[:, kc:kc + 1],
                                      start=(kc == 0), stop=(kc == 7))
                        fw.copy(dve, bfc[b_][:, :], pM[:, 0:8])
                        for tl in range(TPS):
                            cs = slice(tl * 512, tl * 512 + 512)
                            r2 = rstd2.v((slice(None), cs), r2_tok[tl])
                            hb = hh[hi % 2]
                            hi += 1
                            for f in range(fn):
                                pa, pb = pA[f % 2], pB[f % 2]
                                for kc in range(8):
                                    fw.mm(pa[:, :], w1g[b_][:, kc, f * 128:(f + 1) * 128],
                                          xg2.v((slice(None), kc, cs), xg2_tok[tl]), start=(kc == 0), stop=(kc == 7))
                                for kc in range(8):
                                    fw.mm(pb[:, :], w3g[b_][:, kc, f * 128:(f + 1) * 128],
                                          xg2.v((slice(None), kc, cs), xg2_tok[tl]), start=(kc == 0), stop=(kc == 7))
                                t1 = T1[f % 2]
                                fw.actv(t1[:, :], pa[:, :], AF.Silu, bias=bfc[b_][:, f:f + 1])
                                if moe:
                                    t3 = T3[f % 2]
                                    fw.stt(dve, t3[:, :], pb[:, :], bfc[b_][:, 4 + f:5 + f], t1[:, :], ALU.add, ALU.mult)
                                    fw.tt(pool, hb[:, f, :], t3[:, :], cbs[:, tl, :], ALU.mult)
                                else:
                                    fw.stt(dve, hb[:, f, :], pb[:, :], bfc[b_][:, 4 + f:5 + f], t1[:, :],
                                           ALU.add, ALU.mult)
                            for mc in range(8):
                                po = pC[mc % 2]
                                for f in range(fn):
                                    fw.mm(po[:, :], w2g[b_][:, f, mc * 128:(mc + 1) * 128], hb[:, f, :],
                                          start=(f == 0), stop=(f == fn - 1))
                                xr = x1buf.v((slice(None), mc, cs), x1_tok[tl])
                                fw.stt(dve, xr, po[:, :], modv[:, 40 + mc:41 + mc], xr, ALU.mult, ALU.add)
                for tl in range(TPS):
                    c0 = s_i * ST + tl * 512
                    cs = slice(tl * 512, tl * 512 + 512)
                    for kc in range(8):
                        sp.dma(xdst(kc, c0), x1buf.v((slice(None), kc, cs), x1_tok[tl]))
                fw.barrier()
        fw.stack = top_stack0


def prep_B(inp, l, b):
    i2 = l // 2
    moe = (l % 2 == 1)
    pv = np.zeros((128, NPVB), np.float32)
    pv[:, PB_COND:PB_COND + 8] = _pk(inp["c"][b])
    pv[:, PB_NW1:PB_NW1 + 8] = _pk(inp["norm1_w"][l])
    pv[:, PB_NW2:PB_NW2 + 8] = _pk(inp["norm2_w"][l])
    for k in range(6):
        pv[:, PB_BMOD + 8 * k:PB_BMOD + 8 * k + 8] = _pk(inp["b_mod"][l][k * 1024:(k + 1) * 1024])
    m = {f"wg{l}": np.ascontiguousarray(inp["w_in"][l][:, 5640:8712]),
         f"wbr{l}": np.ascontiguousarray(inp["w_branch"][l].reshape(1536, D)),
         f"wout{l}": np.ascontiguousarray(inp["w_out"][l]),
         f"modw{l}": np.ascontiguousarray(inp["w_mod"][l]),
         f"pvB{l}": pv}
    if moe:
        m[f"w1_{l}"] = np.ascontiguousarray(inp["moe_w1"][i2].reshape(NE * D, D_FF))
        m[f"w3_{l}"] = np.ascontiguousarray(inp["moe_w3"][i2].reshape(NE * D, D_FF))
        m[f"w2_{l}"] = np.ascontiguousarray(inp["moe_w2"][i2].reshape(NE * D_FF, D))
        m[f"wr{l}"] = np.ascontiguousarray(inp["moe_router_w"][i2])
        m[f"rb{l}"] = np.ascontiguousarray(inp["moe_router_b"][i2].reshape(8, 1))
    else:
        m[f"w1_{l}"] = np.ascontiguousarray(inp["ffn_w1"][i2])
        m[f"w3_{l}"] = np.ascontiguousarray(inp["ffn_w3"][i2])
        m[f"w2_{l}"] = np.ascontiguousarray(inp["ffn_w2"][i2])
    return m


GROUPS = [[0, 1, 2, 3], [4, 5, 6, 7]]


def build_fused(S, NL=DEPTH):
    SC = S // 4
    TPC = SC // 512
    HW = min(SC, 2048)
    NH = SC // HW
    nc = bass.Bass("TRN2", target_bir_lowering=False)

    def din(name, shape, dt=F32):
        return nc.dram_tensor(name, list(shape), dt, kind="ExternalInput").ap()

    xs = din("xs", [D, SC])
    cst = din("cst", [128, NCST])
    bd_d = din("bd", [128, 4 * 512])
    alq = din("alq", [5, S])
    alk = din("alk", [5, S])
    ident_d = din("ident", [128, 128])
    yidx_d = din("yidx", [128, 8], mybir.dt.int32)
    L = []
    for l in range(NL):
        moe = (l % 2 == 1)
        NEX = NE if moe else 1
        dl = {"w_all": din(f"w_all{l}", [D, NCOL_A]), "modw": din(f"modw{l}", [D, 6 * D]),
              "pvA": din(f"pvA{l}", [128, NPV]), "rowv": din(f"rowv{l}", [1, 256]),
              "wg": din(f"wg{l}", [D, 3072]), "wbr": din(f"wbr{l}", [1536, D]), "wout": din(f"wout{l}", [D, D]),
              "pvB": din(f"pvB{l}", [128, NPVB]),
              "w1": din(f"w1_{l}", [NEX * D, D_FF]), "w3": din(f"w3_{l}", [NEX * D, D_FF]),
              "w2": din(f"w2_{l}", [NEX * D_FF, D])}
        if moe:
            dl["wr"] = din(f"wr{l}", [D, 8])
            dl["rb"] = din(f"rb{l}", [8, 1])
        L.append(dl)
    xo = nc.dram_tensor("xo", [D, SC], F32, kind="ExternalOutput").ap()
    xs_v = xs.rearrange("(c p) s -> p c s", p=128)
    xo_v = xo.rearrange("(c p) s -> p c s", p=128)
    xck = [[nc.dram_tensor(f"xck_{kc}_{hf}", [128, HW], F32) for hf in range(NH)] for kc in range(8)]
    xgk = [[nc.dram_tensor(f"xgk_{kc}_{hf}", [512, HW], F32) for hf in range(NH)] for kc in range(8)]
    PS = 2 if TPC % 2 == 0 else 1
    NP = TPC // PS
    ypk = [[nc.dram_tensor(f"ypk_{tp}_{br}", [PS * 512, 512], BF16) for br in range(3)] for tp in range(NP)]
    yg = [[nc.dram_tensor(f"yg_{tp}_{br}", [4 * PS * 512, 512], BF16) for br in range(3)] for tp in range(NP)]

    def xload(kc, t):
        c0 = 512 * t
        r, o = c0 // SC, c0 % SC
        hf, col = o // HW, o % HW
        return xgk[kc][hf].ap()[r * 128:(r + 1) * 128, col:col + 512]

    def ystore(t, br):
        r, tl = t // TPC, t % TPC
        o = (tl % PS) * 512 + r * 128
        return ypk[tl // PS][br].ap()[o:o + 128, :]

    def ytab(tl, br):
        return yg[tl // PS][br].ap()

    def xsrc(kc, c0):
        return xck[kc][c0 // HW].ap()[:, c0 % HW:c0 % HW + 512]

    def xout(kc, c0):
        return xo_v[:, kc, c0:c0 + 512]

    x_pairs = [(xck[kc][hf], xgk[kc][hf]) for kc in range(8) for hf in range(NH)]
    y_pairs = [(ypk[tp][br], yg[tp][br]) for tp in range(NP) for br in range(3)]

    with contextlib.ExitStack() as st:
        fw = FW(nc, st)
        for kc in range(8):
            for hf in range(NH):
                fw.sp.dma(xck[kc][hf].ap(), xs_v[:, kc, hf * HW:(hf + 1) * HW])
        fw.all_gather(x_pairs, GROUPS)
        for l in range(NL):
            moe = (l % 2 == 1)
            dl = dict(L[l])
            dl.update(cst=cst, bd=bd_d, alq=alq, alk=alk, ident=ident_d, yidx=yidx_d,
                      xload=xload, ystore=ystore, ytab=ytab, xsrc=xsrc,
                      xdst=(xout if l == NL - 1 else xsrc))
            fw.pfx = f"L{l}A_"
            emit_A(nc, fw, S, dl)
            fw.all_gather(y_pairs, GROUPS)
            fw.pfx = f"L{l}B_"
            emit_B(nc, fw, SC, moe, dl)
            if l < NL - 1:
                fw.all_gather(x_pairs, GROUPS)
        fw.finish([])
    return nc


def prep_core(inp, c, xT, S, NL=DEPTH):
    b, r = c // 4, c % 4
    SC = S // 4
    cst, bd, alq, alk = consts_A(r, S)
    p = np.arange(128)
    m = {"xs": np.ascontiguousarray(xT[b][:, r * SC:(r + 1) * SC], dtype=np.float32),
         "cst": cst, "bd": bd, "alq": alq, "alk": alk, "ident": np.eye(128, dtype=np.float32),
         "yidx": None}
    PS = 2 if (SC // 512) % 2 == 0 else 1
    yi = np.zeros((128, 8), np.int32)
    for tl2 in range(PS):
        for h in range(4):
            yi[:, tl2 * 4 + h] = h * (PS * 512) + tl2 * 512 + r * 128 + p
    m["yidx"] = yi
    for l in range(NL):
        m.update(prep_A(inp, l, b, r))
        m.update(prep_B(inp, l, b))
    return m


_NC_CACHE = {}


def kernel(**inputs):
    inp = {k: np.asarray(v) for k, v in inputs.items()}
    x = inp["x"]
    S = x.shape[1]
    SC = S // 4
    xT = [np.ascontiguousarray(x[b].T.astype(np.float32)) for b in range(BATCH)]
    cores = list(range(8))
    if S not in _NC_CACHE:
        _NC_CACHE[S] = build_fused(S)
    in_maps = [prep_core(inp, c, xT, S) for c in cores]
    res = run_bass_kernel_spmd(_NC_CACHE[S], in_maps, core_ids=cores).results
    out = np.empty((BATCH, S, D), np.float32)
    for c in cores:
        b, r = c // 4, c % 4
        out[b, r * SC:(r + 1) * SC, :] = res[c]["xo"].T
    return out
```

```python
import contextlib
import math
import numpy as np
import concourse.bass as bass
import concourse.mybir as mybir
from concourse.bass_utils import run_bass_kernel_spmd

F32 = mybir.dt.float32
BF16 = mybir.dt.bfloat16
ALU = mybir.AluOpType
AF = mybir.ActivationFunctionType

D = 1024
DEPTH = 4
SEQ = 16384
BATCH = 2
D_FF = 2816
NE = 8
EPS = 1e-6
D_IN = 8712


class Tok:
    __slots__ = ("w", "r")

    def __init__(self):
        self.w = None
        self.r = {}


class Chan:
    def __init__(self, sem, name):
        self.sem = sem
        self.count = 0
        self.name = name


class R:
    __slots__ = ("ap", "tok")

    def __init__(self, ap, tok):
        self.ap = ap
        self.tok = tok


class Buf:
    def __init__(self, t, tok=None):
        self.t = t
        self.tok = tok or Tok()

    def __getitem__(self, idx):
        return R(self.t[idx], self.tok)

    def v(self, idx, tok):
        return R(self.t[idx], tok)


def _ap(x):
    return x.ap if isinstance(x, R) else x


class Eng:
    def __init__(self, fw, e, name, sem):
        self.fw = fw
        self.e = e
        self.name = name
        self.ch = Chan(sem, name)
        self.seen = {}

    def _wait(self, deps):
        best = {}
        for d in deps:
            if d is None:
                continue
            ch, c = d
            if best.get(ch, 0) < c:
                best[ch] = c
        for ch, c in best.items():
            if ch is self.ch and self.name == "pe":
                continue
            if self.seen.get(ch, 0) >= c:
                continue
            self.e.wait_ge(ch.sem, c)
            self.seen[ch] = c

    @staticmethod
    def _deps(reads, writes):
        deps = []
        for t in reads:
            deps.append(t.w)
        for t in writes:
            deps.extend(t.r.items())
            deps.append(t.w)
        return deps

    def op(self, fn, reads=(), writes=()):
        self._wait(self._deps(reads, writes))
        inst = fn(self.e)
        self.ch.count += 1
        inst.then_inc(self.ch.sem, 1)
        me = (self.ch, self.ch.count)
        for t in reads:
            t.r[self.ch] = self.ch.count
        for t in writes:
            t.w = me
            t.r = {}
        return inst

    def dma(self, out, in_, **kw):
        reads = [in_.tok] if isinstance(in_, R) else []
        writes = [out.tok] if isinstance(out, R) else []
        ch = self.fw.next_dma_chan(self)
        deps = self._deps(reads, writes)
        if ch.count > 0:
            deps.append((ch, ch.count))
        self._wait(deps)
        inst = self.e.dma_start(out=_ap(out), in_=_ap(in_), **kw)
        ch.count += 16
        inst.then_inc(ch.sem, 16)
        for t in reads:
            t.r[ch] = ch.count
        for t in writes:
            t.w = (ch, ch.count)
            t.r = {}
        return inst


class FW:
    def __init__(self, nc, stack, n_dma_sems=16):
        self.nc = nc
        self.stack = stack
        self.block = stack.enter_context(nc.Block())
        mk = lambda n: stack.enter_context(nc.semaphore(n))
        self.pe = Eng(self, nc.tensor, "pe", mk("s_pe"))
        self.act = Eng(self, nc.scalar, "act", mk("s_act"))
        self.dve = Eng(self, nc.vector, "dve", mk("s_dve"))
        self.pool = Eng(self, nc.gpsimd, "pool", mk("s_pool"))
        self.sp = Eng(self, nc.sync, "sp", mk("s_sp"))
        self.dma_rings = {}
        for en in ("sp", "pool", "act"):
            self.dma_rings[en] = [[Chan(mk(f"s_dma_{en}{i}"), f"dma_{en}{i}") for i in range(n_dma_sems)], 0]
        self.dma_chans = [c for r in self.dma_rings.values() for c in r[0]]
        self.engs = [self.pe, self.act, self.dve, self.pool, self.sp]
        self._rr = 0
        self.pfx = ""
        self.cc = Chan(mk("s_cc"), "cc")

    def next_dma_chan(self, eng):
        ring = self.dma_rings[eng.name]
        ch = ring[0][ring[1] % len(ring[0])]
        ring[1] += 1
        return ch

    def sb(self, name, shape, dt=F32):
        return Buf(self.stack.enter_context(self.nc.sbuf_tensor(self.pfx + "sb_" + name, list(shape), dt)))

    def ps(self, name, shape, dt=F32):
        return Buf(self.stack.enter_context(self.nc.psum_tensor(self.pfx + "ps_" + name, list(shape), dt)))

    @staticmethod
    def _toks(*xs):
        return [x.tok for x in xs if isinstance(x, R)]

    def mm(self, out, lhsT, rhs, start=True, stop=True):
        return self.pe.op(lambda e: e.matmul(out.ap, lhsT.ap, rhs.ap, start=start, stop=stop),
                          self._toks(lhsT, rhs), [out.tok])

    def actv(self, out, in_, func, bias=0.0, scale=1.0):
        return self.act.op(
            lambda e: e.activation(out=out.ap, in_=in_.ap, func=func, bias=_ap(bias), scale=_ap(scale)),
            self._toks(in_, bias, scale), [out.tok])

    def tt(self, eng, out, in0, in1, op):
        return eng.op(lambda e: e.tensor_tensor(out=out.ap, in0=in0.ap, in1=in1.ap, op=op),
                      self._toks(in0, in1), [out.tok])

    def ts(self, eng, out, in0, s1, op0, s2=None, op1=None):
        if s2 is None:
            f = lambda e: e.tensor_scalar(out=out.ap, in0=in0.ap, scalar1=_ap(s1), scalar2=None, op0=op0)
        else:
            f = lambda e: e.tensor_scalar(out=out.ap, in0=in0.ap, scalar1=_ap(s1), scalar2=_ap(s2),
                                          op0=op0, op1=op1)
        return eng.op(f, self._toks(in0, s1, s2), [out.tok])

    def stt(self, eng, out, in0, scalar, in1, op0, op1):
        return eng.op(
            lambda e: e.scalar_tensor_tensor(out=out.ap, in0=in0.ap, scalar=_ap(scalar), in1=in1.ap,
                                             op0=op0, op1=op1),
            self._toks(in0, scalar, in1), [out.tok])

    def copy(self, eng, out, in_):
        if eng is self.act:
            return eng.op(lambda e: e.activation(out=out.ap, in_=in_.ap, func=AF.Copy),
                          self._toks(in_), [out.tok])
        return eng.op(lambda e: e.tensor_copy(out=out.ap, in_=in_.ap), self._toks(in_), [out.tok])

    def recip(self, out, in_):
        return self.dve.op(lambda e: e.reciprocal(out=out.ap, in_=in_.ap), self._toks(in_), [out.tok])

    def memset(self, eng, out, val):
        return eng.op(lambda e: e.memset(out.ap, val), [], [out.tok])

    def alt(self):
        self._rr += 1
        return self.dve if (self._rr & 1) else self.pool

    def gather(self, out, table, idx):
        eng = self.pool
        ch = self.next_dma_chan(eng)
        deps = Eng._deps([idx.tok], [out.tok])
        if ch.count > 0:
            deps.append((ch, ch.count))
        eng._wait(deps)
        inst = eng.e.indirect_dma_start(out=out.ap, out_offset=None, in_=table,
                                        in_offset=bass.IndirectOffsetOnAxis(ap=idx.ap, axis=0))
        ch.count += 16
        inst.then_inc(ch.sem, 16)
        idx.tok.r[ch] = ch.count
        out.tok.w = (ch, ch.count)
        out.tok.r = {}
        return inst

    def all_gather(self, pairs, groups, wait=True):
        self.barrier()
        for src, dst in pairs:
            inst = self.pool.e.collective_compute("AllGather", ALU.bypass, replica_groups=groups,
                                                  ins=[src.ap()], outs=[dst.ap()])
            self.cc.count += 1
            inst.then_inc(self.cc.sem, 1)
        if wait:
            self.cc_wait()

    def cc_wait(self):
        for e in self.engs:
            e._wait([(self.cc, self.cc.count)])

    def barrier(self):
        for e in self.engs:
            deps = [(o.ch, o.ch.count) for o in self.engs if o is not e and o.ch.count > 0]
            deps += [(c, c.count) for c in self.dma_chans if c.count > 0]
            if self.cc.count > 0:
                deps.append((self.cc, self.cc.count))
            e._wait(deps)

    def finish(self, toks):
        self.sp._wait([t.w for t in toks])
        deps = [(o.ch, o.ch.count) for o in self.engs if o is not self.sp and o.ch.count > 0]
        deps += [(c, c.count) for c in self.dma_chans if c.count > 0]
        self.sp._wait(deps)


def rms_rstd(fw, out, ss_psum, n, tmp):
    fw.actv(tmp, ss_psum, AF.Ln, bias=EPS, scale=1.0 / n)
    fw.actv(out, tmp, AF.Exp, scale=-0.5)


A_CH = [("q1", 64), ("q2", 64), ("k1", 64), ("k2", 64), ("av", 128), ("bq", 128), ("bf", 128),
        ("bi", 128), ("bg", 128), ("cq", 128), ("ck", 128), ("cv", 128), ("co", 128),
        ("ci", 128), ("cf", 128)]
A_OFF = {}
_o = 0
for _n, _w in A_CH:
    A_OFF[_n] = (_o, _w)
    _o += _w
NCOL_A = _o
A_IDX = {n: i for i, (n, _) in enumerate(A_CH)}
NPV = 64
PV_COND, PV_NW, PV_BSH, PV_BSC = 0, 8, 16, 24
PV_QNW, PV_KNW, PV_SUBLN, PV_GNORM, PV_CNORM = 32, 33, 34, 35, 36
PV_CWQ, PV_CWK, PV_CBQ, PV_CBK = 37, 41, 45, 46
PV_LBL, PV_LMASK, PV_IB, PV_FB, PV_LAMI, PV_1MLAMI = 47, 51, 55, 56, 57, 58
NCST = 128 + 128 + 512 + 256
MASKVAL = -200.0


def emit_A(nc, fw, S, d):
    NT = S // 512
    NB = S // 128
    w_all, modw, pv_d, rowv = d["w_all"], d["modw"], d["pvA"], d["rowv"]
    cst, bd_d, alq, alk = d["cst"], d["bd"], d["alq"], d["alk"]
    xload, ystore = d["xload"], d["ystore"]

    with contextlib.ExitStack() as st:
        top_stack = fw.stack
        fw.stack = st
        dve, pool, act, pe, sp = fw.dve, fw.pool, fw.act, fw.pe, fw.sp
        pv = fw.sb("pv", [128, NPV])
        ident_f = fw.sb("ident_f", [128, 128])
        ident_b = fw.sb("ident_b", [128, 128], BF16)
        mask2 = fw.sb("mask2", [128, 128])
        rmask = fw.sb("rmask", [128, 512])
        maskD = fw.sb("maskD", [128, 128])
        maskX = fw.sb("maskX", [128, 128])
        ones_b = fw.sb("ones_b", [128, 128], BF16)
        ones_f = fw.sb("ones_f", [128, 128])
        bdt = fw.sb("bdt", [128, 4, 512])
        w_bf = fw.sb("w_bf", [128, 8, NCOL_A], BF16)
        ka = [fw.sb(f"ka{m}", [69, S], BF16) for m in range(2)]
        va = fw.sb("va", [128, NB, 128], BF16)
        qa = [fw.sb(f"qa{m}", [69, 512], BF16) for m in range(2)]
        ka_tok = [[Tok() for _ in range(NT)] for _ in range(2)]
        va_tok = [Tok() for _ in range(NT)]
        modv = fw.sb("modv", [128, 16])
        g1v = fw.sb("g1v", [128, 8])
        sh_b = fw.sb("sh_b", [128, 8], BF16)
        sh_rep = fw.sb("sh_rep", [128, 8, 128], BF16)
        bias_c = fw.sb("bias_c", [128, 16])
        bias_r = fw.sb("bias_r", [128, 3, 128])
        cvec = fw.sb("cvec", [128, 16])
        lamt = fw.sb("lamt", [1, 8])
        Sb = fw.sb("Sb", [128, 128])
        Sb_bf = fw.sb("Sb_bf", [128, 128], BF16)
        Sc = fw.sb("Sc", [128, 129])
        Sc_bf = fw.sb("Sc_bf", [128, 128], BF16)
        n_bc = fw.sb("n_bc", [128, 128], BF16)
        ucq = fw.sb("ucq", [128, 515])
        uck = fw.sb("uck", [128, 515])
        sc_ps = [fw.ps(f"sc{i}", [128, 512]) for i in range(2)]
        O_ps = fw.ps("O_ps", [128, 512])
        L_ps = fw.ps("L_ps", [128, 512])
        pj = fw.ps("pj", [128, 512])
        misc = fw.ps("misc", [128, 512])
        num_ps = fw.ps("num_ps", [128, 512])
        dot_ps = fw.ps("dot_ps", [128, 512])
        m_aT = misc
        m_st = misc
        m_tp = misc
        m_sm = misc
        x32 = [fw.sb(f"x32_{i}", [128, 512]) for i in range(2)]
        xsq = fw.sb("xsq", [128, 8, 512], BF16)
        xg = fw.sb("xg", [128, 8, 512], BF16)
        rstd = fw.sb("rstd", [128, 512])
        rcol = fw.sb("rcol", [128, 4])
        F = {}
        for nm in ["t0", "t1", "t2", "t3", "t4", "t5", "t6", "t7", "t8", "t9", "t10"]:
            F[nm] = fw.sb(nm, [128, 512])
        Bh = {}
        for nm in ["b0", "b1", "b2", "b3", "b4", "b5", "b6"]:
            Bh[nm] = fw.sb(nm, [128, 512], BF16)
        Pt = [fw.sb(f"Pt{i}", [128, 512], BF16) for i in range(3)]
        kst_tm = fw.sb("kst_tm", [128, 4, 128], BF16)
        vb_tm = fw.sb("vb_tm", [128, 4, 128], BF16)
        vc_tm = fw.sb("vc_tm", [128, 4, 129], BF16)
        A2 = [fw.sb(f"A2_{i}", [128, 128], BF16) for i in range(2)]
        dec = fw.sb("dec", [128, 8])
        yo32 = fw.sb("yout0", [128, 512])
        yob = fw.sb("yout_bf", [128, 512], BF16)

        sp.dma(pv[:, :], pv_d[:, :])
        sp.dma(ident_f[:, :], cst[:, 0:128])
        sp.dma(mask2[:, :], cst[:, 128:256])
        sp.dma(rmask[:, :], cst[:, 256:768])
        sp.dma(maskD[:, :], cst[:, 768:896])
        sp.dma(maskX[:, :], cst[:, 896:1024])
        sp.dma(bdt[:, :, :], bd_d.rearrange("p (j q) -> p j q", j=4))
        lamrow = Buf(F["t3"].t[0:1, 0:256], F["t3"].tok)
        sp.dma(lamrow[:, :], rowv[:, :])
        fw.memset(pool, ones_b[:, :], 1.0)
        fw.memset(pool, ones_f[:, :], 1.0)
        fw.copy(dve, ident_b[:, :], ident_f[:, :])
        fw.memset(dve, Sb[:, :], 0.0)
        fw.memset(dve, Sb_bf[:, :], 0.0)
        fw.memset(dve, Sc[:, :], 0.0)
        fw.memset(dve, Sc_bf[:, :], 0.0)
        fw.memset(pool, n_bc[:, :], 0.0)
        fw.memset(pool, ucq[:, 0:3], 0.0)
        fw.memset(pool, uck[:, 0:3], 0.0)
        fw.memset(pool, vc_tm[:, :, 128:129], 1.0)
        w_v = w_all.rearrange("(c p) n -> p c n", p=128)
        for kc in range(8):
            pool.dma(w_bf[:, kc, :], w_v[:, kc, :])
        for m in range(2):
            for tt_ in range(NT):
                pool.dma(ka[m].v((slice(64, 69), slice(512 * tt_, 512 * tt_ + 512)), ka_tok[m][tt_]),
                         alk[:, 512 * tt_:512 * tt_ + 512])
        cond = F["t2"]
        fw.actv(cond[:, 0:8], pv[:, PV_COND:PV_COND + 8], AF.Silu)
        modw_v = modw.rearrange("(c p) n -> p c n", p=128)
        for g in range(16):
            stg = [F["t0"], F["t1"]]
            for hh in range(2):
                sp.dma(R(stg[hh].t[:, :].rearrange("p (c n) -> p c n", c=4), stg[hh].tok),
                       modw_v[:, 4 * hh:4 * hh + 4, g * 128:(g + 1) * 128])
            for kc in range(8):
                fw.mm(m_sm[:, 448 + g:449 + g], stg[kc // 4][:, (kc % 4) * 128:(kc % 4 + 1) * 128],
                      cond[:, kc:kc + 1], start=(kc == 0), stop=(kc == 7))
        fw.tt(dve, modv[:, :], m_sm[:, 448:464], pv[:, PV_BSH:PV_BSH + 16], ALU.add)
        fw.stt(dve, g1v[:, :], modv[:, 8:16], 1.0, pv[:, PV_NW:PV_NW + 8], ALU.add, ALU.mult)
        fw.copy(dve, sh_b[:, :], modv[:, 0:8])
        for kc in range(8):
            fw.ts(fw.alt(), sh_rep[:, kc, :], ones_b[:, :], modv[:, kc:kc + 1], ALU.mult)
        for i, (nm, wd) in enumerate(A_CH):
            off = A_OFF[nm][0]
            for kc in range(8):
                fw.mm(m_sm[0:wd, 448 + i:449 + i], w_bf[:, kc, off:off + wd], sh_b[:, kc:kc + 1],
                      start=(kc == 0), stop=(kc == 7))
        fw.copy(dve, bias_c[:, 0:16], m_sm[:, 448:464])
        for i, nm in enumerate(["av", "bi", "cv"]):
            off = A_OFF[nm][0]
            for kc in range(8):
                fw.mm(m_aT[:, 0:128], sh_rep[:, kc, :], w_bf[:, kc, off:off + 128],
                      start=(kc == 0), stop=(kc == 7))
            fw.copy(dve, bias_r[:, i, :], m_aT[:, 0:128])
        fw.tt(dve, cvec[:, 7:8], bias_c[:, A_IDX["ci"]:A_IDX["ci"] + 1], pv[:, PV_IB:PV_IB + 1], ALU.add)
        fw.tt(dve, cvec[:, 8:9], bias_c[:, A_IDX["cf"]:A_IDX["cf"] + 1], pv[:, PV_FB:PV_FB + 1], ALU.add)
        fw.ts(dve, cvec[:, 0:1], pv[:, PV_QNW:PV_QNW + 1], 0.125, ALU.mult)
        fw.copy(dve, cvec[:, 1:2], pv[:, PV_KNW:PV_KNW + 1])
        lbe = F["t0"]
        fw.actv(lbe[:, 0:4], pv[:, PV_LBL:PV_LBL + 4], AF.Exp)
        fw.tt(dve, lbe[:, 4:8], lbe[:, 0:4], pv[:, PV_LMASK:PV_LMASK + 4], ALU.mult)
        dve.op(lambda e: e.reduce_sum(out=lbe.t[:, 8:9], in_=lbe.t[:, 0:4], axis=mybir.AxisListType.X),
               [lbe.tok], [lbe.tok])
        dve.op(lambda e: e.reduce_sum(out=lbe.t[:, 9:10], in_=lbe.t[:, 4:8], axis=mybir.AxisListType.X),
               [lbe.tok], [lbe.tok])
        fw.recip(lbe[:, 10:11], lbe[:, 8:9])
        fw.tt(dve, cvec[:, 2:3], lbe[:, 9:10], lbe[:, 10:11], ALU.mult)
        fw.ts(dve, cvec[:, 3:4], cvec[:, 2:3], -1.0, ALU.mult, 1.0, ALU.add)
        fw.tt(dve, lamrow[:, 0:64], lamrow[:, 0:64], lamrow[:, 64:128], ALU.mult)
        fw.tt(dve, lamrow[:, 128:192], lamrow[:, 128:192], lamrow[:, 192:256], ALU.mult)
        dve.op(lambda e: e.reduce_sum(out=lamt.t[:, 0:1], in_=lamrow.t[:, 0:64], axis=mybir.AxisListType.X),
               [lamrow.tok], [lamt.tok])
        dve.op(lambda e: e.reduce_sum(out=lamt.t[:, 1:2], in_=lamrow.t[:, 128:192], axis=mybir.AxisListType.X),
               [lamrow.tok], [lamt.tok])
        fw.actv(lamt[:, 2:4], lamt[:, 0:2], AF.Exp)
        fw.tt(dve, lamt[:, 4:5], lamt[:, 2:3], lamt[:, 3:4], ALU.subtract)
        fw.mm(m_sm[:, 470:471], ones_f[0:1, :], lamt[0:1, 4:5])
        fw.tt(dve, cvec[:, 4:5], m_sm[:, 470:471], pv[:, PV_LAMI:PV_LAMI + 1], ALU.add)
        fw.ts(dve, cvec[:, 5:6], cvec[:, 4:5], -1.0, ALU.mult)
        fw.tt(dve, cvec[:, 6:7], pv[:, PV_SUBLN:PV_SUBLN + 1], pv[:, PV_1MLAMI:PV_1MLAMI + 1], ALU.mult)

        def rmsnorm_out(o_sb, wcol, gate, sqb):
            fw.actv(sqb[:, :], o_sb[:, :], AF.Square)
            fw.mm(pj[:, :], ones_b[:, :], sqb[:, :])
            rms_rstd(fw, F["t0"][:, :], pj[:, :], 128, F["t0"][:, :])
            if gate is None:
                fw.stt(dve, yob[:, :], o_sb[:, :], wcol, F["t0"][:, :], ALU.mult, ALU.mult)
            else:
                fw.stt(dve, yo32[:, :], o_sb[:, :], wcol, F["t0"][:, :], ALU.mult, ALU.mult)
                fw.tt(pool, yob[:, :], yo32[:, :], gate[:, :], ALU.mult)

        for t in range(NT):
            c0 = 512 * t
            for m in range(2):
                pool.dma(qa[m][64:69, :], alq[:, c0:c0 + 512])
            for kc in range(8):
                xs = x32[kc % 2]
                sp.dma(xs[:, :], xload(kc, t))
                fw.actv(xsq[:, kc, :], xs[:, :], AF.Square)
                fw.ts(fw.alt(), xg[:, kc, :], xs[:, :], g1v[:, kc:kc + 1], ALU.mult)
            for kc in range(8):
                fw.mm(pj[:, :], ones_b[:, :], xsq[:, kc, :], start=(kc == 0), stop=(kc == 7))
            rms_rstd(fw, rstd[:, :], pj[:, :], D, F["t0"][:, :])
            for kc in range(8):
                fw.tt(fw.alt(), xg[:, kc, :], xg[:, kc, :], rstd[:, :], ALU.mult)

            def proj_mm(nm):
                off, wd = A_OFF[nm]
                for kc in range(8):
                    fw.mm(pj[0:wd, :], w_bf[:, kc, off:off + wd], xg[:, kc, :],
                          start=(kc == 0), stop=(kc == 7))

            def bcol(nm, wd=128):
                i = A_IDX[nm]
                return bias_c[0:wd, i:i + 1]

            def proj_tm(nm, bi, dst3, ncols=128):
                off, wd = A_OFF[nm]
                for blk in range(4):
                    for kc in range(8):
                        fw.mm(m_aT[:, 0:128], xg[:, kc, blk * 128:(blk + 1) * 128], w_bf[:, kc, off:off + 128],
                              start=(kc == 0), stop=(kc == 7))
                    fw.tt(dve, dst3[:, blk, 0:128], m_aT[:, 0:128], bias_r[:, bi, :], ALU.add)

            for nm, m, isq in [("q1", 0, True), ("q2", 1, True), ("k1", 0, False), ("k2", 1, False)]:
                raw = F["t1"]
                proj_mm(nm)
                fw.ts(dve, raw[0:64, :], pj[0:64, :], bcol(nm, 64), ALU.add)
                sqb = Bh["b0"]
                fw.actv(sqb[0:64, :], raw[0:64, :], AF.Square)
                fw.mm(pj[0:64, :], ones_b[0:64, 0:64], sqb[0:64, :])
                rms_rstd(fw, F["t2"][0:64, :], pj[0:64, :], 64, F["t2"][0:64, :])
                if isq:
                    fw.stt(dve, qa[m][0:64, :], raw[0:64, :], cvec[0:64, 0:1], F["t2"][0:64, :],
                           ALU.mult, ALU.mult)
                else:
                    fw.stt(dve, ka[m].v((slice(0, 64), slice(c0, c0 + 512)), ka_tok[m][t]),
                           raw[0:64, :], cvec[0:64, 1:2], F["t2"][0:64, :], ALU.mult, ALU.mult)
            va_t = Buf(va.t, va_tok[t])
            off = A_OFF["av"][0]
            for blk in range(4):
                for kc in range(8):
                    fw.mm(m_aT[:, 0:128], xg[:, kc, blk * 128:(blk + 1) * 128], w_bf[:, kc, off:off + 128],
                          start=(kc == 0), stop=(kc == 7))
                fw.tt(dve, va_t[:, 4 * t + blk, :], m_aT[:, 0:128], bias_r[:, 0, :], ALU.add)

            qf, ff, logf, bb, kmid, est, gs = F["t1"], F["t2"], F["t3"], F["t4"], F["t5"], F["t6"], F["t7"]
            proj_mm("bq")
            fw.actv(qf[:, :], pj[:, :], AF.Silu, bias=bcol("bq"))
            proj_mm("bg")
            fw.actv(gs[:, :], pj[:, :], AF.Silu, bias=bcol("bg"))
            proj_mm("bf")
            fw.actv(ff[:, :], pj[:, :], AF.Sigmoid, bias=bcol("bf"))
            fw.ts(dve, ff[:, :], ff[:, :], cvec[:, 3:4], ALU.mult, cvec[:, 2:3], ALU.add)
            fw.actv(logf[:, :], ff[:, :], AF.Ln)
            fw.ts(pool, ff[:, :], ff[:, :], -1.0, ALU.mult, 1.0, ALU.add)
            dve.op(lambda e: e.tensor_tensor_scan(out=bb.t[:, :], data0=rmask.t[:, :], data1=logf.t[:, :],
                                                  initial=0.0, op0=ALU.mult, op1=ALU.add),
                   [rmask.tok, logf.tok], [bb.tok])
            qin_b, kst_b = Bh["b1"], Bh["b4"]
            fw.actv(est[:, :], bb[:, :], AF.Exp)
            fw.tt(dve, qin_b[:, :], qf[:, :], est[:, :], ALU.mult)
            qx_b, kx_b = Bh["b2"], Bh["b3"]
            qd_b, kd_b = Bh["b5"], Bh["b6"]
            for c in range(8):
                cs = slice(64 * c, 64 * c + 64)
                fw.actv(kmid[:, cs], bb[:, cs], AF.Exp, bias=bb[:, 64 * c + 31:64 * c + 32], scale=-1.0)
            fw.stt(dve, kx_b[:, :], kmid[:, :], 1.0, ff[:, :], ALU.min, ALU.mult)
            fw.recip(kmid[:, :], kmid[:, :])
            fw.stt(dve, qx_b[:, :], kmid[:, :], 1.0, qf[:, :], ALU.min, ALU.mult)
            for c in range(16):
                cs = slice(32 * c, 32 * c + 32)
                fw.actv(kmid[:, cs], bb[:, cs], AF.Exp, bias=bb[:, 32 * c + 15:32 * c + 16], scale=-1.0)
            fw.tt(dve, kd_b[:, :], ff[:, :], kmid[:, :], ALU.mult)
            fw.recip(kmid[:, :], kmid[:, :])
            fw.tt(pool, qd_b[:, :], qf[:, :], kmid[:, :], ALU.mult)
            for c in range(8):
                cs = slice(64 * c, 64 * c + 64)
                fw.actv(est[:, cs], bb[:, cs], AF.Exp, bias=bb[:, 64 * c + 63:64 * c + 64], scale=-1.0)
            fw.tt(dve, kst_b[:, :], ff[:, :], est[:, :], ALU.mult)
            fw.actv(dec[:, :], R(bb.t[:, :].rearrange("p (c l) -> p c l", l=64)[:, :, 63], bb.tok), AF.Exp)
            proj_tm("bi", 1, vb_tm)
            for blk in range(4):
                fw.mm(m_tp[:, 320:448], kst_b[:, blk * 128:(blk + 1) * 128], ident_b[:, :])
                fw.copy(act, kst_tm[:, blk, :], m_tp[:, 320:448])
            for blk in range(4):
                bs = slice(128 * blk, 128 * blk + 128)
                fw.mm(m_aT[:, 0:128], kd_b[:, bs], qd_b[:, bs])
                fw.mm(dot_ps[:, 0:128], kx_b[:, bs], qx_b[:, bs])
                a2 = A2[blk % 2]
                fw.tt(dve, F["t8"][:, 0:128], m_aT[:, 0:128], maskD[:, :], ALU.mult)
                fw.tt(dve, F["t8"][:, 128:256], dot_ps[:, 0:128], maskX[:, :], ALU.mult)
                fw.tt(dve, a2[:, :], F["t8"][:, 0:128], F["t8"][:, 128:256], ALU.add)
                for half in range(2):
                    c = 2 * blk + half
                    cs = slice(64 * c, 64 * c + 64)
                    p0 = 64 * half
                    fw.mm(num_ps[:, cs], Sb_bf[:, :], qin_b[:, cs], start=True, stop=False)
                    fw.mm(num_ps[:, cs], vb_tm[:, blk, :], a2[:, 64 * half:64 * half + 64], start=False, stop=True)
                    fw.mm(m_st[:, 128:256], kst_tm[p0:p0 + 64, blk, :], vb_tm[p0:p0 + 64, blk, :])
                    fw.stt(dve, Sb[:, :], Sb[:, :], dec[:, c:c + 1], m_st[:, 128:256], ALU.mult, ALU.add)
                    fw.copy(act, Sb_bf[:, :], Sb[:, :])
            o_b = F["t5"]
            fw.copy(act, o_b[:, :], num_ps[:, :])
            rmsnorm_out(o_b, pv[:, PV_GNORM:PV_GNORM + 1], gs, Bh["b0"])
            sp.dma(ystore(t, 1), yob[:, :])

            qc, kc_, ipre, lf, bc = F["t1"], F["t2"], F["t3"], F["t4"], F["t6"]
            og = F["t7"]
            for nm, ub, cw, cb, dst in [("cq", ucq, PV_CWQ, PV_CBQ, qc), ("ck", uck, PV_CWK, PV_CBK, kc_)]:
                proj_mm(nm)
                fw.ts(dve, ub[:, 3:515], pj[:, :], bcol(nm), ALU.add)
                fw.ts(dve, dst[:, :], ub[:, 0:512], pv[:, cw:cw + 1], ALU.mult, pv[:, cb:cb + 1], ALU.add)
                for j in range(1, 4):
                    fw.stt(dve, dst[:, :], ub[:, j:j + 512], pv[:, cw + j:cw + j + 1], dst[:, :],
                           ALU.mult, ALU.add)
                fw.actv(dst[:, :], dst[:, :], AF.Silu)
                fw.copy(pool, F["t8"][:, 0:3], ub[:, 512:515])
                fw.copy(pool, ub[:, 0:3], F["t8"][:, 0:3])
            proj_mm("co")
            fw.actv(og[:, :], pj[:, :], AF.Sigmoid, bias=bcol("co"))
            proj_mm("ci")
            fw.ts(dve, ipre[:, :], pj[:, :], cvec[:, 7:8], ALU.add)
            proj_mm("cf")
            fw.actv(lf[:, :], pj[:, :], AF.Sigmoid, bias=cvec[:, 8:9])
            fw.actv(lf[:, :], lf[:, :], AF.Ln)
            dve.op(lambda e: e.tensor_tensor_scan(out=bc.t[:, :], data0=rmask.t[:, :], data1=lf.t[:, :],
                                                  initial=0.0, op0=ALU.mult, op1=ALU.add),
                   [rmask.tok, lf.tok], [bc.tok])
            eqc, imb, estc = F["t8"], F["t9"], F["t10"]
            qt_b, kt_b, kstc_b = Bh["b1"], Bh["b2"], Bh["b4"]
            fw.actv(eqc[:, :], bc[:, :], AF.Exp)
            fw.tt(dve, qt_b[:, :], qc[:, :], eqc[:, :], ALU.mult)
            fw.tt(pool, imb[:, :], ipre[:, :], bc[:, :], ALU.subtract)
            fw.actv(eqc[:, :], imb[:, :], AF.Exp)
            fw.stt(dve, kt_b[:, :], kc_[:, :], 128.0 ** -0.5, eqc[:, :], ALU.mult, ALU.mult)
            for c in range(8):
                cs = slice(64 * c, 64 * c + 64)
                fw.actv(estc[:, cs], imb[:, cs], AF.Exp, bias=bc[:, 64 * c + 63:64 * c + 64])
            fw.stt(dve, kstc_b[:, :], kc_[:, :], 128.0 ** -0.5, estc[:, :], ALU.mult, ALU.mult)
            fw.actv(dec[:, :], R(bc.t[:, :].rearrange("p (c l) -> p c l", l=64)[:, :, 63], bc.tok), AF.Exp)
            proj_tm("cv", 2, vc_tm)
            for blk in range(4):
                fw.mm(m_tp[:, 320:448], kstc_b[:, blk * 128:(blk + 1) * 128], ident_b[:, :])
                fw.copy(act, kst_tm[:, blk, :], m_tp[:, 320:448])
            for blk in range(4):
                bs = slice(128 * blk, 128 * blk + 128)
                fw.mm(m_aT[:, 0:128], kt_b[:, bs], qt_b[:, bs])
                a2 = A2[blk % 2]
                fw.tt(dve, a2[:, :], m_aT[:, 0:128], mask2[:, :], ALU.mult)
                for half in range(2):
                    c = 2 * blk + half
                    cs = slice(64 * c, 64 * c + 64)
                    hs = slice(64 * half, 64 * half + 64)
                    p0 = 64 * half
                    fw.mm(num_ps[:, cs], Sc_bf[:, :], qt_b[:, cs], start=True, stop=False)
                    fw.mm(num_ps[:, cs], vc_tm[:, blk, 0:128], a2[:, hs], start=False, stop=True)
                    fw.mm(dot_ps[:, cs], n_bc[:, :], qt_b[:, cs], start=True, stop=False)
                    fw.mm(dot_ps[:, cs], ones_b[:, :], a2[:, hs], start=False, stop=True)
                    fw.mm(m_st[:, 128:257], kst_tm[p0:p0 + 64, blk, :], vc_tm[p0:p0 + 64, blk, :])
                    fw.stt(dve, Sc[:, :], Sc[:, :], dec[:, c:c + 1], m_st[:, 128:257], ALU.mult, ALU.add)
                    fw.copy(act, Sc_bf[:, :], Sc[:, 0:128])
                    fw.ts(pool, n_bc[:, :], ones_b[:, :], Sc[:, 128:129], ALU.mult)
            den, h_b = F["t8"], F["t9"]
            fw.ts(dve, den[:, :], dot_ps[:, :], -1.0, ALU.mult, 1.0, ALU.max)
            fw.tt(dve, den[:, :], den[:, :], dot_ps[:, :], ALU.max)
            fw.recip(den[:, :], den[:, :])
            fw.tt(dve, h_b[:, :], num_ps[:, :], den[:, :], ALU.mult)
            rmsnorm_out(h_b, pv[:, PV_CNORM:PV_CNORM + 1], og, Bh["b0"])
            sp.dma(ystore(t, 2), yob[:, :])

            om = [F["t1"], F["t2"]]
            lacc = [F["t8"], F["t9"]]
            nblk = 4 * (t + 1)
            pi = 0
            for m in range(2):
                for j in range(nblk):
                    scb = sc_ps[j % 2]
                    P = Pt[pi % 3]
                    pi += 1
                    tj = j // 4
                    kreg = lambda rows: ka[m].v((rows, slice(128 * j, 128 * j + 128)), ka_tok[m][tj])
                    if j < 4 * t:
                        fw.mm(scb[:, :], kreg(slice(0, 69)), qa[m][0:69, :])
                        fw.actv(P[:, :], scb[:, :], AF.Exp)
                    else:
                        jj = j - 4 * t
                        fw.mm(scb[:, :], kreg(slice(0, 64)), qa[m][0:64, :])
                        fw.tt(dve, F["t10"][:, :], scb[:, :], bdt[:, jj, :], ALU.add)
                        fw.actv(P[:, :], F["t10"][:, :], AF.Exp)
                    vreg = va.v((slice(None), j, slice(None)), va_tok[tj])
                    fw.mm(O_ps[:, :], vreg, P[:, :], start=(j == 0), stop=(j == nblk - 1))
                    ai = 1 if (j % 3 == 2) else 0
                    aeng = pool if ai else dve
                    if j == (2 if ai else 0):
                        fw.copy(aeng, lacc[ai][:, :], P[:, :])
                    else:
                        fw.tt(aeng, lacc[ai][:, :], lacc[ai][:, :], P[:, :], ALU.add)
                fw.tt(dve, Bh["b1"][:, :], lacc[0][:, :], lacc[1][:, :], ALU.add)
                fw.mm(L_ps[:, :], ones_b[:, :], Bh["b1"][:, :])
                fw.recip(F["t10"][:, :], L_ps[:, :])
                fw.tt(dve, om[m][:, :], O_ps[:, :], F["t10"][:, :], ALU.mult)
            o_a = F["t4"]
            fw.stt(dve, o_a[:, :], om[1][:, :], cvec[:, 5:6], om[0][:, :], ALU.mult, ALU.add)
            rmsnorm_out(o_a, cvec[:, 6:7], None, Bh["b0"])
            sp.dma(ystore(t, 0), yob[:, :])

        fw.barrier()
        fw.stack = top_stack


def _pk(v):
    return np.ascontiguousarray(np.asarray(v, np.float32).reshape(8, 128).T)


def consts_A(h, S):
    slope = 2.0 ** (-2.0 * (h + 1))
    cst = np.zeros((128, NCST), np.float32)
    cst[:, 0:128] = np.eye(128, dtype=np.float32)
    s = np.arange(128)[:, None]
    t = np.arange(128)[None, :]
    cst[:, 128:256] = ((s // 64 == t // 64) & (s <= t)).astype(np.float32)
    rm = np.ones(512, np.float32)
    rm[::64] = 0.0
    cst[:, 256:768] = rm[None, :]
    cst[:, 768:896] = ((s // 32 == t // 32) & (s <= t)).astype(np.float32)
    cst[:, 896:1024] = ((s // 64 == t // 64) & (s % 64 < 32) & (t % 64 >= 32)).astype(np.float32)
    kk = np.arange(128)[:, None]
    qq = np.arange(512)[None, :]
    bd = np.zeros((128, 4, 512), np.float32)
    for jj in range(4):
        kpos = 128 * jj + kk
        allowed = (kpos // 64) <= (qq // 64)
        bd[:, jj, :] = np.where(allowed, -slope * np.abs(qq - kpos), MASKVAL)
    pos = np.arange(S)
    one = np.ones(S)
    alq = np.stack([-slope * 512.0 * (pos // 512), -slope * 256.0 * ((pos % 512) // 256),
                    -slope * (pos % 256), one, one]).astype(np.float32)
    alk = np.stack([one, one, one, slope * 128.0 * (pos // 128), slope * (pos % 128)]).astype(np.float32)
    return cst, np.ascontiguousarray(bd.reshape(128, 2048)), alq, alk


def prep_A(inp, l, b, h):
    w = inp["w_in"][l]
    cols = []
    for nm, wd in A_CH:
        if nm in ("q1", "q2", "k1", "k2"):
            base = {"q1": 0, "q2": 256, "k1": 512, "k2": 768}[nm] + h * 64
            cols.append(w[:, base:base + 64])
        elif nm == "ci":
            cols.append(np.repeat(w[:, 5632 + h:5633 + h], 128, axis=1))
        elif nm == "cf":
            cols.append(np.repeat(w[:, 5636 + h:5637 + h], 128, axis=1))
        else:
            base = {"av": 1024, "bq": 1536, "bf": 2048, "bi": 2560, "bg": 3072, "cq": 3584,
                    "ck": 4096, "cv": 4608, "co": 5120}[nm] + h * 128
            cols.append(w[:, base:base + 128])
    w_all = np.ascontiguousarray(np.concatenate(cols, axis=1), dtype=np.float32)
    pv = np.zeros((128, NPV), np.float32)
    pv[:, PV_COND:PV_COND + 8] = _pk(inp["c"][b])
    pv[:, PV_NW:PV_NW + 8] = _pk(inp["norm1_w"][l])
    pv[:, PV_BSH:PV_BSH + 8] = _pk(inp["b_mod"][l][0:1024])
    pv[:, PV_BSC:PV_BSC + 8] = _pk(inp["b_mod"][l][1024:2048])
    pv[:, PV_QNW] = np.tile(inp["a_qnorm_w"][l], 2)
    pv[:, PV_KNW] = np.tile(inp["a_knorm_w"][l], 2)
    pv[:, PV_SUBLN] = inp["a_subln_w"][l]
    pv[:, PV_GNORM] = inp["b_gnorm_w"][l]
    pv[:, PV_CNORM] = inp["c_norm_w"][l]
    for j in range(4):
        pv[:, PV_CWQ + j] = inp["c_conv_w"][l][j, h * 128:(h + 1) * 128]
        pv[:, PV_CWK + j] = inp["c_conv_w"][l][j, 512 + h * 128:512 + (h + 1) * 128]
    pv[:, PV_CBQ] = inp["c_conv_b"][l][h * 128:(h + 1) * 128]
    pv[:, PV_CBK] = inp["c_conv_b"][l][512 + h * 128:512 + (h + 1) * 128]
    pv[:, PV_LBL:PV_LBL + 4] = inp["b_lb_logits"][:, h * 128:(h + 1) * 128].T
    for j in range(4):
        pv[:, PV_LMASK + j] = 1.0 if 1 <= j <= l else 0.0
    pv[:, PV_IB] = inp["c_igate_b"][l][h]
    pv[:, PV_FB] = inp["c_fgate_b"][l][h]
    lam_init = 0.8 - 0.6 * math.exp(-0.3 * l)
    pv[:, PV_LAMI] = lam_init
    pv[:, PV_1MLAMI] = 1.0 - lam_init
    rowv = np.concatenate([inp["a_lambda_q1"][l], inp["a_lambda_k1"][l],
                           inp["a_lambda_q2"][l], inp["a_lambda_k2"][l]]).astype(np.float32)[None, :]
    return {f"w_all{l}": w_all, f"pvA{l}": pv, f"rowv{l}": np.ascontiguousarray(rowv)}


NPVB = 80
PB_COND, PB_NW1, PB_NW2, PB_BMOD = 0, 8, 16, 24
FC_GROUPS = [(0, 4), (4, 4), (8, 4), (12, 4), (16, 4), (20, 2)]


def emit_B(nc, fw, SC, moe, d):
    NEX = NE if moe else 1
    ST = 2048 if SC >= 2048 else SC
    NST = SC // ST
    TPS = ST // 512
    wg, wbr, wout, modw, pv_d = d["wg"], d["wbr"], d["wout"], d["modw"], d["pvB"]
    w1, w3, w2, ident_d, yidx_d = d["w1"], d["w3"], d["w2"], d["ident"], d["yidx"]
    xsrc, xdst, ytab = d["xsrc"], d["xdst"], d["ytab"]
    if moe:
        wr, rb_d = d["wr"], d["rb"]
    wg_v = wg.rearrange("(c p) n -> p c n", p=128)
    w1_v = w1.rearrange("(e c p) n -> p e c n", p=128, c=8)
    w3_v = w3.rearrange("(e c p) n -> p e c n", p=128, c=8)
    w2_v = w2.rearrange("(e f p) n -> p e f n", p=128, f=22)
    modw_v = modw.rearrange("(c p) n -> p c n", p=128)

    with contextlib.ExitStack() as st:
        top_stack0 = fw.stack
        fw.stack = st
        dve, pool, act, pe, sp = fw.dve, fw.pool, fw.act, fw.pe, fw.sp
        yidx = fw.sb("yidx", [128, 8], mybir.dt.int32)
        YPS = 2 if (SC // 512) % 2 == 0 else 1
        sp.dma(yidx[:, :], yidx_d[:, :])
        pv = fw.sb("pv", [128, NPVB])
        ones_b = fw.sb("ones_b", [128, 128], BF16)
        ident_f = fw.sb("ident_f", [128, 128])
        modv = fw.sb("modv", [128, 48])
        g1v = fw.sb("g1v", [128, 8])
        g2v = fw.sb("g2v", [128, 8])
        sh1_b = fw.sb("sh1_b", [128, 8], BF16)
        sh2_b = fw.sb("sh2_b", [128, 8], BF16)
        bias_g = fw.sb("bias_g", [128, 24])
        x1buf = fw.sb("x1buf", [128, 8, ST])
        xg2 = fw.sb("xg2", [128, 8, ST], BF16)
        rstd2 = fw.sb("rstd2", [128, ST])
        x1_tok = [Tok() for _ in range(TPS)]
        xg2_tok = [Tok() for _ in range(TPS)]
        r2_tok = [Tok() for _ in range(TPS)]
        T = {nm: fw.sb(nm, [128, 512]) for nm in ["u0", "u1", "u2", "u3"]}
        if moe:
            wr_f = fw.sb("wr_f", [128, 8, 8])
            wr_s = fw.sb("wr_s", [128, 8, 8])
            sh2_f = fw.sb("sh2_f", [128, 8])
            rbias = fw.sb("rbias", [8, 2])
            combT = fw.sb("combT", [8, ST])
            cb_tok = [Tok() for _ in range(TPS)]
            rt = fw.sb("rt", [128, 64])
        pA = [fw.ps(f"pA{i}", [128, 512]) for i in range(2)]
        pB = [fw.ps(f"pB{i}", [128, 512]) for i in range(2)]
        pC = [fw.ps(f"pC{i}", [128, 512]) for i in range(2)]
        pM = fw.ps("pM", [128, 512])
        pBC = fw.ps("pBC", [128, 512])

        sp.dma(pv[:, :], pv_d[:, :])
        sp.dma(ident_f[:, :], ident_d[:, :])
        fw.memset(pool, ones_b[:, :], 1.0)
        cond = T["u2"]
        fw.actv(cond[:, 0:8], pv[:, PB_COND:PB_COND + 8], AF.Silu)
        for g in range(48):
            stg = [T["u0"], T["u1"]]
            for hh in range(2):
                sp.dma(R(stg[hh].t[:, :].rearrange("p (c n) -> p c n", c=4), stg[hh].tok),
                       modw_v[:, 4 * hh:4 * hh + 4, g * 128:(g + 1) * 128])
            for kc in range(8):
                fw.mm(pM[:, g:g + 1], stg[kc // 4][:, (kc % 4) * 128:(kc % 4 + 1) * 128],
                      cond[:, kc:kc + 1], start=(kc == 0), stop=(kc == 7))
        fw.tt(dve, modv[:, :], pM[:, 0:48], pv[:, PB_BMOD:PB_BMOD + 48], ALU.add)
        fw.stt(dve, g1v[:, :], modv[:, 8:16], 1.0, pv[:, PB_NW1:PB_NW1 + 8], ALU.add, ALU.mult)
        fw.stt(dve, g2v[:, :], modv[:, 32:40], 1.0, pv[:, PB_NW2:PB_NW2 + 8], ALU.add, ALU.mult)
        fw.copy(dve, sh1_b[:, :], modv[:, 0:8])
        fw.copy(dve, sh2_b[:, :], modv[:, 24:32])
        if moe:
            sp.dma(wr_f[:, :, :], wr.rearrange("(c p) e -> p c e", p=128))
            sp.dma(rbias[:, 0:1], rb_d[:, :])
            fw.copy(dve, sh2_f[:, :], modv[:, 24:32])
            for kc in range(8):
                fw.ts(dve, wr_s[:, kc, :], wr_f[:, kc, :], g2v[:, kc:kc + 1], ALU.mult)
                fw.mm(pM[0:8, 60:61], wr_f[:, kc, :], sh2_f[:, kc:kc + 1], start=(kc == 0), stop=(kc == 7))
            fw.tt(dve, rbias[:, 1:2], pM[0:8, 60:61], rbias[:, 0:1], ALU.add)

        wgc = None
        fw.cc_wait()
        for s_i in range(NST):
            with contextlib.ExitStack() as st1:
                top_stack = fw.stack
                fw.stack = st1
                wbr_bf = fw.sb(f"wbr_bf{s_i}", [128, 12, D], BF16)
                wout_bf = fw.sb(f"wout_bf{s_i}", [128, 8, D], BF16)
                wgc = [fw.sb(f"wgc{s_i}_{i}", [128, 8, 384], BF16) for i in range(2)]
                xsq = fw.sb(f"xsq{s_i}", [128, 8, 512], BF16)
                xg1 = fw.sb(f"xg1{s_i}", [128, 8, 512], BF16)
                ybf = fw.sb(f"ybf{s_i}", [128, 12, 512], BF16)
                mrg = xsq
                rstd1 = fw.sb(f"rstd1{s_i}", [128, 512])
                fw.stack = top_stack
                for j in range(12):
                    pool.dma(wbr_bf[:, j, :], wbr[j * 128:(j + 1) * 128, :])
                for kc in range(8):
                    pool.dma(wout_bf[:, kc, :], wout[kc * 128:(kc + 1) * 128, :])
                wi = 0

                def load_wgc(mc):
                    nonlocal wi
                    buf = wgc[wi % 2]
                    wi += 1
                    for br in range(3):
                        pool.dma(buf[:, :, br * 128:(br + 1) * 128],
                                 wg_v[:, :, br * 1024 + mc * 128:br * 1024 + (mc + 1) * 128])
                    return buf

                if s_i == 0:
                    for mc in range(8):
                        buf = load_wgc(mc)
                        for br in range(3):
                            for kc in range(8):
                                fw.mm(pM[:, 64 + br * 8 + mc:65 + br * 8 + mc], buf[:, kc, br * 128:(br + 1) * 128],
                                      sh1_b[:, kc:kc + 1], start=(kc == 0), stop=(kc == 7))
                    fw.copy(dve, bias_g[:, :], pM[:, 64:88])
                for tl in range(TPS):
                    c0 = s_i * ST + tl * 512
                    cs = slice(tl * 512, tl * 512 + 512)
                    x1r = lambda kc: x1buf.v((slice(None), kc, cs), x1_tok[tl])
                    for kc in range(8):
                        sp.dma(x1r(kc), xsrc(kc, c0))
                    for kc in range(8):
                        fw.actv(xsq[:, kc, :], x1r(kc), AF.Square)
                    for kc in range(8):
                        fw.mm(pM[:, :], ones_b[:, :], xsq[:, kc, :], start=(kc == 0), stop=(kc == 7))
                    rms_rstd(fw, rstd1[:, :], pM[:, :], D, T["u0"][:, :])
                    for kc in range(8):
                        fw.stt(dve, xg1[:, kc, :], x1r(kc), g1v[:, kc:kc + 1], rstd1[:, :], ALU.mult, ALU.mult)
                    for j in range(12):
                        ic = ((c0 // 512) % YPS) * 4 + j % 4
                        fw.gather(ybf[:, j, :], ytab(c0 // 512, j // 4), yidx[:, ic:ic + 1])
                    for mc in range(8):
                        buf = load_wgc(mc)
                        mg = T["u1"]
                        for br in range(3):
                            pa, pb = pA[br % 2], pB[br % 2]
                            for kc in range(8):
                                fw.mm(pa[:, :], buf[:, kc, br * 128:(br + 1) * 128], xg1[:, kc, :],
                                      start=(kc == 0), stop=(kc == 7))
                            for k4 in range(4):
                                fw.mm(pb[:, :], wbr_bf[:, br * 4 + k4, mc * 128:(mc + 1) * 128], ybf[:, br * 4 + k4, :],
                                      start=(k4 == 0), stop=(k4 == 3))
                            gt = T["u2"]
                            fw.actv(gt[:, :], pa[:, :], AF.Sigmoid, bias=bias_g[:, br * 8 + mc:br * 8 + mc + 1])
                            if br == 0:
                                fw.tt(dve, mg[:, :], gt[:, :], pb[:, :], ALU.mult)
                            else:
                                tmp = T["u3"]
                                fw.tt(dve, tmp[:, :], gt[:, :], pb[:, :], ALU.mult)
                                if br == 1:
                                    fw.tt(pool, mg[:, :], mg[:, :], tmp[:, :], ALU.add)
                                else:
                                    fw.tt(pool, mrg[:, mc, :], mg[:, :], tmp[:, :], ALU.add)
                    for mc in range(8):
                        po = pC[mc % 2]
                        for kc in range(8):
                            fw.mm(po[:, :], wout_bf[:, kc, mc * 128:(mc + 1) * 128], mrg[:, kc, :],
                                  start=(kc == 0), stop=(kc == 7))
                        fw.stt(dve, x1r(mc), po[:, :], modv[:, 16 + mc:17 + mc], x1r(mc), ALU.mult, ALU.add)
                    for kc in range(8):
                        fw.actv(xsq[:, kc, :], x1r(kc), AF.Square)
                    for kc in range(8):
                        fw.mm(pM[:, :], ones_b[:, :], xsq[:, kc, :], start=(kc == 0), stop=(kc == 7))
                    r2 = rstd2.v((slice(None), cs), r2_tok[tl])
                    rms_rstd(fw, r2, pM[:, :], D, T["u0"][:, :])
                    for kc in range(8):
                        fw.stt(dve, xg2.v((slice(None), kc, cs), xg2_tok[tl]), x1r(kc), g2v[:, kc:kc + 1], r2,
                               ALU.mult, ALU.mult)
                    if moe:
                        for kc in range(8):
                            fw.mm(pM[0:8, :], wr_s[:, kc, :], x1r(kc), start=(kc == 0), stop=(kc == 7))
                        lg = T["u2"]
                        fw.tt(dve, lg[0:8, :], pM[0:8, :], R(rstd2.t[0:8, cs], r2_tok[tl]), ALU.mult)
                        fw.ts(dve, lg[0:8, :], lg[0:8, :], rbias[:, 1:2], ALU.add)
                        for blk in range(4):
                            bs = slice(blk * 128, blk * 128 + 128)
                            fw.mm(pM[:, 0:8], lg[0:8, bs], ident_f[0:8, 0:8])
                            L8 = rt[:, 0:8]
                            fw.copy(dve, L8, pM[:, 0:8])
                            dve.op(lambda e: e.reduce_max(out=rt.t[:, 8:9], in_=rt.t[:, 0:8], axis=mybir.AxisListType.X),
                                   [rt.tok], [rt.tok])
                            fw.ts(dve, rt[:, 16:24], rt[:, 0:8], rt[:, 8:9], ALU.is_equal, -1e30, ALU.mult)
                            fw.tt(dve, rt[:, 16:24], rt[:, 16:24], rt[:, 0:8], ALU.add)
                            dve.op(lambda e: e.reduce_max(out=rt.t[:, 9:10], in_=rt.t[:, 16:24], axis=mybir.AxisListType.X),
                                   [rt.tok], [rt.tok])
                            fw.ts(dve, rt[:, 24:32], rt[:, 0:8], rt[:, 9:10], ALU.is_ge)
                            fw.ts(dve, rt[:, 10:11], rt[:, 8:9], -1.0, ALU.mult)
                            fw.actv(rt[:, 32:40], rt[:, 0:8], AF.Exp, bias=rt[:, 10:11])
                            fw.tt(dve, rt[:, 32:40], rt[:, 32:40], rt[:, 24:32], ALU.mult)
                            dve.op(lambda e: e.reduce_sum(out=rt.t[:, 11:12], in_=rt.t[:, 32:40], axis=mybir.AxisListType.X),
                                   [rt.tok], [rt.tok])
                            fw.recip(rt[:, 12:13], rt[:, 11:12])
                            fw.ts(dve, rt[:, 40:48], rt[:, 32:40], rt[:, 12:13], ALU.mult)
                            fw.mm(pM[0:8, 128:256], rt[:, 40:48], ident_f[:, :])
                            fw.copy(dve, combT.v((slice(None), slice(tl * 512 + blk * 128, tl * 512 + blk * 128 + 128)), cb_tok[tl]),
                                    pM[0:8, 128:256])
                fw.barrier()
            with contextlib.ExitStack() as st2:
                top_stack = fw.stack
                fw.stack = st2
                w1g = [fw.sb(f"w1g{s_i}_{i}", [128, 8, 512], BF16) for i in range(2)]
                w3g = [fw.sb(f"w3g{s_i}_{i}", [128, 8, 512], BF16) for i in range(2)]
                w2g = [fw.sb(f"w2g{s_i}_{i}", [128, 4, D], BF16) for i in range(2)]
                hh = [fw.sb(f"hh{s_i}_{i}", [128, 4, 512], BF16) for i in range(2)]
                bfc = [fw.sb(f"bfc{s_i}_{i}", [128, 8]) for i in range(2)]
                if moe:
                    cbs = fw.sb(f"cbs{s_i}", [128, TPS, 512])
                fw.stack = top_stack
                T1 = [T["u0"], T["u1"]]
                T3 = [T["u2"], T["u3"]]
                gi = 0
                hi = 0
                for e in range(NEX):
                    if moe:
                        for tl in range(TPS):
                            cs = slice(tl * 512, tl * 512 + 512)
                            fw.mm(pBC[:, :], R(ident_f.t[0:8, e:e + 1].to_broadcast([8, 128]), ident_f.tok),
                                  combT.v((slice(None), cs), cb_tok[tl]))
                            fw.copy(act, cbs[:, tl, :], pBC[:, :])
                    for (f0, fn) in FC_GROUPS:
                        b_ = gi % 2
                        gi += 1
                        pool.dma(w1g[b_][:, :, 0:fn * 128], w1_v[:, e, :, f0 * 128:(f0 + fn) * 128])
                        pool.dma(w3g[b_][:, :, 0:fn * 128], w3_v[:, e, :, f0 * 128:(f0 + fn) * 128])
                        pool.dma(w2g[b_][:, 0:fn, :], w2_v[:, e, f0:f0 + fn, :])
                        for f in range(fn):
                            for kc in range(8):
                                fw.mm(pM[:, f:f + 1], w1g[b_][:, kc, f * 128:(f + 1) * 128], sh2_b[:, kc:kc + 1],
                                      start=(kc == 0), stop=(kc == 7))
                            for kc in range(8):
                                fw.mm(pM[:, 4 + f:5 + f], w3g[b_][:, kc, f * 128:(f + 1) * 128], sh2_b[:, kc:kc + 1],
                                      start=(kc == 0), stop=(kc == 7))
                        fw.copy(dve, bfc[b_][:, :], pM[:, 0:8])
                        for tl in range(TPS):
                            cs = slice(tl * 512, tl * 512 + 512)
                            r2 = rstd2.v((slice(None), cs), r2_tok[tl])
                            hb = hh[hi % 2]
                            hi += 1
                            for f in range(fn):
                                pa, pb = pA[f % 2], pB[f % 2]
                                for kc in range(8):
                                    fw.mm(pa[:, :], w1g[b_][:, kc, f * 128:(f + 1) * 128],
                                          xg2.v((slice(None), kc, cs), xg2_tok[tl]), start=(kc == 0), stop=(kc == 7))
                                for kc in range(8):
                                    fw.mm(pb[:, :], w3g[b_][:, kc, f * 128:(f + 1) * 128],
                                          xg2.v((slice(None), kc, cs), xg2_tok[tl]), start=(kc == 0), stop=(kc == 7))
                                t1 = T1[f % 2]
                                fw.actv(t1[:, :], pa[:, :], AF.Silu, bias=bfc[b_][:, f:f + 1])
                                if moe:
                                    t3 = T3[f % 2]
                                    fw.stt(dve, t3[:, :], pb[:, :], bfc[b_][:, 4 + f:5 + f], t1[:, :], ALU.add, ALU.mult)
                                    fw.tt(pool, hb[:, f, :], t3[:, :], cbs[:, tl, :], ALU.mult)
                                else:
                                    fw.stt(dve, hb[:, f, :], pb[:, :], bfc[b_][:, 4 + f:5 + f], t1[:, :],
                                           ALU.add, ALU.mult)
                            for mc in range(8):
                                po = pC[mc % 2]
                                for f in range(fn):
                                    fw.mm(po[:, :], w2g[b_][:, f, mc * 128:(mc + 1) * 128], hb[:, f, :],
                                          start=(f == 0), stop=(f == fn - 1))
                                xr = x1buf.v((slice(None), mc, cs), x1_tok[tl])
                                fw.stt(dve, xr, po[:, :], modv[:, 40 + mc:41 + mc], xr, ALU.mult, ALU.add)
                for tl in range(TPS):
                    c0 = s_i * ST + tl * 512
                    cs = slice(tl * 512, tl * 512 + 512)
                    for kc in range(8):
                        sp.dma(xdst(kc, c0), x1buf.v((slice(None), kc, cs), x1_tok[tl]))
                fw.barrier()
        fw.stack = top_stack0


def prep_B(inp, l, b):
    i2 = l // 2
    moe = (l % 2 == 1)
    pv = np.zeros((128, NPVB), np.float32)
    pv[:, PB_COND:PB_COND + 8] = _pk(inp["c"][b])
    pv[:, PB_NW1:PB_NW1 + 8] = _pk(inp["norm1_w"][l])
    pv[:, PB_NW2:PB_NW2 + 8] = _pk(inp["norm2_w"][l])
    for k in range(6):
        pv[:, PB_BMOD + 8 * k:PB_BMOD + 8 * k + 8] = _pk(inp["b_mod"][l][k * 1024:(k + 1) * 1024])
    m = {f"wg{l}": np.ascontiguousarray(inp["w_in"][l][:, 5640:8712]),
         f"wbr{l}": np.ascontiguousarray(inp["w_branch"][l].reshape(1536, D)),
         f"wout{l}": np.ascontiguousarray(inp["w_out"][l]),
         f"modw{l}": np.ascontiguousarray(inp["w_mod"][l]),
         f"pvB{l}": pv}
    if moe:
        m[f"w1_{l}"] = np.ascontiguousarray(inp["moe_w1"][i2].reshape(NE * D, D_FF))
        m[f"w3_{l}"] = np.ascontiguousarray(inp["moe_w3"][i2].reshape(NE * D, D_FF))
        m[f"w2_{l}"] = np.ascontiguousarray(inp["moe_w2"][i2].reshape(NE * D_FF, D))
        m[f"wr{l}"] = np.ascontiguousarray(inp["moe_router_w"][i2])
        m[f"rb{l}"] = np.ascontiguousarray(inp["moe_router_b"][i2].reshape(8, 1))
    else:
        m[f"w1_{l}"] = np.ascontiguousarray(inp["ffn_w1"][i2])
        m[f"w3_{l}"] = np.ascontiguousarray(inp["ffn_w3"][i2])
        m[f"w2_{l}"] = np.ascontiguousarray(inp["ffn_w2"][i2])
    return m


GROUPS = [[0, 1, 2, 3], [4, 5, 6, 7]]


def build_fused(S, NL=DEPTH):
    SC = S // 4
    TPC = SC // 512
    HW = min(SC, 2048)
    NH = SC // HW
    nc = bass.Bass("TRN2", target_bir_lowering=False)

    def din(name, shape, dt=F32):
        return nc.dram_tensor(name, list(shape), dt, kind="ExternalInput").ap()

    xs = din("xs", [D, SC])
    cst = din("cst", [128, NCST])
    bd_d = din("bd", [128, 4 * 512])
    alq = din("alq", [5, S])
    alk = din("alk", [5, S])
    ident_d = din("ident", [128, 128])
    yidx_d = din("yidx", [128, 8], mybir.dt.int32)
    L = []
    for l in range(NL):
        moe = (l % 2 == 1)
        NEX = NE if moe else 1
        dl = {"w_all": din(f"w_all{l}", [D, NCOL_A]), "modw": din(f"modw{l}", [D, 6 * D]),
              "pvA": din(f"pvA{l}", [128, NPV]), "rowv": din(f"rowv{l}", [1, 256]),
              "wg": din(f"wg{l}", [D, 3072]), "wbr": din(f"wbr{l}", [1536, D]), "wout": din(f"wout{l}", [D, D]),
              "pvB": din(f"pvB{l}", [128, NPVB]),
              "w1": din(f"w1_{l}", [NEX * D, D_FF]), "w3": din(f"w3_{l}", [NEX * D, D_FF]),
              "w2": din(f"w2_{l}", [NEX * D_FF, D])}
        if moe:
            dl["wr"] = din(f"wr{l}", [D, 8])
            dl["rb"] = din(f"rb{l}", [8, 1])
        L.append(dl)
    xo = nc.dram_tensor("xo", [D, SC], F32, kind="ExternalOutput").ap()
    xs_v = xs.rearrange("(c p) s -> p c s", p=128)
    xo_v = xo.rearrange("(c p) s -> p c s", p=128)
    xck = [[nc.dram_tensor(f"xck_{kc}_{hf}", [128, HW], F32) for hf in range(NH)] for kc in range(8)]
    xgk = [[nc.dram_tensor(f"xgk_{kc}_{hf}", [512, HW], F32) for hf in range(NH)] for kc in range(8)]
    PS = 2 if TPC % 2 == 0 else 1
    NP = TPC // PS
    ypk = [[nc.dram_tensor(f"ypk_{tp}_{br}", [PS * 512, 512], BF16) for br in range(3)] for tp in range(NP)]
    yg = [[nc.dram_tensor(f"yg_{tp}_{br}", [4 * PS * 512, 512], BF16) for br in range(3)] for tp in range(NP)]

    def xload(kc, t):
        c0 = 512 * t
        r, o = c0 // SC, c0 % SC
        hf, col = o // HW, o % HW
        return xgk[kc][hf].ap()[r * 128:(r + 1) * 128, col:col + 512]

    def ystore(t, br):
        r, tl = t // TPC, t % TPC
        o = (tl % PS) * 512 + r * 128
        return ypk[tl // PS][br].ap()[o:o + 128, :]

    def ytab(tl, br):
        return yg[tl // PS][br].ap()

    def xsrc(kc, c0):
        return xck[kc][c0 // HW].ap()[:, c0 % HW:c0 % HW + 512]

    def xout(kc, c0):
        return xo_v[:, kc, c0:c0 + 512]

    x_pairs = [(xck[kc][hf], xgk[kc][hf]) for kc in range(8) for hf in range(NH)]
    y_pairs = [(ypk[tp][br], yg[tp][br]) for tp in range(NP) for br in range(3)]

    with contextlib.ExitStack() as st:
        fw = FW(nc, st)
        for kc in range(8):
            for hf in range(NH):
                fw.sp.dma(xck[kc][hf].ap(), xs_v[:, kc, hf * HW:(hf + 1) * HW])
        fw.all_gather(x_pairs, GROUPS)
        for l in range(NL):
            moe = (l % 2 == 1)
            dl = dict(L[l])
            dl.update(cst=cst, bd=bd_d, alq=alq, alk=alk, ident=ident_d, yidx=yidx_d,
                      xload=xload, ystore=ystore, ytab=ytab, xsrc=xsrc,
                      xdst=(xout if l == NL - 1 else xsrc))
            fw.pfx = f"L{l}A_"
            emit_A(nc, fw, S, dl)
            fw.all_gather(y_pairs, GROUPS, wait=False)
            fw.pfx = f"L{l}B_"
            emit_B(nc, fw, SC, moe, dl)
            if l < NL - 1:
                fw.all_gather(x_pairs, GROUPS)
        fw.finish([])
    return nc


def prep_core(inp, c, xT, S, NL=DEPTH):
    b, r = c // 4, c % 4
    SC = S // 4
    cst, bd, alq, alk = consts_A(r, S)
    p = np.arange(128)
    m = {"xs": np.ascontiguousarray(xT[b][:, r * SC:(r + 1) * SC], dtype=np.float32),
         "cst": cst, "bd": bd, "alq": alq, "alk": alk, "ident": np.eye(128, dtype=np.float32),
         "yidx": None}
    PS = 2 if (SC // 512) % 2 == 0 else 1
    yi = np.zeros((128, 8), np.int32)
    for tl2 in range(PS):
        for h in range(4):
            yi[:, tl2 * 4 + h] = h * (PS * 512) + tl2 * 512 + r * 128 + p
    m["yidx"] = yi
    for l in range(NL):
        m.update(prep_A(inp, l, b, r))
        m.update(prep_B(inp, l, b))
    return m


_NC_CACHE = {}


def kernel(**inputs):
    inp = {k: np.asarray(v) for k, v in inputs.items()}
    x = inp["x"]
    S = x.shape[1]
    SC = S // 4
    xT = [np.ascontiguousarray(x[b].T.astype(np.float32)) for b in range(BATCH)]
    cores = list(range(8))
    if S not in _NC_CACHE:
        _NC_CACHE[S] = build_fused(S)
    in_maps = [prep_core(inp, c, xT, S) for c in cores]
    res = run_bass_kernel_spmd(_NC_CACHE[S], in_maps, core_ids=cores).results
    out = np.empty((BATCH, S, D), np.float32)
    for c in cores:
        b, r = c // 4, c % 4
        out[b, r * SC:(r + 1) * SC, :] = res[c]["xo"].T
    return out
```
